# Optimizing a Trainium2 kernel written in Bass

```python
import math
import jax, jax.numpy as jnp
from jax import lax
import numpy as np

D_MODEL = 1024
BATCH = 2
SEQ = 8192
DEPTH = 2

N_MIXERS = 2
N_HEADS = 8
HEAD_DIM = D_MODEL // N_HEADS
DIFF_HALF = HEAD_DIM // 2
MOBA_BLOCK = 256
MOBA_TOPK = 3
Q_BLOCK = 128
D_FF = -(-8 * D_MODEL // (3 * 256)) * 256
N_BUCKETS = 32
MAX_EXACT = N_BUCKETS // 2
MAX_DISTANCE = 2048
N_MOBA_LAYERS = (DEPTH + 1) // 2
N_DIFF_LAYERS = DEPTH // 2
RMS_EPS = 1e-6
NEG_INF = -1e30

kernel_name = "hybrid_moba_diffattn_sandwich_adaln"


def rms_norm(x, g):
    x32 = x.astype(jnp.float32)
    y = x32 * lax.rsqrt(jnp.mean(x32 * x32, axis=-1, keepdims=True) + RMS_EPS)
    return (y * g.astype(jnp.float32)).astype(x.dtype)


def t5_bucket(rel):
    n = jnp.maximum(rel, 0)
    nf = jnp.maximum(n, 1).astype(jnp.float32)
    large = MAX_EXACT + (jnp.log(nf / MAX_EXACT) / math.log(MAX_DISTANCE / MAX_EXACT)
                         * (N_BUCKETS - MAX_EXACT)).astype(jnp.int32)
    large = jnp.minimum(large, N_BUCKETS - 1)
    return jnp.where(n < MAX_EXACT, n, large)


def moba_attention(h, w_qkv, w_o, rel_table):
    B, S, _ = h.shape
    pad = (-S) % MOBA_BLOCK
    s_pad = S + pad
    nb = s_pad // MOBA_BLOCK
    nq = s_pad // Q_BLOCK
    k_sel_n = min(MOBA_TOPK, nb)
    q, k, v = jnp.split(h @ w_qkv, 3, axis=-1)

    def heads(t):
        t = t.reshape(B, S, N_HEADS, HEAD_DIM).transpose(0, 2, 1, 3)
        return jnp.pad(t, ((0, 0), (0, 0), (0, pad), (0, 0)))

    q = heads(q) * (HEAD_DIM ** -0.5)
    k, v = heads(k), heads(v)
    k_blk = k.reshape(B, N_HEADS, nb, MOBA_BLOCK, HEAD_DIM)
    v_blk = v.reshape(B, N_HEADS, nb, MOBA_BLOCK, HEAD_DIM)
    k_mean = jnp.mean(k_blk, axis=3)
    q_blocks = q.reshape(B, N_HEADS, nq, Q_BLOCK, HEAD_DIM).transpose(2, 0, 1, 3, 4)
    b_idx = jnp.arange(B)[:, None, None, None]
    h_idx = jnp.arange(N_HEADS)[None, :, None, None]
    table_t = rel_table.T

    def one_block(args):
        qb, qi = args
        q_pos = qi * Q_BLOCK + jnp.arange(Q_BLOCK)
        own = qi // (MOBA_BLOCK // Q_BLOCK)
        gate = jnp.einsum('bhqd,bhnd->bhqn', qb, k_mean).astype(jnp.float32)
        gate = jnp.where(jnp.arange(nb) < own, gate, NEG_INF)
        _, sel = lax.top_k(gate, k_sel_n)
        slot_valid = jnp.repeat(jnp.arange(k_sel_n) < own, MOBA_BLOCK)
        k_sel = k_blk[b_idx, h_idx, sel].reshape(B, N_HEADS, Q_BLOCK, k_sel_n * MOBA_BLOCK, HEAD_DIM)
        v_sel = v_blk[b_idx, h_idx, sel].reshape(B, N_HEADS, Q_BLOCK, k_sel_n * MOBA_BLOCK, HEAD_DIM)
        k_pos_sel = (sel[..., None] * MOBA_BLOCK + jnp.arange(MOBA_BLOCK)).reshape(
            B, N_HEADS, Q_BLOCK, k_sel_n * MOBA_BLOCK)
        bias_sel = table_t[h_idx, t5_bucket(q_pos[None, None, :, None] - k_pos_sel)]
        s_sel = jnp.einsum('bhqd,bhqkd->bhqk', qb, k_sel).astype(jnp.float32) + bias_sel.astype(jnp.float32)
        s_sel = jnp.where(slot_valid, s_sel, NEG_INF)
        k_own = lax.dynamic_slice_in_dim(k, own * MOBA_BLOCK, MOBA_BLOCK, axis=2)
        v_own = lax.dynamic_slice_in_dim(v, own * MOBA_BLOCK, MOBA_BLOCK, axis=2)
        k_pos_own = own * MOBA_BLOCK + jnp.arange(MOBA_BLOCK)
        rel_own = q_pos[:, None] - k_pos_own[None, :]
        bias_own = jnp.moveaxis(rel_table[t5_bucket(rel_own)], -1, 0)
        s_own = jnp.einsum('bhqd,bhkd->bhqk', qb, k_own).astype(jnp.float32) + bias_own[None].astype(jnp.float32)
        s_own = jnp.where(rel_own >= 0, s_own, NEG_INF)
        p = jax.nn.softmax(jnp.concatenate([s_sel, s_own], axis=-1), axis=-1).astype(v.dtype)
        n_sel = k_sel_n * MOBA_BLOCK
        return (jnp.einsum('bhqk,bhqkd->bhqd', p[..., :n_sel], v_sel)
                + jnp.einsum('bhqk,bhkd->bhqd', p[..., n_sel:], v_own))

    o = lax.map(one_block, (q_blocks, jnp.arange(nq)))
    o = o.transpose(1, 0, 3, 2, 4).reshape(B, s_pad, D_MODEL)[:, :S]
    return o @ w_o


def diff_attention(h, w_qkv, w_o, lam, subln_g, rel_table, layer_idx):
    B, S, _ = h.shape
    nq = S // Q_BLOCK
    lambda_init = 0.8 - 0.6 * math.exp(-0.3 * layer_idx)
    q, k, v = jnp.split(h @ w_qkv, 3, axis=-1)
    q = q.reshape(B, S, N_HEADS, 2, DIFF_HALF).transpose(0, 2, 3, 1, 4) * (DIFF_HALF ** -0.5)
    k = k.reshape(B, S, N_HEADS, 2, DIFF_HALF).transpose(0, 2, 3, 1, 4)
    v = v.reshape(B, S, N_HEADS, HEAD_DIM).transpose(0, 2, 1, 3)
    lam32 = lam.astype(jnp.float32)
    lam_full = (jnp.exp(jnp.sum(lam32[0] * lam32[1])) - jnp.exp(jnp.sum(lam32[2] * lam32[3]))
                + lambda_init)
    q_blocks = q.reshape(B, N_HEADS, 2, nq, Q_BLOCK, DIFF_HALF).transpose(3, 0, 1, 2, 4, 5)
    k_pos = jnp.arange(S)

    def one_block(args):
        qb, qi = args
        q_pos = qi * Q_BLOCK + jnp.arange(Q_BLOCK)
        rel = q_pos[:, None] - k_pos[None, :]
        bias = jnp.moveaxis(rel_table[t5_bucket(rel)], -1, 0)
        s = jnp.einsum('bhcqd,bhckd->bhcqk', qb, k).astype(jnp.float32) + bias[None, :, None].astype(jnp.float32)
        s = jnp.where(rel >= 0, s, NEG_INF)
        p = jax.nn.softmax(s, axis=-1)
        a = p[:, :, 0] - lam_full * p[:, :, 1]
        return jnp.einsum('bhqk,bhkd->bhqd', a.astype(v.dtype), v)

    o = lax.map(one_block, (q_blocks, jnp.arange(nq)))
    o = o.transpose(1, 2, 0, 3, 4).reshape(B, N_HEADS, S, HEAD_DIM)
    o = rms_norm(o, subln_g) * (1.0 - lambda_init)
    o = o.transpose(0, 2, 1, 3).reshape(B, S, D_MODEL)
    return o @ w_o


def swiglu(h, w_in, w_out):
    g, u = jnp.split(h @ w_in, 2, axis=-1)
    return (jax.nn.silu(g) * u) @ w_out


def setup_inputs(seed: int = 0) -> dict:
    key = jax.random.key(seed)
    ks = jax.random.split(key, 16)
    f32 = jnp.float32
    D = D_MODEL
    return {
        "x": jax.random.normal(ks[0], (BATCH, SEQ, D), f32),
        "c": jax.random.normal(ks[1], (BATCH, D), f32),
        "rel_bias": 0.5 * jax.random.normal(ks[2], (N_BUCKETS, N_HEADS), f32),
        "ada_w": jax.random.normal(ks[3], (DEPTH, D, 6 * D), f32) * D ** -0.5,
        "ada_b": 0.01 * jax.random.normal(ks[4], (DEPTH, 6 * D), f32),
        "norm_g": 1.0 + 0.1 * jax.random.normal(ks[5], (DEPTH, 4, D), f32),
        "moba_w_qkv": jax.random.normal(ks[6], (N_MOBA_LAYERS, D, 3 * D), f32) * D ** -0.5,
        "moba_w_o": jax.random.normal(ks[7], (N_MOBA_LAYERS, D, D), f32) * D ** -0.5,
        "diff_w_qkv": jax.random.normal(ks[8], (N_DIFF_LAYERS, D, 3 * D), f32) * D ** -0.5,
        "diff_w_o": jax.random.normal(ks[9], (N_DIFF_LAYERS, D, D), f32) * D ** -0.5,
        "diff_lambda": 0.1 * jax.random.normal(ks[10], (N_DIFF_LAYERS, 4, DIFF_HALF), f32),
        "diff_subln_g": 1.0 + 0.1 * jax.random.normal(ks[11], (N_DIFF_LAYERS, HEAD_DIM), f32),
        "ffn_w_in": jax.random.normal(ks[12], (DEPTH, D, 2 * D_FF), f32) * D ** -0.5,
        "ffn_w_out": jax.random.normal(ks[13], (DEPTH, D_FF, D), f32) * D_FF ** -0.5,
    }


def reference(x, c, rel_bias, ada_w, ada_b, norm_g, moba_w_qkv, moba_w_o, diff_w_qkv, diff_w_o,
              diff_lambda, diff_subln_g, ffn_w_in, ffn_w_out):
    c_act = jax.nn.silu(c)
    for i in range(DEPTH):
        mod = (c_act @ ada_w[i] + ada_b[i])[:, None, :]
        sh_a, sc_a, g_a, sh_f, sc_f, g_f = jnp.split(mod, 6, axis=-1)
        h = rms_norm(x, norm_g[i, 0]) * (1.0 + sc_a) + sh_a
        if i % N_MIXERS == 0:
            y = moba_attention(h, moba_w_qkv[i // 2], moba_w_o[i // 2], rel_bias)
        else:
            y = diff_attention(h, diff_w_qkv[i // 2], diff_w_o[i // 2], diff_lambda[i // 2],
                               diff_subln_g[i // 2], rel_bias, i)
        x = x + g_a * rms_norm(y, norm_g[i, 1])
        h = rms_norm(x, norm_g[i, 2]) * (1.0 + sc_f) + sh_f
        x = x + g_f * rms_norm(swiglu(h, ffn_w_in[i], ffn_w_out[i]), norm_g[i, 3])
    return x
```

```python
import math
from contextlib import ExitStack
import numpy as np
import ml_dtypes
import concourse.bass as bass
import concourse.mybir as mybir
from concourse.bass_utils import run_bass_kernel_spmd

F32 = mybir.dt.float32
BF16 = mybir.dt.bfloat16
I32 = mybir.dt.int32
ACT = mybir.ActivationFunctionType
ALU = mybir.AluOpType
AX = mybir.AxisListType

D = 1024
SEQ = 8192
NH = 8
DFF = 2816
NJ = DFF // 128
TOK = 2048
NCORES = 8
EPS = 1e-6
OFF = 384
EW = 2432
NEAR_DMAX = 1536
LAMBDA_INIT = 0.8 - 0.6 * math.exp(-0.3 * 1)

ENGS = ("tensor", "vector", "scalar", "gpsimd", "sync")
SEM_CAP = 2048
DMA_RING = 8


class Ins:
    __slots__ = ("eng", "fn", "dma", "raw", "oth", "idx", "sig", "dslot", "dval", "waiters", "cc")


class Prog:
    def __init__(self, nc):
        self.nc = nc
        self.ins = []
        self.last_w = {}
        self.rd_eng = {}
        self.rd_dma = {}

    def op(self, eng, fn, reads=(), writes=(), dma=False, cc=False):
        i = Ins()
        i.eng, i.fn, i.dma, i.cc = eng, fn, dma or cc, cc
        i.idx = len(self.ins)
        i.raw, i.oth = set(), set()
        i.sig = None
        i.waiters = False
        for r in reads:
            w = self.last_w.get(r)
            if w is not None:
                i.raw.add(w)
        for r in writes:
            w = self.last_w.get(r)
            if w is not None:
                i.oth.add(w)
            for rd in self.rd_eng.get(r, {}).values():
                i.oth.add(rd)
            for rd in self.rd_dma.get(r, ()):
                i.oth.add(rd)
        for r in reads:
            if i.dma:
                self.rd_dma.setdefault(r, []).append(i.idx)
            else:
                self.rd_eng.setdefault(r, {})[eng] = i.idx
        for r in writes:
            self.last_w[r] = i.idx
            self.rd_eng[r] = {}
            self.rd_dma[r] = []
        i.oth -= i.raw
        i.oth.discard(i.idx)
        i.raw.discard(i.idx)
        self.ins.append(i)
        return i

    def _needed(self, i):
        out = []
        for kind, ds in (("raw", i.raw), ("oth", i.oth)):
            for d in ds:
                dd = self.ins[d]
                if dd.dma or dd.eng != i.eng or i.dma:
                    out.append(d)
                elif i.eng == "tensor":
                    continue
                elif kind == "raw":
                    out.append(d)
        return out

    def emit(self):
        nc = self.nc
        ins = self.ins
        need = [self._needed(i) for i in ins]
        for i, nd in zip(ins, need):
            for d in nd:
                ins[d].waiters = True
        sigcount = {e: 0 for e in ENGS}
        dmacount = {e: 0 for e in ENGS}
        ncc = 0
        for i in ins:
            if i.cc:
                i.sig = ncc
                ncc += 1
            elif i.dma:
                n = dmacount[i.eng]
                dmacount[i.eng] += 1
                i.dslot = n % DMA_RING
                i.dval = 16 * (n // DMA_RING + 1)
                i.sig = n
            elif i.waiters:
                i.sig = sigcount[i.eng]
                sigcount[i.eng] += 1
        with ExitStack() as st:
            esems, dsems = {}, {}
            for e in ENGS:
                k = (sigcount[e] + SEM_CAP - 1) // SEM_CAP
                esems[e] = [st.enter_context(nc.semaphore(f"s_{e}_{j}")) for j in range(k)]
                k = min(DMA_RING, dmacount[e])
                dsems[e] = [st.enter_context(nc.semaphore(f"d_{e}_{j}")) for j in range(k)]
            ccsems = [st.enter_context(nc.semaphore(f"cc_{j}")) for j in range(ncc)]
            block = st.enter_context(nc.Block())

            def target(d):
                dd = ins[d]
                if dd.cc:
                    return (ccsems[dd.sig], 1)
                if dd.dma:
                    return (dsems[dd.eng][dd.dslot], dd.dval)
                return (esems[dd.eng][dd.sig // SEM_CAP], dd.sig % SEM_CAP + 1)

            def run(ename, eng):
                seen = {}
                for i, nd in zip(ins, need):
                    if i.eng != ename:
                        continue
                    waits = {}
                    for d in nd:
                        s, v = target(d)
                        key = id(s)
                        if seen.get(key, 0) >= v:
                            continue
                        if key not in waits or waits[key][1] < v:
                            waits[key] = (s, v)
                    if i.dma and not i.cc and i.sig >= DMA_RING:
                        s = dsems[ename][i.dslot]
                        v = i.dval - 16
                        key = id(s)
                        if seen.get(key, 0) < v and (key not in waits or waits[key][1] < v):
                            waits[key] = (s, v)
                    for key, (s, v) in waits.items():
                        eng.wait_ge(s, v)
                        seen[key] = v
                    r = i.fn(eng)
                    if i.cc:
                        r.then_inc(ccsems[i.sig])
                    elif i.dma:
                        r.then_inc(dsems[ename][i.dslot], 16)
                    elif i.sig is not None:
                        r.then_inc(esems[ename][i.sig // SEM_CAP], 1)
                n = dmacount[ename]
                for slot in range(min(DMA_RING, n)):
                    uses = (n - slot + DMA_RING - 1) // DMA_RING
                    s = dsems[ename][slot]
                    if seen.get(id(s), 0) < 16 * uses:
                        eng.wait_ge(s, 16 * uses)
                if ename == "gpsimd":
                    for s in ccsems:
                        if seen.get(id(s), 0) < 1:
                            eng.wait_ge(s, 1)

            block.tensor(lambda e: run("tensor", e))
            block.vector(lambda e: run("vector", e))
            block.scalar(lambda e: run("scalar", e))
            block.gpsimd(lambda e: run("gpsimd", e))
            block.sync(lambda e: run("sync", e))
        return dict(sig=sigcount, dma=dmacount, n=len(ins))


def I(m, *a, **k):
    return lambda e: getattr(e, m)(*a, **k)


class Ctx:
    def __init__(self, nc, st):
        self.nc, self.st = nc, st
        self.P = Prog(nc)
        self.pb = [st.enter_context(nc.psum_tensor(f"pb{i}", [128, 512], F32)) for i in range(8)]

    def sb(self, name, shape, dt):
        return self.st.enter_context(self.nc.sbuf_tensor(name, shape, dt))

    def consts(self):
        P = self.P
        self.ident = self.sb("ident", [128, 128], BF16)
        P.op("gpsimd", I("memset", self.ident[:], 1.0), writes=["ident"])
        P.op("gpsimd", I("affine_select", out=self.ident[:], in_=self.ident[:], pattern=[[-1, 128]],
                         compare_op=ALU.is_equal, fill=0.0, base=0, channel_multiplier=1),
             reads=["ident"], writes=["ident"])
        self.eps = self.sb("eps", [128, 1], F32)
        P.op("vector", I("memset", self.eps[:], EPS), writes=["eps"])


def emit_mod(C, cT, adaw, adab, ng):
    nc, P = C.nc, C.P
    cT_sb = C.sb("cT_sb", [128, 8], F32)
    cact = C.sb("cact", [128, 8], F32)
    ones = C.sb("ones_f", [128, 128], F32)
    cbc = C.sb("cbc", [128, 8, 128], F32)
    shb = C.sb("shb", [128, 2, D], F32)
    gbc = C.sb("gbc", [128, 4, D], F32)
    CW = 256
    wch = [C.sb(f"adaw_ch{i}", [128, 8, CW], F32) for i in range(2)]
    bch = [C.sb(f"adab_ch{i}", [128, CW], F32) for i in range(2)]
    mtmp = [C.sb(f"mtmp{i}", [128, CW], F32) for i in range(2)]
    P.op("sync", I("dma_start", out=cT_sb[:], in_=cT), writes=["cT_sb"], dma=True)
    P.op("sync", I("dma_start", out=gbc[:].rearrange("p a d -> p (a d)"),
                   in_=ng.rearrange("(o a) d -> o (a d)", o=1).partition_broadcast(128)), writes=["gbc"], dma=True)
    P.op("scalar", I("activation", out=cact[:], in_=cT_sb[:], func=ACT.Silu), reads=["cT_sb"], writes=["cact"])
    P.op("vector", I("memset", ones[:], 1.0), writes=["ones_f"])
    for k in range(8):
        P.op("vector", I("tensor_scalar", out=cbc[:, k, :], in0=ones[:], scalar1=cact[:, k:k + 1], scalar2=None,
                         op0=ALU.mult), reads=["ones_f", "cact"], writes=["cbc"])
    adaw_v = adaw.rearrange("(k p) n -> p k n", p=128)
    for n in range(6 * D // CW):
        s = n % 2
        piece, c0 = (n * CW) // D, (n * CW) % D
        P.op("sync", I("dma_start", out=wch[s][:], in_=adaw_v[:, :, n * CW:(n + 1) * CW]),
             writes=[f"adaw_ch{s}"], dma=True)
        P.op("sync", I("dma_start", out=bch[s][:], in_=adab[0:1, n * CW:(n + 1) * CW].partition_broadcast(128)),
             writes=[f"adab_ch{s}"], dma=True)
        bank = 6 + s
        for k in range(8):
            P.op("tensor", I("matmul", C.pb[bank][:, 0:CW], lhsT=cbc[:, k, :], rhs=wch[s][:, k, :], start=(k == 0),
                             stop=(k == 7)), reads=["cbc", f"adaw_ch{s}"], writes=[f"pb{bank}"])
        if piece in (0, 3):
            P.op("vector", I("tensor_tensor", out=shb[:, piece // 3, c0:c0 + CW], in0=C.pb[bank][:, 0:CW], in1=bch[s][:],
                             op=ALU.add), reads=[f"pb{bank}", f"adab_ch{s}"], writes=["shb"])
        else:
            P.op("vector", I("tensor_tensor", out=mtmp[s][:], in0=C.pb[bank][:, 0:CW], in1=bch[s][:], op=ALU.add),
                 reads=[f"pb{bank}", f"adab_ch{s}"], writes=[f"mtmp{s}"])
            gi = {1: 0, 2: 1, 4: 2, 5: 3}[piece]
            if piece in (1, 4):
                P.op("vector", I("scalar_tensor_tensor", out=gbc[:, gi, c0:c0 + CW], in0=mtmp[s][:], scalar=1.0,
                                 in1=gbc[:, gi, c0:c0 + CW], op0=ALU.add, op1=ALU.mult),
                     reads=[f"mtmp{s}", "gbc"], writes=["gbc"])
            else:
                P.op("vector", I("tensor_tensor", out=gbc[:, gi, c0:c0 + CW], in0=mtmp[s][:], in1=gbc[:, gi, c0:c0 + CW],
                                 op=ALU.mult), reads=[f"mtmp{s}", "gbc"], writes=["gbc"])
    return shb, gbc


def emit_rstd(C, ss, rstd, n, res_r, res_w):
    P = C.P
    P.op("scalar", I("activation", out=rstd, in_=ss, func=ACT.Ln, scale=1.0 / n, bias=C.eps[:]),
         reads=list(res_r) + ["eps"], writes=res_w)
    P.op("scalar", I("activation", out=rstd, in_=rstd, func=ACT.Exp, scale=-0.5), reads=res_w, writes=res_w)


def emit_norm_mod_T(C, xt, xres, A, B, AB_res, hTg, hTg_res, blk, uid):
    P = C.P
    W = C.w
    s = uid % 2
    junk, ss, rstd, tmp, hbf = W["junk"], W["ss"][s], W["rstd"][s], W["tmp"][s], W["hbf"][s]
    P.op("vector", I("memset", ss[:], 0.0), writes=[f"ss{s}"])
    P.op("scalar", I("activation", out=junk[:], in_=xt, func=ACT.Square, accum_out=ss[:]),
         reads=[xres, f"ss{s}"], writes=["junk", f"ss{s}"])
    emit_rstd(C, ss[:], rstd[:], D, [f"ss{s}"], [f"rstd{s}"])
    P.op("vector", I("scalar_tensor_tensor", out=tmp[:], in0=xt, scalar=rstd[:], in1=A, op0=ALU.mult, op1=ALU.mult),
         reads=[xres, f"rstd{s}", AB_res], writes=[f"tmp{s}"])
    P.op("gpsimd", I("tensor_tensor", out=hbf[:], in0=tmp[:], in1=B, op=ALU.add),
         reads=[f"tmp{s}", AB_res], writes=[f"hbf{s}"])
    bank = 4 + s
    pT = C.pb[bank][:].bitcast(BF16)
    for k in range(8):
        P.op("tensor", I("transpose", out=pT[:, k * 128:(k + 1) * 128], in_=hbf[:, k * 128:(k + 1) * 128],
                         identity=C.ident[:]), reads=[f"hbf{s}", "ident"], writes=[f"pb{bank}"])
    P.op("scalar", I("copy", out=hTg[:, :, blk * 128:(blk + 1) * 128],
                     in_=pT.rearrange("p (k t) -> p k t", k=8)), reads=[f"pb{bank}"], writes=[hTg_res])


def alloc_work(C):
    C.w = dict(
        junk=C.sb("junk", [128, D], BF16),
        ss=[C.sb(f"ss{i}", [128, 1], F32) for i in range(2)],
        rstd=[C.sb(f"rstd{i}", [128, 1], F32) for i in range(2)],
        tmp=[C.sb(f"tmp{i}", [128, D], F32) for i in range(2)],
        hbf=[C.sb(f"hbf{i}", [128, D], BF16) for i in range(2)],
    )


def build_pre():
    nc = bass.Bass("TRN2", target_bir_lowering=False)
    x_in = nc.dram_tensor("x", [TOK, D], F32, kind="ExternalInput").ap()
    cT = nc.dram_tensor("cT", [128, 8], F32, kind="ExternalInput").ap()
    adaw = nc.dram_tensor("adaw", [D, 6 * D], F32, kind="ExternalInput").ap()
    adab = nc.dram_tensor("adab", [1, 6 * D], F32, kind="ExternalInput").ap()
    ng = nc.dram_tensor("ng", [4, D], F32, kind="ExternalInput").ap()
    hT_out = nc.dram_tensor("hT", [8, 128, TOK], BF16, kind="ExternalOutput").ap()
    with ExitStack() as st:
        C = Ctx(nc, st)
        P = C.P
        C.consts()
        alloc_work(C)
        shb, gbc = emit_mod(C, cT, adaw, adab, ng)
        xt = [C.sb(f"xt{i}", [128, D], F32) for i in range(2)]
        hTg = [C.sb(f"hTg{i}", [128, 8, 512], BF16) for i in range(2)]
        for tt in range(TOK // 128):
            s = tt % 2
            g, blk = tt // 4, tt % 4
            P.op("sync", I("dma_start", out=xt[s][:], in_=x_in[tt * 128:(tt + 1) * 128, :]), writes=[f"xt{s}"], dma=True)
            emit_norm_mod_T(C, xt[s][:], f"xt{s}", gbc[:, 0, :], shb[:, 0, :], "gbc", hTg[g % 2], f"hTg{g % 2}", blk, tt)
            if blk == 3:
                P.op("sync", I("dma_start", out=hT_out[:, :, g * 512:(g + 1) * 512].rearrange("k p t -> p k t"),
                               in_=hTg[g % 2][:]), reads=[f"hTg{g % 2}"], dma=True)
        print("pre", P.emit())
    return nc


def t5_lo():
    n = np.arange(0, 4096)
    nf = np.maximum(n, 1).astype(np.float32)
    large = 16 + (np.log(nf / np.float32(16)) / np.float32(math.log(2048 / 16)) * np.float32(16)).astype(np.int32)
    large = np.minimum(large, 31)
    b = np.where(n < 16, n, large)
    lo = [int(np.argmax(b == k)) for k in range(32)]
    assert all((b == k).any() for k in range(32))
    assert lo[31] <= NEAR_DMAX + 128 - 127, lo
    return lo


def build_attn(kind):
    moba = kind == "moba"
    nc = bass.Bass("TRN2", target_bir_lowering=False)
    hT_all = nc.dram_tensor("hT_all", [4, 8, 128, TOK], BF16, kind="ExternalInput").ap()
    wqkv = nc.dram_tensor("wqkv", [D, 768], F32, kind="ExternalInput").ap()
    relbT = nc.dram_tensor("relbT", [1, 64], F32, kind="ExternalInput").ap()
    if not moba:
        lam = nc.dram_tensor("lam", [1, 256], F32, kind="ExternalInput").ap()
        subg = nc.dram_tensor("subg", [1, 128], F32, kind="ExternalInput").ap()
    oT = nc.dram_tensor("oT", [2, 128, SEQ], BF16, kind="ExternalOutput").ap()
    Gd_t = nc.dram_tensor("Gd", [2, 2560], F32)
    Gd = Gd_t.ap()
    lo = t5_lo()
    DK = 128 if moba else 64
    scale = DK ** -0.5
    with ExitStack() as st:
        C = Ctx(nc, st)
        P = C.P
        C.consts()
        wsb = C.sb("wsb", [128, 8, 768], BF16)
        P.op("gpsimd", I("dma_start", out=wsb[:], in_=wqkv.rearrange("(k p) n -> p k n", p=128)), writes=["wsb"], dma=True)
        tab = C.sb("tab", [128, 2, 32], F32)
        etab = C.sb("etab", [128, 2, 32], F32)
        cdf = C.sb("cdf", [128, 2, 32], F32)
        P.op("sync", I("dma_start", out=tab[:].rearrange("p a b -> p (a b)"), in_=relbT.partition_broadcast(128)),
             writes=["tab"], dma=True)
        P.op("scalar", I("activation", out=etab[:], in_=tab[:], func=ACT.Exp), reads=["tab"], writes=["etab"])
        P.op("vector", I("tensor_copy", out=cdf[:, :, 0:1], in_=etab[:, :, 0:1]), reads=["etab"], writes=["cdf"])
        P.op("vector", I("tensor_tensor", out=cdf[:, :, 1:32], in0=etab[:, :, 1:32], in1=etab[:, :, 0:31],
                         op=ALU.subtract), reads=["etab"], writes=["cdf"])
        reli = C.sb("reli", [128, 20], I32)
        relf = C.sb("relf", [128, 20], F32)
        Gt = C.sb("Gt", [128, 2, 20], F32)
        Gtmp = C.sb("Gtmp", [128, 20], F32)
        P.op("gpsimd", I("iota", reli[:], pattern=[[1, 20]], base=-511, channel_multiplier=20), writes=["reli"])
        P.op("vector", I("tensor_copy", out=relf[:], in_=reli[:]), reads=["reli"], writes=["relf"])
        P.op("vector", I("memset", Gt[:], 0.0), writes=["Gt"])
        for hh in range(2):
            for b in range(32):
                P.op("vector", I("tensor_scalar", out=Gtmp[:], in0=relf[:], scalar1=float(lo[b]),
                                 scalar2=cdf[:, hh, b:b + 1], op0=ALU.is_ge, op1=ALU.mult),
                     reads=["relf", "cdf"], writes=["Gtmp"])
                P.op("vector", I("tensor_tensor", out=Gt[:, hh, :], in0=Gt[:, hh, :], in1=Gtmp[:], op=ALU.add),
                     reads=["Gt", "Gtmp"], writes=["Gt"])
        P.op("sync", I("dma_start", out=Gd.rearrange("a (p j) -> p a j", j=20), in_=Gt[:]), reads=["Gt"],
             writes=["Gd"], dma=True)
        hank = C.sb("hank", [128, EW], BF16)
        antiI = C.sb("antiI", [128, 128], BF16)
        E = C.sb("E", [128, 2, EW], BF16)
        P.op("gpsimd", I("memset", antiI[:], 1.0), writes=["antiI"])
        P.op("gpsimd", I("affine_select", out=antiI[:], in_=antiI[:], pattern=[[1, 128]], compare_op=ALU.is_equal,
                         fill=0.0, base=-127, channel_multiplier=1), reads=["antiI"], writes=["antiI"])
        for hh in range(2):
            src = bass.AP(Gd_t, hh * 2560, [[1, 128], [1, EW]])
            P.op("gpsimd", I("dma_start", out=hank[:], in_=src), reads=["Gd"], writes=["hank"], dma=True)
            for c0 in range(0, EW, 512):
                w = min(512, EW - c0)
                P.op("tensor", I("matmul", C.pb[7][:, 0:w], lhsT=antiI[:], rhs=hank[:, c0:c0 + w], start=True, stop=True),
                     reads=["antiI", "hank"], writes=["pb7"])
                P.op("vector", I("tensor_copy", out=E[:, hh, c0:c0 + w], in_=C.pb[7][:, 0:w]), reads=["pb7"], writes=["E"])
        b31 = C.sb("b31", [128, 2], F32)
        P.op("vector", I("tensor_copy", out=b31[:], in_=tab[:, :, 31]), reads=["tab"], writes=["b31"])
        if moba:
            selM = C.sb("selM", [32, 32 * 128], BF16)
            P.op("gpsimd", I("memset", selM[:], 1.0), writes=["selM"])
            P.op("gpsimd", I("affine_select", out=selM[:], in_=selM[:], pattern=[[1, 4096]], compare_op=ALU.is_ge,
                             fill=0.0, base=0, channel_multiplier=-128), reads=["selM"], writes=["selM"])
            P.op("gpsimd", I("affine_select", out=selM[:], in_=selM[:], pattern=[[-1, 4096]], compare_op=ALU.is_ge,
                             fill=0.0, base=127, channel_multiplier=128), reads=["selM"], writes=["selM"])
            kmf = C.sb("kmf", [128, 32], F32)
            kmb = C.sb("kmb", [128, 32], BF16)
            gate = [C.sb(f"gate{i}", [128, 32], F32) for i in range(2)]
            m8 = [C.sb(f"m8{i}", [128, 8], F32) for i in range(2)]
            sel = [C.sb(f"sel{i}", [128, 32], F32) for i in range(2)]
            nmq = [C.sb(f"nmq{i}", [128, 32], BF16) for i in range(2)]
            nmT = [C.sb(f"nmT{i}", [32, 512], BF16) for i in range(2)]
        else:
            lam_sb = C.sb("lam_sb", [128, 256], F32)
            lp = C.sb("lp", [128, 128], F32)
            ls = C.sb("ls", [128, 2], F32)
            le = C.sb("le", [128, 2], F32)
            neglam = C.sb("neglam", [128, 1], F32)
            subg_bc = C.sb("subg_bc", [128, 128], F32)
            P.op("sync", I("dma_start", out=lam_sb[:], in_=lam.partition_broadcast(128)), writes=["lam_sb"], dma=True)
            P.op("sync", I("dma_start", out=subg_bc[:], in_=subg.partition_broadcast(128)), writes=["subg_bc"], dma=True)
            lv = lam_sb[:].rearrange("p (a b c) -> p a b c", a=2, b=2)
            P.op("vector", I("tensor_tensor", out=lp[:].rearrange("p (a c) -> p a c", a=2), in0=lv[:, :, 0, :],
                             in1=lv[:, :, 1, :], op=ALU.mult), reads=["lam_sb"], writes=["lp"])
            P.op("vector", I("tensor_reduce", out=ls[:], in_=lp[:].rearrange("p (a c) -> p a c", a=2), axis=AX.X,
                             op=ALU.add), reads=["lp"], writes=["ls"])
            P.op("scalar", I("activation", out=le[:], in_=ls[:], func=ACT.Exp), reads=["ls"], writes=["le"])
            P.op("vector", I("tensor_tensor", out=neglam[:], in0=le[:, 1:2], in1=le[:, 0:1], op=ALU.subtract),
                 reads=["le"], writes=["neglam"])
            P.op("vector", I("tensor_scalar", out=neglam[:], in0=neglam[:], scalar1=-LAMBDA_INIT, scalar2=None,
                             op0=ALU.add), reads=["neglam"], writes=["neglam"])
            P.op("vector", I("tensor_scalar", out=subg_bc[:], in0=subg_bc[:], scalar1=1.0 - LAMBDA_INIT, scalar2=None,
                             op0=ALU.mult), reads=["subg_bc"], writes=["subg_bc"])
            t1 = [C.sb(f"t1_{i}", [128, 128], F32) for i in range(2)]
            of = [C.sb(f"of{i}", [128, 128], F32) for i in range(2)]
            junk = C.sb("junkd", [128, 128], BF16)
            ssd = [C.sb(f"ssd{i}", [128, 1], F32) for i in range(2)]
            rsd = [C.sb(f"rsd{i}", [128, 1], F32) for i in range(2)]
        QT = C.sb("QT", [128, SEQ], BF16)
        KT = C.sb("KT", [128, SEQ], BF16)
        Vp = C.sb("Vp", [128, 64, 130], BF16)
        hTg = [C.sb(f"hTg{i}", [128, 8, 512], BF16) for i in range(2)]
        NMAP = 1 if moba else 2
        PT = [C.sb(f"PT{i}", [128, 512], BF16) for i in range(4)]
        rec = [C.sb(f"rec{i}", [128, 2], F32) for i in range(2)]
        obf = [C.sb(f"obf{i}", [128, 128], BF16) for i in range(2)]
        OTg = [C.sb(f"OTg{i}", [128, 512], BF16) for i in range(2)]
        uid = 0
        for hh in range(2):
            P.op("vector", I("memset", Vp[:, :, 128:129], 1.0), reads=[], writes=["Vp"])
            for g in range(16):
                s = g % 2
                r, off = g // 4, (g % 4) * 512
                P.op("sync", I("dma_start", out=hTg[s][:], in_=hT_all[r, :, :, off:off + 512].rearrange("k p t -> p k t")),
                     writes=[f"hTg{s}"], dma=True)
                for which, dst, eng in ((0, QT, "scalar"), (1, KT, "vector")):
                    bank = which
                    c0 = hh * 384 + which * 128
                    for k in range(8):
                        P.op("tensor", I("matmul", C.pb[bank][:], lhsT=wsb[:, k, c0:c0 + 128], rhs=hTg[s][:, k, :],
                                         start=(k == 0), stop=(k == 7)), reads=["wsb", f"hTg{s}"], writes=[f"pb{bank}"])
                    if eng == "scalar":
                        P.op("scalar", I("copy", out=dst[:, g * 512:(g + 1) * 512], in_=C.pb[bank][:]),
                             reads=[f"pb{bank}"], writes=["QT"])
                    else:
                        P.op("vector", I("tensor_copy", out=dst[:, g * 512:(g + 1) * 512], in_=C.pb[bank][:]),
                             reads=[f"pb{bank}"], writes=["KT"])
                c0 = hh * 384 + 256
                for i in range(4):
                    for k in range(8):
                        P.op("tensor", I("matmul", C.pb[2][:, i * 128:(i + 1) * 128], lhsT=hTg[s][:, k, i * 128:(i + 1) * 128],
                                         rhs=wsb[:, k, c0:c0 + 128], start=(k == 0), stop=(k == 7)),
                             reads=["wsb", f"hTg{s}"], writes=["pb2"])
                P.op("vector", I("tensor_copy", out=Vp[:, g * 4:(g + 1) * 4, 0:128],
                                 in_=C.pb[2][:].rearrange("p (i d) -> p i d", i=4)), reads=["pb2"], writes=["Vp"])
            if moba:
                P.op("vector", I("tensor_reduce", out=kmf[:], in_=KT[:].rearrange("p (b t) -> p b t", t=256), axis=AX.X,
                                 op=ALU.add), reads=["KT"], writes=["kmf"])
                P.op("vector", I("tensor_copy", out=kmb[:], in_=kmf[:]), reads=["kmf"], writes=["kmb"])
            for qg in range(16):
                q0 = qg * 512
                if moba:
                    ns = qg % 2
                    for i in range(4):
                        qt = qg * 4 + i
                        own = qt // 2
                        gs = uid % 2
                        uid += 1
                        if own <= 3:
                            P.op("vector", I("memset", sel[gs][:], 0.0), writes=[f"sel{gs}"])
                            P.op("vector", I("memset", sel[gs][:, 0:own + 1], 1.0), writes=[f"sel{gs}"])
                        else:
                            P.op("tensor", I("matmul", C.pb[7][:, 0:32], lhsT=QT[:, qt * 128:(qt + 1) * 128], rhs=kmb[:],
                                             start=True, stop=True), reads=["QT", "kmb"], writes=["pb7"])
                            P.op("vector", I("tensor_copy", out=gate[gs][:], in_=C.pb[7][:, 0:32]), reads=["pb7"],
                                 writes=[f"gate{gs}"])
                            P.op("vector", I("memset", gate[gs][:, own:32], -1e30), writes=[f"gate{gs}"])
                            P.op("vector", I("max", out=m8[gs][:], in_=gate[gs][:]), reads=[f"gate{gs}"], writes=[f"m8{gs}"])
                            P.op("vector", I("tensor_scalar", out=sel[gs][:], in0=gate[gs][:], scalar1=m8[gs][:, 2:3],
                                             scalar2=None, op0=ALU.is_ge), reads=[f"gate{gs}", f"m8{gs}"],
                                 writes=[f"sel{gs}"])
                            P.op("vector", I("memset", sel[gs][:, own:own + 1], 1.0), writes=[f"sel{gs}"])
                        P.op("vector", I("tensor_scalar", out=nmq[gs][:], in0=sel[gs][:], scalar1=-1.0, scalar2=30000.0,
                                         op0=ALU.add, op1=ALU.mult), reads=[f"sel{gs}"], writes=[f"nmq{gs}"])
                        pT7 = C.pb[7][:].bitcast(BF16)
                        P.op("tensor", I("transpose", out=pT7[0:32, 512:640], in_=nmq[gs][:], identity=C.ident[:]),
                             reads=[f"nmq{gs}", "ident"], writes=["pb7"])
                        P.op("vector", I("tensor_copy", out=nmT[ns][:, i * 128:(i + 1) * 128], in_=pT7[0:32, 512:640]),
                             reads=["pb7"], writes=[f"nmT{ns}"])
                nkt = 4 * qg + 4
                for kt in range(nkt):
                    Dq = q0 - kt * 128
                    near = Dq <= NEAR_DMAX
                    for mp in range(NMAP):
                        sb_ = uid % 3
                        ps_ = uid % 4
                        uid += 1
                        r0 = mp * 64 if not moba else 0
                        P.op("tensor", I("matmul", C.pb[sb_][:], lhsT=KT[r0:r0 + DK, kt * 128:(kt + 1) * 128],
                                         rhs=QT[r0:r0 + DK, q0:q0 + 512], start=True, stop=not moba),
                             reads=["KT", "QT"], writes=[f"pb{sb_}"])
                        if moba:
                            j = kt // 2
                            P.op("tensor", I("matmul", C.pb[sb_][:], lhsT=selM[:, j * 128:(j + 1) * 128], rhs=nmT[ns][:],
                                             start=False, stop=True), reads=["selM", f"nmT{ns}"], writes=[f"pb{sb_}"])
                        if near:
                            P.op("scalar", I("activation", out=PT[ps_][:], in_=C.pb[sb_][:], func=ACT.Exp, scale=scale),
                                 reads=[f"pb{sb_}"], writes=[f"PT{ps_}"])
                            P.op("vector", I("tensor_tensor", out=PT[ps_][:], in0=PT[ps_][:],
                                             in1=E[:, hh, Dq + OFF:Dq + OFF + 512], op=ALU.mult),
                                 reads=[f"PT{ps_}", "E"], writes=[f"PT{ps_}"])
                        else:
                            P.op("scalar", I("activation", out=PT[ps_][:], in_=C.pb[sb_][:], func=ACT.Exp, scale=scale,
                                             bias=b31[:, hh:hh + 1]), reads=[f"pb{sb_}", "b31"], writes=[f"PT{ps_}"])
                        for i in range(4):
                            if kt > 4 * qg + i:
                                continue
                            ob = 3 + 2 * mp + i // 2
                            oc = (i % 2) * 256
                            P.op("tensor", I("matmul", C.pb[ob][:, oc:oc + 129], lhsT=PT[ps_][:, i * 128:(i + 1) * 128],
                                             rhs=Vp[:, kt, 0:129], start=(kt == 0 and i % 2 == 0), stop=(kt == 4 * qg + i),
                                             skip_group_check=True),
                                 reads=[f"PT{ps_}", "Vp"], writes=[f"pb{ob}"])
                og = qg % 2
                pT7 = C.pb[7][:].bitcast(BF16)
                for i in range(4):
                    fs = uid % 2
                    uid += 1
                    ob0, oc = 3 + i // 2, (i % 2) * 256
                    P.op("vector", I("reciprocal", out=rec[fs][:, 0:1], in_=C.pb[ob0][:, oc + 128:oc + 129]),
                         reads=[f"pb{ob0}"], writes=[f"rec{fs}"])
                    if moba:
                        P.op("vector", I("tensor_scalar", out=obf[fs][:], in0=C.pb[ob0][:, oc:oc + 128],
                                         scalar1=rec[fs][:, 0:1], scalar2=None, op0=ALU.mult),
                             reads=[f"pb{ob0}", f"rec{fs}"], writes=[f"obf{fs}"])
                    else:
                        ob1 = 5 + i // 2
                        P.op("vector", I("reciprocal", out=rec[fs][:, 1:2], in_=C.pb[ob1][:, oc + 128:oc + 129]),
                             reads=[f"pb{ob1}"], writes=[f"rec{fs}"])
                        P.op("vector", I("tensor_tensor", out=rec[fs][:, 1:2], in0=rec[fs][:, 1:2], in1=neglam[:],
                                         op=ALU.mult), reads=[f"rec{fs}", "neglam"], writes=[f"rec{fs}"])
                        P.op("vector", I("tensor_scalar", out=t1[fs][:], in0=C.pb[ob0][:, oc:oc + 128],
                                         scalar1=rec[fs][:, 0:1], scalar2=None, op0=ALU.mult),
                             reads=[f"pb{ob0}", f"rec{fs}"], writes=[f"t1_{fs}"])
                        P.op("vector", I("scalar_tensor_tensor", out=of[fs][:], in0=C.pb[ob1][:, oc:oc + 128],
                                         scalar=rec[fs][:, 1:2], in1=t1[fs][:], op0=ALU.mult, op1=ALU.add),
                             reads=[f"pb{ob1}", f"rec{fs}", f"t1_{fs}"], writes=[f"of{fs}"])
                        P.op("vector", I("memset", ssd[fs][:], 0.0), writes=[f"ssd{fs}"])
                        P.op("scalar", I("activation", out=junk[:], in_=of[fs][:], func=ACT.Square, accum_out=ssd[fs][:]),
                             reads=[f"of{fs}", f"ssd{fs}"], writes=["junkd", f"ssd{fs}"])
                        emit_rstd(C, ssd[fs][:], rsd[fs][:], 128, [f"ssd{fs}"], [f"rsd{fs}"])
                        P.op("vector", I("scalar_tensor_tensor", out=obf[fs][:], in0=of[fs][:], scalar=rsd[fs][:],
                                         in1=subg_bc[:], op0=ALU.mult, op1=ALU.mult),
                             reads=[f"of{fs}", f"rsd{fs}", "subg_bc"], writes=[f"obf{fs}"])
                    P.op("tensor", I("transpose", out=pT7[:, i * 128:(i + 1) * 128], in_=obf[fs][:], identity=C.ident[:]),
                         reads=[f"obf{fs}", "ident"], writes=["pb7"])
                P.op("scalar", I("copy", out=OTg[og][:], in_=pT7[:, 0:512]), reads=["pb7"], writes=[f"OTg{og}"])
                P.op("sync", I("dma_start", out=oT[hh, :, q0:q0 + 512], in_=OTg[og][:]), reads=[f"OTg{og}"], dma=True)
        print("attn", kind, P.emit())
    return nc


def build_post():
    nc = bass.Bass("TRN2", target_bir_lowering=False)
    x_in = nc.dram_tensor("x", [TOK, D], F32, kind="ExternalInput").ap()
    OT = nc.dram_tensor("OT", [8, 128, TOK], BF16, kind="ExternalInput").ap()
    cT = nc.dram_tensor("cT", [128, 8], F32, kind="ExternalInput").ap()
    adaw = nc.dram_tensor("adaw", [D, 6 * D], F32, kind="ExternalInput").ap()
    adab = nc.dram_tensor("adab", [1, 6 * D], F32, kind="ExternalInput").ap()
    ng = nc.dram_tensor("ng", [4, D], F32, kind="ExternalInput").ap()
    wo = nc.dram_tensor("wo", [D, D], F32, kind="ExternalInput").ap()
    win_r = nc.dram_tensor("win_r", [NJ, 128, 2048], F32, kind="ExternalInput").ap()
    wout = nc.dram_tensor("wout", [DFF, D], F32, kind="ExternalInput").ap()
    x_out = nc.dram_tensor("x_out", [TOK, D], F32, kind="ExternalOutput").ap()
    with ExitStack() as st:
        C = Ctx(nc, st)
        P = C.P
        C.consts()
        alloc_work(C)
        shb, gbc = emit_mod(C, cT, adaw, adab, ng)
        wo_sb = C.sb("wo_sb", [128, 8, D], BF16)
        wout_sb = C.sb("wout_sb", [128, NJ, D], BF16)
        wo_v = wo.rearrange("(h p) n -> p h n", p=128)
        for h0 in range(0, 8, 2):
            P.op("gpsimd", I("dma_start", out=wo_sb[:, h0:h0 + 2, :], in_=wo_v[:, h0:h0 + 2, :]), writes=["wo_sb"], dma=True)
        wout_v = wout.rearrange("(j p) n -> p j n", p=128)
        for j0 in range(0, NJ, 2):
            P.op("gpsimd", I("dma_start", out=wout_sb[:, j0:j0 + 2, :], in_=wout_v[:, j0:j0 + 2, :]), writes=["wout_sb"],
                 dma=True)
        xg = [C.sb(f"xg{i}", [128, 4, D], F32) for i in range(1)] * 2
        OTg = [C.sb(f"OTg{i}", [128, 8, 512], BF16) for i in range(1)] * 2
        h2T = C.sb("h2T", [128, 8, 512], BF16)
        actT = C.sb("actT", [128, NJ, 512], BF16)
        wch = [C.sb(f"wch{i}", [128, 2, 8, 128], BF16) for i in range(3)]
        sg = [C.sb(f"sg{i}", [128, 512], F32) for i in range(2)]
        ssy = [C.sb(f"ssy{i}", [128, 2], F32) for i in range(2)]
        ss1 = [C.sb(f"ss1_{i}", [128, 1], F32) for i in range(2)]
        rsy = [C.sb(f"rsy{i}", [128, 1], F32) for i in range(2)]
        W = C.w
        uid = 0
        wcount = 0

        def resid_update(xtile, xres, banks, Grow, u):
            s = u % 2
            P.op("vector", I("memset", ssy[s][:], 0.0), writes=[f"ssy{s}"])
            for half in range(2):
                P.op("scalar", I("activation", out=W["junk"][:, 0:512], in_=C.pb[banks[half]][:], func=ACT.Square,
                                 accum_out=ssy[s][:, half:half + 1]), reads=[f"pb{banks[half]}", f"ssy{s}"],
                     writes=["junk", f"ssy{s}"])
            P.op("vector", I("tensor_tensor", out=ss1[s][:], in0=ssy[s][:, 0:1], in1=ssy[s][:, 1:2], op=ALU.add),
                 reads=[f"ssy{s}"], writes=[f"ss1_{s}"])
            emit_rstd(C, ss1[s][:], rsy[s][:], D, [f"ss1_{s}"], [f"rsy{s}"])
            for half in range(2):
                hs = slice(half * 512, (half + 1) * 512)
                P.op("vector", I("scalar_tensor_tensor", out=W["tmp"][s][:, hs], in0=C.pb[banks[half]][:], scalar=rsy[s][:],
                                 in1=Grow[:, hs], op0=ALU.mult, op1=ALU.mult),
                     reads=[f"pb{banks[half]}", f"rsy{s}", "gbc"], writes=[f"tmp{s}"])
            P.op("gpsimd", I("tensor_tensor", out=xtile, in0=xtile, in1=W["tmp"][s][:], op=ALU.add),
                 reads=[xres, f"tmp{s}"], writes=[xres])

        for tg in range(4):
            gs = 0
            P.op("sync", I("dma_start", out=xg[gs][:], in_=x_in[tg * 512:(tg + 1) * 512, :].rearrange("(i p) d -> p i d", p=128)),
                 writes=[f"xg{gs}"], dma=True)
            P.op("sync", I("dma_start", out=OTg[gs][:], in_=OT[:, :, tg * 512:(tg + 1) * 512].rearrange("h p t -> p h t")),
                 writes=[f"OTg{gs}"], dma=True)
            for i in range(4):
                banks = (0, 1) if i % 2 == 0 else (2, 3)
                for half in range(2):
                    for h in range(8):
                        P.op("tensor", I("matmul", C.pb[banks[half]][:], lhsT=OTg[gs][:, h, i * 128:(i + 1) * 128],
                                         rhs=wo_sb[:, h, half * 512:(half + 1) * 512], start=(h == 0), stop=(h == 7)),
                             reads=[f"OTg{gs}", "wo_sb"], writes=[f"pb{banks[half]}"])
                resid_update(xg[gs][:, i, :], f"xg{gs}", banks, gbc[:, 1, :], uid)
                uid += 1
                emit_norm_mod_T(C, xg[gs][:, i, :], f"xg{gs}", gbc[:, 2, :], shb[:, 1, :], "gbc", h2T, "h2T", i, uid)
                uid += 1
            for j in range(NJ):
                ws = wcount % 3
                wcount += 1
                P.op("gpsimd", I("dma_start", out=wch[ws][:].rearrange("p a k n -> p (a k n)"), in_=win_r[j]),
                     writes=[f"wch{ws}"], dma=True)
                bg, bu = (0, 1) if j % 2 == 0 else (2, 3)
                for a, bank in ((0, bg), (1, bu)):
                    for k in range(8):
                        P.op("tensor", I("matmul", C.pb[bank][:], lhsT=wch[ws][:, a, k, :], rhs=h2T[:, k, :], start=(k == 0),
                                         stop=(k == 7)), reads=[f"wch{ws}", "h2T"], writes=[f"pb{bank}"])
                s2 = j % 2
                P.op("scalar", I("activation", out=sg[s2][:], in_=C.pb[bg][:], func=ACT.Silu), reads=[f"pb{bg}"],
                     writes=[f"sg{s2}"])
                P.op("vector", I("tensor_tensor", out=actT[:, j, :], in0=sg[s2][:], in1=C.pb[bu][:], op=ALU.mult),
                     reads=[f"sg{s2}", f"pb{bu}"], writes=["actT"])
            for i in range(4):
                banks = (6, 7) if i % 2 == 0 else (0, 1)
                for half in range(2):
                    for j in range(NJ):
                        P.op("tensor", I("matmul", C.pb[banks[half]][:], lhsT=actT[:, j, i * 128:(i + 1) * 128],
                                         rhs=wout_sb[:, j, half * 512:(half + 1) * 512], start=(j == 0), stop=(j == NJ - 1)),
                             reads=["actT", "wout_sb"], writes=[f"pb{banks[half]}"])
                resid_update(xg[gs][:, i, :], f"xg{gs}", banks, gbc[:, 3, :], uid)
                uid += 1
            P.op("sync", I("dma_start", out=x_out[tg * 512:(tg + 1) * 512, :].rearrange("(i p) d -> p i d", p=128),
                           in_=xg[gs][:]), reads=[f"xg{gs}"], dma=True)
        print("post", P.emit())
    return nc


_CACHE = {}


def _prog(name, fn, *a):
    key = (name,) + a
    if key not in _CACHE:
        _CACHE[key] = fn(*a)
    return _CACHE[key]


def _run(nc, in_maps):
    res = run_bass_kernel_spmd(nc, in_maps, core_ids=list(range(NCORES)))
    return res.results


def kernel(x, c, rel_bias, ada_w, ada_b, norm_g, moba_w_qkv, moba_w_o, diff_w_qkv, diff_w_o, diff_lambda,
           diff_subln_g, ffn_w_in, ffn_w_out):
    f = lambda a: np.ascontiguousarray(np.asarray(a, dtype=np.float32))
    x, c, rel_bias, ada_w, ada_b, norm_g = f(x), f(c), f(rel_bias), f(ada_w), f(ada_b), f(norm_g)
    wqkv_l = [f(moba_w_qkv)[0], f(diff_w_qkv)[0]]
    wo_l = [f(moba_w_o)[0], f(diff_w_o)[0]]
    lam, subg = f(diff_lambda), f(diff_subln_g)
    ffn_w_in, ffn_w_out = f(ffn_w_in), f(ffn_w_out)
    xs = [np.ascontiguousarray(x[cc // 4, (cc % 4) * TOK:(cc % 4 + 1) * TOK, :]) for cc in range(NCORES)]
    cTs = [np.ascontiguousarray(c[cc // 4].reshape(8, 128).T) for cc in range(NCORES)]
    for layer in range(2):
        kind = "moba" if layer == 0 else "diff"
        adaw, adab, ng = ada_w[layer], ada_b[layer].reshape(1, -1), norm_g[layer]
        res = _run(_prog("pre", build_pre), [dict(x=xs[cc], cT=cTs[cc], adaw=adaw, adab=adab, ng=ng) for cc in range(NCORES)])
        hT = [res[cc]["hT"] for cc in range(NCORES)]
        hT_all = [np.ascontiguousarray(np.stack(hT[b * 4:(b + 1) * 4], 0)) for b in range(2)]
        wq, wk, wv = np.split(wqkv_l[layer], 3, axis=1)
        ins = []
        for cc in range(NCORES):
            hs = [2 * (cc % 4), 2 * (cc % 4) + 1]
            w = np.concatenate([np.concatenate([m[:, h * 128:(h + 1) * 128] for m in (wq, wk, wv)], 1) for h in hs], 1)
            d = dict(hT_all=hT_all[cc // 4], wqkv=np.ascontiguousarray(w),
                     relbT=np.ascontiguousarray(rel_bias[:, hs].T.reshape(1, 64)))
            if kind == "diff":
                d["lam"] = lam.reshape(1, 256)
                d["subg"] = subg.reshape(1, 128)
            ins.append(d)
        res = _run(_prog("attn", build_attn, kind), ins)
        oT = [res[cc]["oT"] for cc in range(NCORES)]
        win = ffn_w_in[layer]
        win_r = np.ascontiguousarray(
            np.stack([win[:, :DFF].reshape(8, 128, NJ, 128), win[:, DFF:].reshape(8, 128, NJ, 128)], 0)
            .transpose(3, 2, 0, 1, 4).reshape(NJ, 128, 2048))
        ins = []
        for cc in range(NCORES):
            b, r = cc // 4, cc % 4
            OT = np.concatenate([oT[b * 4 + j][:, :, r * TOK:(r + 1) * TOK] for j in range(4)], 0)
            ins.append(dict(x=xs[cc], OT=np.ascontiguousarray(OT), cT=cTs[cc], adaw=adaw, adab=adab, ng=ng,
                            wo=wo_l[layer], win_r=win_r, wout=ffn_w_out[layer]))
        res = _run(_prog("post", build_post), ins)
        xs = [res[cc]["x_out"] for cc in range(NCORES)]
    out = np.empty((2, SEQ, D), np.float32)
    for cc in range(NCORES):
        out[cc // 4, (cc % 4) * TOK:(cc % 4 + 1) * TOK, :] = xs[cc]
    return out
```

```python
import math
from contextlib import ExitStack
import numpy as np
import ml_dtypes
import concourse.bass as bass
import concourse.mybir as mybir
from concourse.bass_utils import run_bass_kernel_spmd

F32 = mybir.dt.float32
BF16 = mybir.dt.bfloat16
I32 = mybir.dt.int32
ACT = mybir.ActivationFunctionType
ALU = mybir.AluOpType
AX = mybir.AxisListType

D = 1024
SEQ = 8192
NH = 8
DFF = 2816
NJ = DFF // 128
TOK = 2048
NCORES = 8
EPS = 1e-6
OFF = 384
EW = 2432
NEAR_DMAX = 1536
LAMBDA_INIT = 0.8 - 0.6 * math.exp(-0.3 * 1)

ENGS = ("tensor", "vector", "scalar", "gpsimd", "sync")
SEM_CAP = 2048
DMA_RING = 8


class Ins:
    __slots__ = ("eng", "fn", "dma", "raw", "oth", "idx", "sig", "dslot", "dval", "waiters", "cc")


class Prog:
    def __init__(self, nc):
        self.nc = nc
        self.ins = []
        self.last_w = {}
        self.rd_eng = {}
        self.rd_dma = {}
        self.pending = {}
        self.last_eng = {}
        self.dma_hist = {e: [] for e in ENGS}
        self.cc_hist = []

    def barrier(self):
        deps = set(self.last_eng.values())
        for e in ENGS:
            deps.update(self.dma_hist[e][-DMA_RING:])
        deps.update(self.cc_hist)
        for e in ENGS:
            self.pending[e] = set(deps) | self.pending.get(e, set())

    def op(self, eng, fn, reads=(), writes=(), dma=False, cc=False):
        i = Ins()
        i.eng, i.fn, i.dma, i.cc = eng, fn, dma or cc, cc
        i.idx = len(self.ins)
        i.raw, i.oth = set(), set()
        i.sig = None
        i.waiters = False
        for r in reads:
            for w in self.last_w.get(r, ()):
                i.raw.add(w)
        for r in writes:
            ws = self.last_w.get(r, ())
            if not (i.dma and ws and all(self.ins[w].dma for w in ws)):
                for w in ws:
                    i.oth.add(w)
            for rd in self.rd_eng.get(r, {}).values():
                i.oth.add(rd)
            for rd in self.rd_dma.get(r, ()):
                i.oth.add(rd)
        for r in reads:
            if i.dma:
                self.rd_dma.setdefault(r, []).append(i.idx)
            else:
                self.rd_eng.setdefault(r, {})[eng] = i.idx
        for r in writes:
            ws = self.last_w.get(r, ())
            if i.dma and ws and all(self.ins[w].dma for w in ws) and not self.rd_eng.get(r) and not self.rd_dma.get(r):
                self.last_w[r] = list(ws) + [i.idx]
            else:
                self.last_w[r] = [i.idx]
            self.rd_eng[r] = {}
            self.rd_dma[r] = []
        if self.pending.get(eng):
            i.oth |= self.pending.pop(eng)
        if i.cc:
            self.cc_hist.append(i.idx)
        elif i.dma:
            self.dma_hist[eng].append(i.idx)
        else:
            self.last_eng[eng] = i.idx
        i.oth -= i.raw
        i.oth.discard(i.idx)
        i.raw.discard(i.idx)
        self.ins.append(i)
        return i

    def _needed(self, i):
        out = []
        for kind, ds in (("raw", i.raw), ("oth", i.oth)):
            for d in ds:
                dd = self.ins[d]
                if dd.dma or dd.eng != i.eng or i.dma:
                    out.append(d)
                elif i.eng == "tensor":
                    continue
                elif kind == "raw":
                    out.append(d)
        return out

    def emit(self):
        nc = self.nc
        ins = self.ins
        need = [self._needed(i) for i in ins]
        for i, nd in zip(ins, need):
            for d in nd:
                ins[d].waiters = True
        sigcount = {e: 0 for e in ENGS}
        dmacount = {e: 0 for e in ENGS}
        ncc = 0
        for i in ins:
            if i.cc:
                i.sig = ncc
                ncc += 1
            elif i.dma:
                n = dmacount[i.eng]
                dmacount[i.eng] += 1
                i.dslot = n % DMA_RING
                i.dval = 16 * (n // DMA_RING + 1)
                i.sig = n
            elif i.waiters:
                i.sig = sigcount[i.eng]
                sigcount[i.eng] += 1
        with ExitStack() as st:
            esems, dsems = {}, {}
            for e in ENGS:
                k = (sigcount[e] + SEM_CAP - 1) // SEM_CAP
                esems[e] = [st.enter_context(nc.semaphore(f"s_{e}_{j}")) for j in range(k)]
                k = min(DMA_RING, dmacount[e])
                dsems[e] = [st.enter_context(nc.semaphore(f"d_{e}_{j}")) for j in range(k)]
            ccsems = [st.enter_context(nc.semaphore(f"cc_{j}")) for j in range(ncc)]
            block = st.enter_context(nc.Block())

            def target(d):
                dd = ins[d]
                if dd.cc:
                    return (ccsems[dd.sig], 1)
                if dd.dma:
                    return (dsems[dd.eng][dd.dslot], dd.dval)
                return (esems[dd.eng][dd.sig // SEM_CAP], dd.sig % SEM_CAP + 1)

            def run(ename, eng):
                seen = {}
                for i, nd in zip(ins, need):
                    if i.eng != ename:
                        continue
                    waits = {}
                    for d in nd:
                        s, v = target(d)
                        key = id(s)
                        if seen.get(key, 0) >= v:
                            continue
                        if key not in waits or waits[key][1] < v:
                            waits[key] = (s, v)
                    if i.dma and not i.cc and i.sig >= DMA_RING:
                        s = dsems[ename][i.dslot]
                        v = i.dval - 16
                        key = id(s)
                        if seen.get(key, 0) < v and (key not in waits or waits[key][1] < v):
                            waits[key] = (s, v)
                    for key, (s, v) in waits.items():
                        eng.wait_ge(s, v)
                        seen[key] = v
                    r = i.fn(eng)
                    if i.cc:
                        r.then_inc(ccsems[i.sig])
                    elif i.dma:
                        r.then_inc(dsems[ename][i.dslot], 16)
                    elif i.sig is not None:
                        r.then_inc(esems[ename][i.sig // SEM_CAP], 1)
                n = dmacount[ename]
                for slot in range(min(DMA_RING, n)):
                    uses = (n - slot + DMA_RING - 1) // DMA_RING
                    s = dsems[ename][slot]
                    if seen.get(id(s), 0) < 16 * uses:
                        eng.wait_ge(s, 16 * uses)
                if ename == "gpsimd":
                    for s in ccsems:
                        if seen.get(id(s), 0) < 1:
                            eng.wait_ge(s, 1)

            block.tensor(lambda e: run("tensor", e))
            block.vector(lambda e: run("vector", e))
            block.scalar(lambda e: run("scalar", e))
            block.gpsimd(lambda e: run("gpsimd", e))
            block.sync(lambda e: run("sync", e))
        return dict(sig=sigcount, dma=dmacount, n=len(ins))


def I(m, *a, **k):
    return lambda e: getattr(e, m)(*a, **k)


def _dsize(dt):
    return 4 if dt in (F32, I32) else 2


ARENA_BYTES = 134 * 1024


class Ctx:
    def __init__(self, nc, st):
        self.nc, self.st = nc, st
        self.P = Prog(nc)
        self.pb = [st.enter_context(nc.psum_tensor(f"pb{i}", [128, 512], F32)) for i in range(8)]
        self.arena = None
        self.aoff = 0

    def fixed(self, name, shape, dt):
        return self.st.enter_context(self.nc.sbuf_tensor(name, shape, dt))

    def phase(self):
        if self.arena is None:
            self.arena = self.fixed("arena", [128, ARENA_BYTES // 4], F32)
        self.P.barrier()
        self.aoff = 0

    def sb(self, name, shape, dt):
        n = 1
        for v in shape[1:]:
            n *= v
        nb = (n * _dsize(dt) + 31) // 32 * 32
        assert self.aoff + nb <= ARENA_BYTES, (name, self.aoff, nb)
        v = self.arena[0:shape[0], self.aoff // 4:(self.aoff + nb) // 4]
        self.aoff += nb
        if dt != F32:
            v = v.bitcast(dt)
        v = v[:, 0:n]
        if len(shape) == 3:
            v = v.rearrange("p (a b) -> p a b", a=shape[1])
        return v

    def consts(self):
        P = self.P
        self.ident = self.fixed("ident", [128, 128], BF16)
        P.op("gpsimd", I("memset", self.ident[:], 1.0), writes=["ident"])
        P.op("gpsimd", I("affine_select", out=self.ident[:], in_=self.ident[:], pattern=[[-1, 128]],
                         compare_op=ALU.is_equal, fill=0.0, base=0, channel_multiplier=1),
             reads=["ident"], writes=["ident"])
        self.eps = self.fixed("eps", [128, 1], F32)
        P.op("vector", I("memset", self.eps[:], EPS), writes=["eps"])


def emit_rstd(C, ss, rstd, n, res_r, res_w):
    P = C.P
    P.op("scalar", I("activation", out=rstd, in_=ss, func=ACT.Ln, scale=1.0 / n, bias=C.eps[:]),
         reads=list(res_r) + ["eps"], writes=res_w)
    P.op("scalar", I("activation", out=rstd, in_=rstd, func=ACT.Exp, scale=-0.5), reads=res_w, writes=res_w)


def alloc_work(C, CW=128):
    C.w = dict(
        junk=C.sb("junk", [128, D], BF16),
        ss=[C.sb(f"ss{i}", [128, 1], F32) for i in range(2)],
        rstd=[C.sb(f"rstd{i}", [128, 1], F32) for i in range(2)],
        tmp=[C.sb(f"tmp{i}", [128, D], F32) for i in range(2)],
        hbf=[C.sb(f"hbf{i}", [128, D], BF16) for i in range(2)],
    )
    C.mw = dict(
        CW=CW,
        wch=[C.sb(f"adaw_ch{i}", [128, 8, CW], F32) for i in range(2)],
        bch=[C.sb(f"adab_ch{i}", [128, CW], F32) for i in range(2)],
        mtmp=[C.sb(f"mtmp{i}", [128, CW], F32) for i in range(2)],
    )


def emit_mod_piece(C, layer, piece, dst, dres, gi):
    P, M = C.P, C.mw
    CW = M["CW"]
    if gi is not None:
        P.op("sync", I("dma_start", out=dst, in_=C.dr["ng"][layer * 4 + gi:layer * 4 + gi + 1, :].partition_broadcast(128)),
             writes=[dres], dma=True)
    adaw_v = C.dr["adaw"][layer].rearrange("(k p) n -> p k n", p=128)
    for n in range(D // CW):
        s = C.mcount % 2
        C.mcount += 1
        col = piece * D + n * CW
        c0 = n * CW
        P.op("sync", I("dma_start", out=M["wch"][s][:], in_=adaw_v[:, :, col:col + CW]), writes=[f"adaw_ch{s}"], dma=True)
        P.op("sync", I("dma_start", out=M["bch"][s][:], in_=C.dr["adab"][layer:layer + 1, col:col + CW].partition_broadcast(128)),
             writes=[f"adab_ch{s}"], dma=True)
        bank = 6 + s
        for k in range(8):
            P.op("tensor", I("matmul", C.pb[bank][:, 0:CW], lhsT=C.cbc[:, k, :], rhs=M["wch"][s][:, k, :], start=(k == 0),
                             stop=(k == 7)), reads=["cbc", f"adaw_ch{s}"], writes=[f"pb{bank}"])
        if gi is None:
            P.op("vector", I("tensor_tensor", out=dst[:, c0:c0 + CW], in0=C.pb[bank][:, 0:CW], in1=M["bch"][s][:], op=ALU.add),
                 reads=[f"pb{bank}", f"adab_ch{s}"], writes=[dres])
        else:
            P.op("vector", I("tensor_tensor", out=M["mtmp"][s][:], in0=C.pb[bank][:, 0:CW], in1=M["bch"][s][:], op=ALU.add),
                 reads=[f"pb{bank}", f"adab_ch{s}"], writes=[f"mtmp{s}"])
            if piece in (1, 4):
                P.op("vector", I("scalar_tensor_tensor", out=dst[:, c0:c0 + CW], in0=M["mtmp"][s][:], scalar=1.0,
                                 in1=dst[:, c0:c0 + CW], op0=ALU.add, op1=ALU.mult), reads=[f"mtmp{s}", dres], writes=[dres])
            else:
                P.op("vector", I("tensor_tensor", out=dst[:, c0:c0 + CW], in0=M["mtmp"][s][:], in1=dst[:, c0:c0 + CW],
                                 op=ALU.mult), reads=[f"mtmp{s}", dres], writes=[dres])


def emit_norm_mod_T(C, xt, xres, A, Ares, B, Bres, hTg, hTg_res, blk, uid, defer=False):
    P = C.P
    W = C.w
    s = uid % 2
    junk, ss, rstd, tmp, hbf = W["junk"], W["ss"][s], W["rstd"][s], W["tmp"][s], W["hbf"][s]
    P.op("vector", I("memset", ss[:], 0.0), writes=[f"ss{s}"])
    P.op("scalar", I("activation", out=junk[:], in_=xt, func=ACT.Square, accum_out=ss[:]),
         reads=[xres, f"ss{s}"], writes=["junk", f"ss{s}"])
    emit_rstd(C, ss[:], rstd[:], D, [f"ss{s}"], [f"rstd{s}"])
    P.op("vector", I("scalar_tensor_tensor", out=tmp[:], in0=xt, scalar=rstd[:], in1=A, op0=ALU.mult, op1=ALU.mult),
         reads=[xres, f"rstd{s}", Ares], writes=[f"tmp{s}"])
    P.op("gpsimd", I("tensor_tensor", out=hbf[:], in0=tmp[:], in1=B, op=ALU.add),
         reads=[f"tmp{s}", Bres], writes=[f"hbf{s}"])
    if not defer:
        emit_norm_T_part2(C, hTg, hTg_res, blk, uid)


def emit_norm_T_part2(C, hTg, hTg_res, blk, uid):
    P = C.P
    s = uid % 2
    hbf = C.w["hbf"][s]
    bank = 4 + s
    pT = C.pb[bank][:].bitcast(BF16)
    for k in range(8):
        P.op("tensor", I("transpose", out=pT[:, k * 128:(k + 1) * 128], in_=hbf[:, k * 128:(k + 1) * 128],
                         identity=C.ident[:]), reads=[f"hbf{s}", "ident"], writes=[f"pb{bank}"])
    P.op("scalar", I("copy", out=hTg[:, :, blk * 128:(blk + 1) * 128],
                     in_=pT.rearrange("p (k t) -> p k t", k=8)), reads=[f"pb{bank}"], writes=[hTg_res])


def t5_lo():
    n = np.arange(0, 4096)
    nf = np.maximum(n, 1).astype(np.float32)
    large = 16 + (np.log(nf / np.float32(16)) / np.float32(math.log(2048 / 16)) * np.float32(16)).astype(np.int32)
    large = np.minimum(large, 31)
    b = np.where(n < 16, n, large)
    assert all((b == k).any() for k in range(32))
    lo = [int(np.argmax(b == k)) for k in range(32)]
    assert lo[31] <= NEAR_DMAX + 128 - 127, lo
    return lo


def emit_setup(C):
    nc, P, dr = C.nc, C.P, C.dr
    C.consts()
    C.mcount = 0
    C.xres = C.fixed("xres", [128, TOK // 128, D], F32)
    C.cbc = C.fixed("cbc", [128, 8, 128], F32)
    C.b31 = C.fixed("b31", [128, 2], F32)
    C.oh = C.fixed("oh_sb", [128, 4], F32)
    C.phase()
    for tt in range(TOK // 128):
        P.op("sync", I("dma_start", out=C.xres[:, tt, :], in_=dr["x"][tt * 128:(tt + 1) * 128, :]), writes=[f"x{tt}"], dma=True)
    P.op("sync", I("dma_start", out=C.oh[:], in_=dr["oh"]), writes=["oh"], dma=True)
    cT_sb = C.sb("cT_sb", [128, 8], F32)
    cact = C.sb("cact", [128, 8], F32)
    ones = C.sb("ones_f", [128, 128], F32)
    P.op("sync", I("dma_start", out=cT_sb[:], in_=dr["cT"]), writes=["cT_sb"], dma=True)
    P.op("scalar", I("activation", out=cact[:], in_=cT_sb[:], func=ACT.Silu), reads=["cT_sb"], writes=["cact"])
    P.op("vector", I("memset", ones[:], 1.0), writes=["ones_f"])
    for k in range(8):
        P.op("vector", I("tensor_scalar", out=C.cbc[:, k, :], in0=ones[:], scalar1=cact[:, k:k + 1], scalar2=None,
                         op0=ALU.mult), reads=["ones_f", "cact"], writes=["cbc"])
    lo = t5_lo()
    tab = C.sb("tab", [128, 2, 32], F32)
    etab = C.sb("etab", [128, 2, 32], F32)
    cdf = C.sb("cdf", [128, 2, 32], F32)
    P.op("sync", I("dma_start", out=tab[:].rearrange("p a b -> p (a b)"), in_=dr["relbT"].partition_broadcast(128)),
         writes=["tab"], dma=True)
    P.op("scalar", I("activation", out=etab[:], in_=tab[:], func=ACT.Exp), reads=["tab"], writes=["etab"])
    P.op("vector", I("tensor_copy", out=cdf[:, :, 0:1], in_=etab[:, :, 0:1]), reads=["etab"], writes=["cdf"])
    P.op("vector", I("tensor_tensor", out=cdf[:, :, 1:32], in0=etab[:, :, 1:32], in1=etab[:, :, 0:31],
                     op=ALU.subtract), reads=["etab"], writes=["cdf"])
    P.op("vector", I("tensor_copy", out=C.b31[:], in_=tab[:, :, 31]), reads=["tab"], writes=["b31"])
    reli = C.sb("reli", [128, 20], I32)
    relf = C.sb("relf", [128, 20], F32)
    Gt = C.sb("Gt", [128, 2, 20], F32)
    Gtmp = C.sb("Gtmp", [128, 20], F32)
    P.op("gpsimd", I("iota", reli[:], pattern=[[1, 20]], base=-511, channel_multiplier=20), writes=["reli"])
    P.op("vector", I("tensor_copy", out=relf[:], in_=reli[:]), reads=["reli"], writes=["relf"])
    P.op("vector", I("memset", Gt[:], 0.0), writes=["Gt"])
    for hh in range(2):
        for b in range(32):
            P.op("vector", I("tensor_scalar", out=Gtmp[:], in0=relf[:], scalar1=float(lo[b]),
                             scalar2=cdf[:, hh, b:b + 1], op0=ALU.is_ge, op1=ALU.mult),
                 reads=["relf", "cdf"], writes=["Gtmp"])
            P.op("vector", I("tensor_tensor", out=Gt[:, hh, :], in0=Gt[:, hh, :], in1=Gtmp[:], op=ALU.add),
                 reads=["Gt", "Gtmp"], writes=["Gt"])
    P.op("sync", I("dma_start", out=dr["Gd"].rearrange("a (p j) -> p a j", j=20), in_=Gt[:]), reads=["Gt"],
         writes=["Gd"], dma=True)
    C.E = C.sb("E", [128, 2, EW], BF16)
    hank = C.sb("hank", [128, EW], BF16)
    antiI = C.sb("antiI", [128, 128], BF16)
    P.op("gpsimd", I("memset", antiI[:], 1.0), writes=["antiI"])
    P.op("gpsimd", I("affine_select", out=antiI[:], in_=antiI[:], pattern=[[1, 128]], compare_op=ALU.is_equal,
                     fill=0.0, base=-127, channel_multiplier=1), reads=["antiI"], writes=["antiI"])
    for hh in range(2):
        src = bass.AP(dr["Gd_t"], hh * 2560, [[1, 128], [1, EW]])
        P.op("gpsimd", I("dma_start", out=hank[:], in_=src), reads=["Gd"], writes=["hank"], dma=True)
        for c0 in range(0, EW, 512):
            w = min(512, EW - c0)
            P.op("tensor", I("matmul", C.pb[7][:, 0:w], lhsT=antiI[:], rhs=hank[:, c0:c0 + w], start=True, stop=True),
                 reads=["antiI", "hank"], writes=["pb7"])
            P.op("vector", I("tensor_copy", out=C.E[:, hh, c0:c0 + w], in_=C.pb[7][:, 0:w]), reads=["pb7"], writes=["E"])
    P.op("sync", I("dma_start", out=dr["E_d"], in_=C.E.rearrange("p a u -> p (a u)")), reads=["E"], writes=["E_d"], dma=True)


def emit_phase_A(C, layer):
    P, dr = C.P, C.dr
    C.phase()
    alloc_work(C, 512)
    A0 = C.sb("A0", [128, D], F32)
    shA = C.sb("shA", [128, D], F32)
    hTg = [C.sb(f"hTg{i}", [128, 8, 512], BF16) for i in range(2)]
    emit_mod_piece(C, layer, 1, A0, "A0", 0)
    emit_mod_piece(C, layer, 0, shA, "shA", None)
    for tt in range(TOK // 128):
        g, blk = tt // 4, tt % 4
        emit_norm_mod_T(C, C.xres[:, tt, :], f"x{tt}", A0, "A0", shA, "shA", hTg[g % 2], f"hTg{g % 2}", blk, tt)
        if blk == 3:
            nm = f"{layer}_{g}"
            P.op("sync", I("dma_start", out=dr[f"hT_loc{nm}"].rearrange("(k p) t -> p k t", p=128), in_=hTg[g % 2]),
                 reads=[f"hTg{g % 2}"], writes=[f"hT_loc{nm}"], dma=True)
            P.op("gpsimd", I("collective_compute", "AllGather", ALU.bypass, replica_groups=[[0, 1, 2, 3], [4, 5, 6, 7]],
                             ins=[dr[f"hT_loc{nm}_cc"].opt()], outs=[dr[f"hT_all{nm}_cc"].opt()]),
                 reads=[f"hT_loc{nm}"], writes=[f"hT_all{nm}"], cc=True)


def emit_phase_B(C, layer, kind):
    P, dr = C.P, C.dr
    moba = kind == "moba"
    DK = 128 if moba else 64
    scale = DK ** -0.5
    b31 = C.b31
    C.phase()
    E = C.sb("E", [128, 2, EW], BF16)
    P.op("sync", I("dma_start", out=E.rearrange("p a u -> p (a u)"), in_=dr["E_d"]), reads=["E_d"], writes=["E"], dma=True)
    hT_all = [dr[f"hT_all{layer}_{g}"].rearrange("(r k p) t -> r p k t", r=4, k=8) for g in range(4)]
    oT_loc = [dr[f"oT_loc{layer}_{c}"].rearrange("(h p) t -> h p t", h=2) for c in range(4)]
    wsb = C.sb("wsb", [128, 8, 768], BF16)
    P.op("gpsimd", I("dma_start", out=wsb, in_=dr["wqkv"][layer].rearrange("(k p) n -> p k n", p=128)), writes=["wsb"], dma=True)
    if moba:
        selM = C.sb("selM", [32, 32 * 128], BF16)
        P.op("gpsimd", I("memset", selM, 1.0), writes=["selM"])
        P.op("gpsimd", I("affine_select", out=selM, in_=selM, pattern=[[1, 4096]], compare_op=ALU.is_ge,
                         fill=0.0, base=0, channel_multiplier=-128), reads=["selM"], writes=["selM"])
        P.op("gpsimd", I("affine_select", out=selM, in_=selM, pattern=[[-1, 4096]], compare_op=ALU.is_ge,
                         fill=0.0, base=127, channel_multiplier=128), reads=["selM"], writes=["selM"])
        kmf = C.sb("kmf", [128, 32], F32)
        kmb = C.sb("kmb", [128, 32], BF16)
        gate = [C.sb(f"gate{i}", [128, 32], F32) for i in range(2)]
        m8 = [C.sb(f"m8{i}", [128, 8], F32) for i in range(2)]
        sel = [C.sb(f"sel{i}", [128, 32], F32) for i in range(2)]
        nmq = [C.sb(f"nmq{i}", [128, 32], BF16) for i in range(2)]
        nmT = [C.sb(f"nmT{i}", [32, 512], BF16) for i in range(2)]
    else:
        lam_sb = C.sb("lam_sb", [128, 256], F32)
        lp = C.sb("lp", [128, 128], F32)
        ls = C.sb("ls", [128, 2], F32)
        le = C.sb("le", [128, 2], F32)
        neglam = C.sb("neglam", [128, 1], F32)
        subg_bc = C.sb("subg_col", [128, 1], F32)
        P.op("sync", I("dma_start", out=lam_sb, in_=dr["lam"].partition_broadcast(128)), writes=["lam_sb"], dma=True)
        P.op("sync", I("dma_start", out=subg_bc, in_=dr["subg"]), writes=["subg_bc"], dma=True)
        lv = lam_sb.rearrange("p (a b c) -> p a b c", a=2, b=2)
        P.op("vector", I("tensor_tensor", out=lp.rearrange("p (a c) -> p a c", a=2), in0=lv[:, :, 0, :],
                         in1=lv[:, :, 1, :], op=ALU.mult), reads=["lam_sb"], writes=["lp"])
        P.op("vector", I("tensor_reduce", out=ls, in_=lp.rearrange("p (a c) -> p a c", a=2), axis=AX.X,
                         op=ALU.add), reads=["lp"], writes=["ls"])
        P.op("scalar", I("activation", out=le, in_=ls, func=ACT.Exp), reads=["ls"], writes=["le"])
        P.op("vector", I("tensor_tensor", out=neglam, in0=le[:, 1:2], in1=le[:, 0:1], op=ALU.subtract),
             reads=["le"], writes=["neglam"])
        P.op("vector", I("tensor_scalar", out=neglam, in0=neglam, scalar1=-LAMBDA_INIT, scalar2=None,
                         op0=ALU.add), reads=["neglam"], writes=["neglam"])
        P.op("vector", I("tensor_scalar", out=subg_bc, in0=subg_bc, scalar1=1.0 - LAMBDA_INIT, scalar2=None,
                         op0=ALU.mult), reads=["subg_bc"], writes=["subg_bc"])
        t1 = C.sb("t1", [128, 512], F32)
        of = C.sb("of", [128, 512], F32)
        sq = C.sb("sq", [128, 512], F32)
        rsd = C.sb("rsd", [128, 512], F32)
    QT = C.sb("QT", [128, SEQ], BF16)
    KT = C.sb("KT", [128, SEQ], BF16)
    Vp = C.sb("Vp", [128, 64, 130], BF16)
    hTg = [C.sb(f"hTg{i}", [128, 8, 512], BF16) for i in range(2)]
    NMAP = 1 if moba else 2
    PT = [C.sb(f"PT{i}", [128, 512], BF16) for i in range(4)]
    rl = [C.sb(f"rl{i}", [1, 512], F32) for i in range(2)]
    RLsb = C.sb("RLsb", [128, 512], F32)
    ones_f = C.sb("ones_f", [128, 128], F32)
    ones_c = C.sb("ones_c", [128, 1], BF16)
    P.op("vector", I("memset", ones_f, 1.0), writes=["ones_f"])
    P.op("vector", I("memset", ones_c, 1.0), writes=["ones_c"])
    OTg = [C.sb(f"OTg{i}", [128, 512], BF16) for i in range(2)]
    def cast_weights():
        for r0 in range(0, D, 256):
            P.op("gpsimd", I("dma_start", out=dr[f"wo_bf{layer}"][r0:r0 + 256, :], in_=dr["wo"][layer][r0:r0 + 256, :]),
                 reads=["QT"], writes=[f"wo_bf{layer}"], dma=True)
        for j in range(NJ):
            P.op("gpsimd", I("dma_start", out=dr[f"win_bf{layer}"][j], in_=dr["win_r"][layer * NJ + j]),
                 reads=["QT"], writes=[f"win_bf{layer}"], dma=True)
        for r0 in range(0, DFF, 256):
            P.op("gpsimd", I("dma_start", out=dr[f"wout_bf{layer}"][r0:r0 + 256, :], in_=dr["wout"][layer][r0:r0 + 256, :]),
                 reads=["QT"], writes=[f"wout_bf{layer}"], dma=True)

    cnt = dict(u=0, g=0, f=0)
    for hh in range(2):
        P.op("vector", I("memset", Vp[:, :, 128:129], 1.0), reads=[], writes=["Vp"])
        for gi_ in range(16):
            g = (gi_ // 4) + 4 * (gi_ % 4)
            s = gi_ % 2
            r = g // 4
            P.op("sync", I("dma_start", out=hTg[s], in_=hT_all[g % 4][r]),
                 reads=[f"hT_all{layer}_{g % 4}"], writes=[f"hTg{s}"], dma=True)
            for which, dst, eng in ((0, QT, "scalar"), (1, KT, "vector")):
                bank = which
                c0 = hh * 384 + which * 128
                for k in range(8):
                    P.op("tensor", I("matmul", C.pb[bank][:], lhsT=wsb[:, k, c0:c0 + 128], rhs=hTg[s][:, k, :],
                                     start=(k == 0), stop=(k == 7)), reads=["wsb", f"hTg{s}"], writes=[f"pb{bank}"])
                if eng == "scalar":
                    P.op("scalar", I("copy", out=dst[:, g * 512:(g + 1) * 512], in_=C.pb[bank][:]),
                         reads=[f"pb{bank}"], writes=["QT"])
                else:
                    P.op("vector", I("tensor_copy", out=dst[:, g * 512:(g + 1) * 512], in_=C.pb[bank][:]),
                         reads=[f"pb{bank}"], writes=["KT"])
            c0 = hh * 384 + 256
            for i in range(4):
                for k in range(8):
                    P.op("tensor", I("matmul", C.pb[2][:, i * 128:(i + 1) * 128], lhsT=hTg[s][:, k, i * 128:(i + 1) * 128],
                                     rhs=wsb[:, k, c0:c0 + 128], start=(k == 0), stop=(k == 7)),
                         reads=["wsb", f"hTg{s}"], writes=["pb2"])
            P.op("vector", I("tensor_copy", out=Vp[:, g * 4:(g + 1) * 4, 0:128],
                             in_=C.pb[2][:].rearrange("p (i d) -> p i d", i=4)), reads=["pb2"], writes=["Vp"])
        if hh == 0:
            cast_weights()
        if moba:
            P.op("vector", I("tensor_reduce", out=kmf, in_=KT.rearrange("p (b t) -> p b t", t=256), axis=AX.X,
                             op=ALU.add), reads=["KT"], writes=["kmf"])
            P.op("vector", I("tensor_copy", out=kmb, in_=kmf), reads=["kmf"], writes=["kmb"])
        def gating(qg):
            ns = qg % 2
            for i in range(4):
                qt = qg * 4 + i
                own = qt // 2
                gs = cnt["g"] % 2
                cnt["g"] += 1
                if own <= 3:
                    P.op("vector", I("memset", sel[gs], 0.0), writes=[f"sel{gs}"])
                    P.op("vector", I("memset", sel[gs][:, 0:own + 1], 1.0), writes=[f"sel{gs}"])
                else:
                    P.op("tensor", I("matmul", C.pb[7][:, 0:32], lhsT=QT[:, qt * 128:(qt + 1) * 128], rhs=kmb,
                                     start=True, stop=True), reads=["QT", "kmb"], writes=["pb7"])
                    P.op("vector", I("tensor_copy", out=gate[gs], in_=C.pb[7][:, 0:32]), reads=["pb7"],
                         writes=[f"gate{gs}"])
                    P.op("vector", I("memset", gate[gs][:, own:32], -1e30), writes=[f"gate{gs}"])
                    P.op("vector", I("max", out=m8[gs], in_=gate[gs]), reads=[f"gate{gs}"], writes=[f"m8{gs}"])
                    P.op("vector", I("tensor_scalar", out=sel[gs], in0=gate[gs], scalar1=m8[gs][:, 2:3],
                                     scalar2=None, op0=ALU.is_ge), reads=[f"gate{gs}", f"m8{gs}"],
                         writes=[f"sel{gs}"])
                    P.op("vector", I("memset", sel[gs][:, own:own + 1], 1.0), writes=[f"sel{gs}"])
                P.op("vector", I("tensor_scalar", out=nmq[gs], in0=sel[gs], scalar1=-1.0, scalar2=30000.0,
                                 op0=ALU.add, op1=ALU.mult), reads=[f"sel{gs}"], writes=[f"nmq{gs}"])
                pT7 = C.pb[7][:].bitcast(BF16)
                P.op("tensor", I("transpose", out=pT7[0:32, 512:640], in_=nmq[gs], identity=C.ident[:]),
                     reads=[f"nmq{gs}", "ident"], writes=["pb7"])
                P.op("vector", I("tensor_copy", out=nmT[ns][:, i * 128:(i + 1) * 128], in_=pT7[0:32, 512:640]),
                     reads=["pb7"], writes=[f"nmT{ns}"])

        def emit_qk(pr):
            qg, kt, mp, u = pr
            q0 = qg * 512
            sb_ = u % 3
            r0 = mp * 64 if not moba else 0
            P.op("tensor", I("matmul", C.pb[sb_][:], lhsT=KT[r0:r0 + DK, kt * 128:(kt + 1) * 128],
                             rhs=QT[r0:r0 + DK, q0:q0 + 512], start=True, stop=not moba),
                 reads=["KT", "QT"], writes=[f"pb{sb_}"])
            if moba:
                j = kt // 2
                P.op("tensor", I("matmul", C.pb[sb_][:], lhsT=selM[:, j * 128:(j + 1) * 128], rhs=nmT[qg % 2],
                                 start=False, stop=True), reads=["selM", f"nmT{qg % 2}"], writes=[f"pb{sb_}"])

        def emit_rest(pr):
            qg, kt, mp, u = pr
            q0 = qg * 512
            sb_, ps_ = u % 3, u % 4
            Dq = q0 - kt * 128
            if Dq <= NEAR_DMAX:
                P.op("scalar", I("activation", out=PT[ps_], in_=C.pb[sb_][:], func=ACT.Exp, scale=scale),
                     reads=[f"pb{sb_}"], writes=[f"PT{ps_}"])
                P.op("vector", I("tensor_tensor", out=PT[ps_], in0=PT[ps_],
                                 in1=E[:, hh, Dq + OFF:Dq + OFF + 512], op=ALU.mult),
                     reads=[f"PT{ps_}", "E"], writes=[f"PT{ps_}"])
            else:
                P.op("scalar", I("activation", out=PT[ps_], in_=C.pb[sb_][:], func=ACT.Exp, scale=scale,
                                 bias=b31[:, hh:hh + 1]), reads=[f"pb{sb_}", "b31"], writes=[f"PT{ps_}"])
            last = (kt == 4 * qg + 3)
            P.op("tensor", I("matmul", C.pb[3 + mp][:], lhsT=Vp[:, kt, 0:128], rhs=PT[ps_], start=(kt == 0), stop=last),
                 reads=[f"PT{ps_}", "Vp"], writes=[f"pb{3 + mp}"])
            P.op("tensor", I("matmul", C.pb[5 + mp][0:1, :], lhsT=ones_c[:, 0:1], rhs=PT[ps_], start=(kt == 0), stop=last),
                 reads=[f"PT{ps_}", "ones_c"], writes=[f"pb{5 + mp}"])

        def finalize(qg):
            og = qg % 2
            for mp in range(NMAP):
                P.op("vector", I("reciprocal", out=rl[mp], in_=C.pb[5 + mp][0:1, :]), reads=[f"pb{5 + mp}"], writes=[f"rl{mp}"])
            if not moba:
                P.op("vector", I("tensor_scalar", out=rl[1], in0=rl[1], scalar1=neglam[0:1, 0:1], scalar2=None, op0=ALU.mult),
                     reads=["rl1", "neglam"], writes=["rl1"])
            for mp in range(NMAP):
                P.op("tensor", I("matmul", C.pb[7][:], lhsT=ones_f[0:1, :], rhs=rl[mp], start=True, stop=True),
                     reads=["ones_f", f"rl{mp}"], writes=["pb7"])
                P.op("scalar", I("copy", out=RLsb, in_=C.pb[7][:]), reads=["pb7"], writes=["RLsb"])
                if moba:
                    P.op("vector", I("tensor_tensor", out=OTg[og], in0=C.pb[3][:], in1=RLsb, op=ALU.mult),
                         reads=["pb3", "RLsb"], writes=[f"OTg{og}"])
                elif mp == 0:
                    P.op("vector", I("tensor_tensor", out=t1, in0=C.pb[3][:], in1=RLsb, op=ALU.mult),
                         reads=["pb3", "RLsb"], writes=["t1"])
                else:
                    P.op("vector", I("tensor_tensor", out=of, in0=C.pb[4][:], in1=RLsb, op=ALU.mult),
                         reads=["pb4", "RLsb"], writes=["of"])
            if not moba:
                P.op("vector", I("tensor_tensor", out=of, in0=of, in1=t1, op=ALU.add), reads=["of", "t1"], writes=["of"])
                P.op("scalar", I("activation", out=sq, in_=of, func=ACT.Square), reads=["of"], writes=["sq"])
                P.op("tensor", I("matmul", C.pb[7][:], lhsT=ones_f, rhs=sq, start=True, stop=True),
                     reads=["ones_f", "sq"], writes=["pb7"])
                P.op("scalar", I("activation", out=rsd, in_=C.pb[7][:], func=ACT.Ln, scale=1.0 / 128, bias=C.eps[:]),
                     reads=["pb7", "eps"], writes=["rsd"])
                P.op("scalar", I("activation", out=rsd, in_=rsd, func=ACT.Exp, scale=-0.5), reads=["rsd"], writes=["rsd"])
                P.op("vector", I("scalar_tensor_tensor", out=OTg[og], in0=of, scalar=subg_bc[:, 0:1], in1=rsd, op0=ALU.mult,
                                 op1=ALU.mult), reads=["of", "subg_bc", "rsd"], writes=[f"OTg{og}"])
            ch = qg // 4
            nm = f"{layer}_{ch}"
            P.op("sync", I("dma_start", out=oT_loc[ch][hh][:, (qg % 4) * 512:(qg % 4 + 1) * 512], in_=OTg[og]),
                 reads=[f"OTg{og}"], writes=[f"oT_loc{nm}"], dma=True)
            if hh == 1 and qg % 4 == 3:
                P.op("gpsimd", I("collective_compute", "AllGather", ALU.bypass, replica_groups=[[0, 1, 2, 3], [4, 5, 6, 7]],
                                 ins=[dr[f"oT_loc{nm}_cc"].opt()], outs=[dr[f"oT_all{nm}_cc"].opt()]),
                     reads=[f"oT_loc{nm}"], writes=[f"oT_all{nm}"], cc=True)

        pairs = []
        for qg in range(16):
            for kt in range(4 * qg + 4):
                for mp in range(NMAP):
                    pairs.append((qg, kt, mp, cnt["u"]))
                    cnt["u"] += 1
        LOOK = 2
        for idx in range(len(pairs) + LOOK):
            if idx < len(pairs):
                pr = pairs[idx]
                if moba and pr[1] == 0 and pr[2] == 0:
                    if pr[0] == 0:
                        gating(0)
                    if pr[0] + 1 < 16:
                        gating(pr[0] + 1)
                emit_qk(pr)
            if idx >= LOOK:
                pr = pairs[idx - LOOK]
                emit_rest(pr)
                if pr[1] == 4 * pr[0] + 3 and pr[2] == NMAP - 1:
                    finalize(pr[0])


def resid_update(C, xtile, xres, banks, Grow, Gres, u):
    P, W = C.P, C.w
    R = C.rw
    s = u % 2
    P.op("vector", I("memset", R["ssy"][s], 0.0), writes=[f"ssy{s}"])
    for half in range(2):
        P.op("scalar", I("activation", out=W["junk"][:, 0:512], in_=C.pb[banks[half]][:], func=ACT.Square,
                         accum_out=R["ssy"][s][:, half:half + 1]), reads=[f"pb{banks[half]}", f"ssy{s}"],
             writes=["junk", f"ssy{s}"])
    P.op("vector", I("tensor_tensor", out=R["ss1"][s], in0=R["ssy"][s][:, 0:1], in1=R["ssy"][s][:, 1:2], op=ALU.add),
         reads=[f"ssy{s}"], writes=[f"ss1_{s}"])
    emit_rstd(C, R["ss1"][s], R["rsy"][s], D, [f"ss1_{s}"], [f"rsy{s}"])
    for half in range(2):
        hs = slice(half * 512, (half + 1) * 512)
        P.op("vector", I("scalar_tensor_tensor", out=W["tmp"][s][:, hs], in0=C.pb[banks[half]][:], scalar=R["rsy"][s],
                         in1=Grow[:, hs], op0=ALU.mult, op1=ALU.mult),
             reads=[f"pb{banks[half]}", f"rsy{s}", Gres], writes=[f"tmp{s}"])
    P.op("gpsimd", I("tensor_tensor", out=xtile, in0=xtile, in1=W["tmp"][s], op=ALU.add),
         reads=[xres, f"tmp{s}"], writes=[xres])


def alloc_resid(C):
    C.rw = dict(
        ssy=[C.sb(f"ssy{i}", [128, 2], F32) for i in range(2)],
        ss1=[C.sb(f"ss1_{i}", [128, 1], F32) for i in range(2)],
        rsy=[C.sb(f"rsy{i}", [128, 1], F32) for i in range(2)],
    )


def emit_phase_C1(C, layer):
    P, dr = C.P, C.dr
    C.phase()
    alloc_work(C, 512)
    alloc_resid(C)
    G1 = C.sb("G1", [128, D], F32)
    emit_mod_piece(C, layer, 2, G1, "G1", 1)
    wo_sb = C.sb("wo_sb", [128, 8, D], BF16)
    wo_v = dr[f"wo_bf{layer}"].rearrange("(h p) n -> p h n", p=128)
    for h0 in range(0, 8, 4):
        P.op("sync", I("dma_start", out=wo_sb[:, h0:h0 + 4, :], in_=wo_v[:, h0:h0 + 4, :]), reads=[f"wo_bf{layer}"],
             writes=["wo_sb"], dma=True)
    cand = [C.sb(f"cand{i}", [128, 8, 512], BF16) for i in range(2)]
    OTg = [C.sb(f"OTg{i}", [128, 8, 512], BF16) for i in range(2)]
    oT_all = [dr[f"oT_all{layer}_{c}"].rearrange("(h p) t -> p h t", p=128) for c in range(4)]
    uid = 0
    cc_ = 0
    for tg in range(4):
        gs = tg % 2
        for c in range(4):
            cs = cc_ % 2
            cc_ += 1
            P.op("sync", I("dma_start", out=cand[cs], in_=oT_all[c][:, :, tg * 512:(tg + 1) * 512]),
                 reads=[f"oT_all{layer}_{c}"], writes=[f"cand{cs}"], dma=True)
            for eng, part, hs_ in (("vector", "a", slice(0, 8)),):
                if c == 0:
                    P.op(eng, I("tensor_scalar", out=OTg[gs][:, hs_, :], in0=cand[cs][:, hs_, :], scalar1=C.oh[:, 0:1],
                                scalar2=None, op0=ALU.mult), reads=[f"cand{cs}", "oh"], writes=[f"OTg{gs}{part}"])
                else:
                    P.op(eng, I("scalar_tensor_tensor", out=OTg[gs][:, hs_, :], in0=cand[cs][:, hs_, :],
                                scalar=C.oh[:, c:c + 1], in1=OTg[gs][:, hs_, :], op0=ALU.mult, op1=ALU.add),
                         reads=[f"cand{cs}", "oh", f"OTg{gs}{part}"], writes=[f"OTg{gs}{part}"])
        for i in range(4):
            tt = tg * 4 + i
            banks = (0, 1) if i % 2 == 0 else (2, 3)
            for half in range(2):
                for h in range(8):
                    P.op("tensor", I("matmul", C.pb[banks[half]][:], lhsT=OTg[gs][:, h, i * 128:(i + 1) * 128],
                                     rhs=wo_sb[:, h, half * 512:(half + 1) * 512], start=(h == 0), stop=(h == 7)),
                         reads=[f"OTg{gs}a", "wo_sb"], writes=[f"pb{banks[half]}"])
            resid_update(C, C.xres[:, tt, :], f"x{tt}", banks, G1, "G1", uid)
            uid += 1


def emit_phase_C2(C, layer):
    P, dr = C.P, C.dr
    C.phase()
    alloc_work(C)
    alloc_resid(C)
    A2 = C.sb("A2", [128, D], F32)
    shF = C.sb("shF", [128, D], F32)
    G3 = C.sb("G3", [128, D], F32)
    wout_sb = C.sb("wout_sb", [128, NJ, D], BF16)
    wout_v = dr[f"wout_bf{layer}"].rearrange("(j p) n -> p j n", p=128)
    for j0 in range(0, NJ, 6):
        j1 = min(NJ, j0 + 6)
        P.op("sync", I("dma_start", out=wout_sb[:, j0:j1, :], in_=wout_v[:, j0:j1, :]), reads=[f"wout_bf{layer}"],
             writes=["wout_sb"], dma=True)
    emit_mod_piece(C, layer, 4, A2, "A2", 2)
    emit_mod_piece(C, layer, 3, shF, "shF", None)
    emit_mod_piece(C, layer, 5, G3, "G3", 3)
    h2T = [C.sb(f"h2T{i}", [128, 8, 512], BF16) for i in range(2)]
    actT = C.sb("actT", [128, NJ, 512], BF16)
    wch = [C.sb(f"wch{i}", [128, 2, 8 * 128], BF16) for i in range(2)]
    sg = [C.sb(f"sg{i}", [128, 512], BF16) for i in range(2)]
    uid = 0
    wcount = 0
    def pre1(tg, i):
        tt = tg * 4 + i
        emit_norm_mod_T(C, C.xres[:, tt, :], f"x{tt}", A2, "A2", shF, "shF", h2T[tg % 2], f"h2T{tg % 2}", i, tg * 4 + i,
                        defer=True)

    def pre2(tg, i):
        emit_norm_T_part2(C, h2T[tg % 2], f"h2T{tg % 2}", i, tg * 4 + i)

    for i in range(4):
        pre1(0, i)
        pre2(0, i)
    for tg in range(4):
        for j in range(NJ):
            if tg + 1 < 4 and j % 5 == 1 and j // 5 < 4:
                pre1(tg + 1, j // 5)
            if tg + 1 < 4 and j % 5 == 4 and j // 5 < 4:
                pre2(tg + 1, j // 5)
            ws = wcount % 2
            wcount += 1
            P.op("sync", I("dma_start", out=wch[ws].rearrange("p a n -> p (a n)"), in_=dr[f"win_bf{layer}"][j]),
                 reads=[f"win_bf{layer}"], writes=[f"wch{ws}"], dma=True)
            bg, bu = (0, 1) if j % 2 == 0 else (2, 3)
            for a, bank in ((0, bg), (1, bu)):
                for k in range(8):
                    P.op("tensor", I("matmul", C.pb[bank][:], lhsT=wch[ws][:, a, k * 128:(k + 1) * 128],
                                     rhs=h2T[tg % 2][:, k, :], start=(k == 0), stop=(k == 7)),
                         reads=[f"wch{ws}", f"h2T{tg % 2}"], writes=[f"pb{bank}"])
            s2 = j % 2
            P.op("scalar", I("activation", out=sg[s2], in_=C.pb[bg][:], func=ACT.Silu), reads=[f"pb{bg}"],
                 writes=[f"sg{s2}"])
            P.op("vector", I("tensor_tensor", out=actT[:, j, :], in0=sg[s2], in1=C.pb[bu][:], op=ALU.mult),
                 reads=[f"sg{s2}", f"pb{bu}"], writes=["actT"])
        for i in range(4):
            tt = tg * 4 + i
            banks = (6, 7) if i % 2 == 0 else (0, 1)
            for half in range(2):
                for j in range(NJ):
                    P.op("tensor", I("matmul", C.pb[banks[half]][:], lhsT=actT[:, j, i * 128:(i + 1) * 128],
                                     rhs=wout_sb[:, j, half * 512:(half + 1) * 512], start=(j == 0), stop=(j == NJ - 1)),
                         reads=["actT", "wout_sb"], writes=[f"pb{banks[half]}"])
            resid_update(C, C.xres[:, tt, :], f"x{tt}", banks, G3, "G3", uid)
            uid += 1


def build_fused(stop=None):
    nc = bass.Bass("TRN2", target_bir_lowering=False)
    ext = lambda name, shape, dt=F32: nc.dram_tensor(name, shape, dt, kind="ExternalInput").ap()
    dr = dict(
        x=ext("x", [TOK, D]), cT=ext("cT", [128, 8]), oh=ext("oh", [128, 4]),
        adaw=ext("adaw", [2, D, 6 * D]), adab=ext("adab", [2, 6 * D]), ng=ext("ng", [8, D]),
        wqkv=ext("wqkv", [2, D, 768]), relbT=ext("relbT", [1, 64]), lam=ext("lam", [1, 256]), subg=ext("subg", [128, 1]),
        wo=ext("wo", [2, D, D]), win_r=ext("win_r", [2 * NJ, 128, 2048]), wout=ext("wout", [2, DFF, D]),
    )
    out = nc.dram_tensor("out", [TOK, D], F32, kind="ExternalOutput").ap()
    dr["Gd_t"] = nc.dram_tensor("Gd", [2, 2560], F32)
    dr["Gd"] = dr["Gd_t"].ap()
    dr["E_d"] = nc.dram_tensor("E_d", [128, 2 * EW], BF16).ap()
    for layer in range(2):
        dr[f"wo_bf{layer}"] = nc.dram_tensor(f"wo_bf{layer}", [D, D], BF16).ap()
        dr[f"win_bf{layer}"] = nc.dram_tensor(f"win_bf{layer}", [NJ, 128, 2048], BF16).ap()
        dr[f"wout_bf{layer}"] = nc.dram_tensor(f"wout_bf{layer}", [DFF, D], BF16).ap()
        for j in range(4):
            for nm, rows, cols in (("hT_loc", 8 * 128, 512), ("hT_all", 4 * 8 * 128, 512), ("oT_loc", 2 * 128, TOK),
                                   ("oT_all", 4 * 2 * 128, TOK)):
                t = nc.dram_tensor(f"{nm}{layer}_{j}", [rows, cols // 2], F32).ap()
                dr[f"{nm}{layer}_{j}_cc"] = t
                dr[f"{nm}{layer}_{j}"] = t.bitcast(BF16)
    with ExitStack() as st:
        C = Ctx(nc, st)
        C.dr = dr
        P = C.P
        emit_setup(C)
        seq = []
        for layer in range(2):
            seq += [("A", layer), ("B", layer), ("C1", layer), ("C2", layer)]
        for ph, layer in seq:
            if stop == "S":
                break
            if ph == "A":
                emit_phase_A(C, layer)
            elif ph == "B":
                emit_phase_B(C, layer, "moba" if layer == 0 else "diff")
            elif ph == "C1":
                emit_phase_C1(C, layer)
            else:
                emit_phase_C2(C, layer)
            if stop == f"{ph}{layer}":
                break
        C.P.barrier()
        for tt in range(TOK // 128):
            P.op("sync", I("dma_start", out=out[tt * 128:(tt + 1) * 128, :], in_=C.xres[:, tt, :]), reads=[f"x{tt}"], dma=True)
        print("fused", P.emit())
    return nc


_CACHE = {}


def kernel(x, c, rel_bias, ada_w, ada_b, norm_g, moba_w_qkv, moba_w_o, diff_w_qkv, diff_w_o, diff_lambda,
           diff_subln_g, ffn_w_in, ffn_w_out):
    f = lambda a: np.ascontiguousarray(np.asarray(a, dtype=np.float32))
    x, c, rel_bias, ada_w, ada_b, norm_g = f(x), f(c), f(rel_bias), f(ada_w), f(ada_b), f(norm_g)
    wqkv_l = [f(moba_w_qkv)[0], f(diff_w_qkv)[0]]
    wo = np.ascontiguousarray(np.stack([f(moba_w_o)[0], f(diff_w_o)[0]], 0))
    lam, subg = f(diff_lambda).reshape(1, 256), f(diff_subln_g).reshape(128, 1)
    ffn_w_in, ffn_w_out = f(ffn_w_in), f(ffn_w_out)
    win_r = np.ascontiguousarray(np.concatenate([
        np.stack([ffn_w_in[l][:, :DFF].reshape(8, 128, NJ, 128), ffn_w_in[l][:, DFF:].reshape(8, 128, NJ, 128)], 0)
        .transpose(3, 2, 0, 1, 4).reshape(NJ, 128, 2048) for l in range(2)], 0))
    ng = np.ascontiguousarray(norm_g.reshape(8, D))
    if "nc" not in _CACHE:
        _CACHE["nc"] = build_fused()
    in_maps = []
    for cc in range(NCORES):
        b, r = cc // 4, cc % 4
        hs = [2 * r, 2 * r + 1]
        ws = []
        for l in range(2):
            wq, wk, wv = np.split(wqkv_l[l], 3, axis=1)
            ws.append(np.concatenate([np.concatenate([m[:, h * 128:(h + 1) * 128] for m in (wq, wk, wv)], 1) for h in hs], 1))
        oh = np.zeros((128, 4), np.float32)
        oh[:, r] = 1.0
        in_maps.append(dict(
            x=np.ascontiguousarray(x[b, r * TOK:(r + 1) * TOK, :]), cT=np.ascontiguousarray(c[b].reshape(8, 128).T), oh=oh,
            adaw=ada_w, adab=ada_b, ng=ng, wqkv=np.ascontiguousarray(np.stack(ws, 0)),
            relbT=np.ascontiguousarray(rel_bias[:, hs].T.reshape(1, 64)), lam=lam, subg=subg,
            wo=wo, win_r=win_r, wout=ffn_w_out))
    res = run_bass_kernel_spmd(_CACHE["nc"], in_maps, core_ids=list(range(NCORES))).results
    out = np.empty((2, SEQ, D), np.float32)
    for cc in range(NCORES):
        out[cc // 4, (cc % 4) * TOK:(cc % 4 + 1) * TOK, :] = res[cc]["out"]
    return out
```

```python
import math
from contextlib import ExitStack
import numpy as np
import ml_dtypes
import concourse.bass as bass
import concourse.mybir as mybir
from concourse.bass_utils import run_bass_kernel_spmd

F32 = mybir.dt.float32
BF16 = mybir.dt.bfloat16
I32 = mybir.dt.int32
ACT = mybir.ActivationFunctionType
ALU = mybir.AluOpType
AX = mybir.AxisListType

D = 1024
SEQ = 8192
NH = 8
DFF = 2816
NJ = DFF // 128
TOK = 2048
NCORES = 8
EPS = 1e-6
OFF = 384
EW = 2432
NEAR_DMAX = 1536
LAMBDA_INIT = 0.8 - 0.6 * math.exp(-0.3 * 1)

ENGS = ("tensor", "vector", "scalar", "gpsimd", "sync")
SEM_CAP = 2048
DMA_RING = 8


class Ins:
    __slots__ = ("eng", "fn", "dma", "raw", "oth", "idx", "sig", "dslot", "dval", "waiters", "cc")


class Prog:
    def __init__(self, nc):
        self.nc = nc
        self.ins = []
        self.last_w = {}
        self.rd_eng = {}
        self.rd_dma = {}
        self.pending = {}
        self.last_eng = {}
        self.dma_hist = {e: [] for e in ENGS}
        self.cc_hist = []

    def barrier(self):
        deps = set(self.last_eng.values())
        for e in ENGS:
            deps.update(self.dma_hist[e][-DMA_RING:])
        deps.update(self.cc_hist)
        for e in ENGS:
            self.pending[e] = set(deps) | self.pending.get(e, set())

    def op(self, eng, fn, reads=(), writes=(), dma=False, cc=False):
        i = Ins()
        i.eng, i.fn, i.dma, i.cc = eng, fn, dma or cc, cc
        i.idx = len(self.ins)
        i.raw, i.oth = set(), set()
        i.sig = None
        i.waiters = False
        for r in reads:
            for w in self.last_w.get(r, ()):
                i.raw.add(w)
        for r in writes:
            ws = self.last_w.get(r, ())
            if not (i.dma and ws and all(self.ins[w].dma for w in ws)):
                for w in ws:
                    i.oth.add(w)
            for rd in self.rd_eng.get(r, {}).values():
                i.oth.add(rd)
            for rd in self.rd_dma.get(r, ()):
                i.oth.add(rd)
        for r in reads:
            if i.dma:
                self.rd_dma.setdefault(r, []).append(i.idx)
            else:
                self.rd_eng.setdefault(r, {})[eng] = i.idx
        for r in writes:
            ws = self.last_w.get(r, ())
            if i.dma and ws and all(self.ins[w].dma for w in ws) and not self.rd_eng.get(r) and not self.rd_dma.get(r):
                self.last_w[r] = list(ws) + [i.idx]
            else:
                self.last_w[r] = [i.idx]
            self.rd_eng[r] = {}
            self.rd_dma[r] = []
        if self.pending.get(eng):
            i.oth |= self.pending.pop(eng)
        if i.cc:
            self.cc_hist.append(i.idx)
        elif i.dma:
            self.dma_hist[eng].append(i.idx)
        else:
            self.last_eng[eng] = i.idx
        i.oth -= i.raw
        i.oth.discard(i.idx)
        i.raw.discard(i.idx)
        self.ins.append(i)
        return i

    def _needed(self, i):
        out = []
        for kind, ds in (("raw", i.raw), ("oth", i.oth)):
            for d in ds:
                dd = self.ins[d]
                if dd.dma or dd.eng != i.eng or i.dma:
                    out.append(d)
                elif i.eng == "tensor":
                    continue
                elif kind == "raw":
                    out.append(d)
        return out

    def emit(self):
        nc = self.nc
        ins = self.ins
        need = [self._needed(i) for i in ins]
        for i, nd in zip(ins, need):
            for d in nd:
                ins[d].waiters = True
        sigcount = {e: 0 for e in ENGS}
        dmacount = {e: 0 for e in ENGS}
        ncc = 0
        for i in ins:
            if i.cc:
                i.sig = ncc
                ncc += 1
            elif i.dma:
                n = dmacount[i.eng]
                dmacount[i.eng] += 1
                i.dslot = n % DMA_RING
                i.dval = 16 * (n // DMA_RING + 1)
                i.sig = n
            elif i.waiters:
                i.sig = sigcount[i.eng]
                sigcount[i.eng] += 1
        with ExitStack() as st:
            esems, dsems = {}, {}
            for e in ENGS:
                k = (sigcount[e] + SEM_CAP - 1) // SEM_CAP
                esems[e] = [st.enter_context(nc.semaphore(f"s_{e}_{j}")) for j in range(k)]
                k = min(DMA_RING, dmacount[e])
                dsems[e] = [st.enter_context(nc.semaphore(f"d_{e}_{j}")) for j in range(k)]
            ccsems = [st.enter_context(nc.semaphore(f"cc_{j}")) for j in range(ncc)]
            block = st.enter_context(nc.Block())

            def target(d):
                dd = ins[d]
                if dd.cc:
                    return (ccsems[dd.sig], 1)
                if dd.dma:
                    return (dsems[dd.eng][dd.dslot], dd.dval)
                return (esems[dd.eng][dd.sig // SEM_CAP], dd.sig % SEM_CAP + 1)

            def run(ename, eng):
                seen = {}
                for i, nd in zip(ins, need):
                    if i.eng != ename:
                        continue
                    waits = {}
                    for d in nd:
                        s, v = target(d)
                        key = id(s)
                        if seen.get(key, 0) >= v:
                            continue
                        if key not in waits or waits[key][1] < v:
                            waits[key] = (s, v)
                    if i.dma and not i.cc and i.sig >= DMA_RING:
                        s = dsems[ename][i.dslot]
                        v = i.dval - 16
                        key = id(s)
                        if seen.get(key, 0) < v and (key not in waits or waits[key][1] < v):
                            waits[key] = (s, v)
                    for key, (s, v) in waits.items():
                        eng.wait_ge(s, v)
                        seen[key] = v
                    r = i.fn(eng)
                    if i.cc:
                        r.then_inc(ccsems[i.sig])
                    elif i.dma:
                        r.then_inc(dsems[ename][i.dslot], 16)
                    elif i.sig is not None:
                        r.then_inc(esems[ename][i.sig // SEM_CAP], 1)
                n = dmacount[ename]
                for slot in range(min(DMA_RING, n)):
                    uses = (n - slot + DMA_RING - 1) // DMA_RING
                    s = dsems[ename][slot]
                    if seen.get(id(s), 0) < 16 * uses:
                        eng.wait_ge(s, 16 * uses)
                if ename == "gpsimd":
                    for s in ccsems:
                        if seen.get(id(s), 0) < 1:
                            eng.wait_ge(s, 1)

            block.tensor(lambda e: run("tensor", e))
            block.vector(lambda e: run("vector", e))
            block.scalar(lambda e: run("scalar", e))
            block.gpsimd(lambda e: run("gpsimd", e))
            block.sync(lambda e: run("sync", e))
        return dict(sig=sigcount, dma=dmacount, n=len(ins))


def I(m, *a, **k):
    return lambda e: getattr(e, m)(*a, **k)


def _dsize(dt):
    return 4 if dt in (F32, I32) else 2


ARENA_BYTES = 134 * 1024


class Ctx:
    def __init__(self, nc, st):
        self.nc, self.st = nc, st
        self.P = Prog(nc)
        self.pb = [st.enter_context(nc.psum_tensor(f"pb{i}", [128, 512], F32)) for i in range(8)]
        self.arena = None
        self.aoff = 0

    def fixed(self, name, shape, dt):
        return self.st.enter_context(self.nc.sbuf_tensor(name, shape, dt))

    def phase(self):
        if self.arena is None:
            self.arena = self.fixed("arena", [128, ARENA_BYTES // 4], F32)
        self.P.barrier()
        self.aoff = 0

    def sb(self, name, shape, dt):
        n = 1
        for v in shape[1:]:
            n *= v
        nb = (n * _dsize(dt) + 31) // 32 * 32
        assert self.aoff + nb <= ARENA_BYTES, (name, self.aoff, nb)
        v = self.arena[0:shape[0], self.aoff // 4:(self.aoff + nb) // 4]
        self.aoff += nb
        if dt != F32:
            v = v.bitcast(dt)
        v = v[:, 0:n]
        if len(shape) == 3:
            v = v.rearrange("p (a b) -> p a b", a=shape[1])
        return v

    def consts(self):
        P = self.P
        self.ident = self.fixed("ident", [128, 128], BF16)
        P.op("gpsimd", I("memset", self.ident[:], 1.0), writes=["ident"])
        P.op("gpsimd", I("affine_select", out=self.ident[:], in_=self.ident[:], pattern=[[-1, 128]],
                         compare_op=ALU.is_equal, fill=0.0, base=0, channel_multiplier=1),
             reads=["ident"], writes=["ident"])
        self.eps = self.fixed("eps", [128, 1], F32)
        P.op("vector", I("memset", self.eps[:], EPS), writes=["eps"])


def emit_rstd(C, ss, rstd, n, res_r, res_w):
    P = C.P
    P.op("scalar", I("activation", out=rstd, in_=ss, func=ACT.Ln, scale=1.0 / n, bias=C.eps[:]),
         reads=list(res_r) + ["eps"], writes=res_w)
    P.op("scalar", I("activation", out=rstd, in_=rstd, func=ACT.Exp, scale=-0.5), reads=res_w, writes=res_w)


def alloc_work(C, CW=128):
    C.w = dict(
        junk=C.sb("junk", [128, D], BF16),
        ss=[C.sb(f"ss{i}", [128, 1], F32) for i in range(2)],
        rstd=[C.sb(f"rstd{i}", [128, 1], F32) for i in range(2)],
        tmp=[C.sb(f"tmp{i}", [128, D], F32) for i in range(2)],
        hbf=[C.sb(f"hbf{i}", [128, D], BF16) for i in range(2)],
    )
    C.mw = dict(
        CW=CW,
        wch=[C.sb(f"adaw_ch{i}", [128, 8, CW], F32) for i in range(2)],
        bch=[C.sb(f"adab_ch{i}", [128, CW], F32) for i in range(2)],
        mtmp=[C.sb(f"mtmp{i}", [128, CW], F32) for i in range(2)],
    )


def emit_mod_piece(C, layer, piece, dst, dres, gi):
    P, M = C.P, C.mw
    CW = M["CW"]
    if gi is not None:
        P.op("sync", I("dma_start", out=dst, in_=C.dr["ng"][layer * 4 + gi:layer * 4 + gi + 1, :].partition_broadcast(128)),
             writes=[dres], dma=True)
    adaw_v = C.dr["adaw"][layer].rearrange("(k p) n -> p k n", p=128)
    for n in range(D // CW):
        s = C.mcount % 2
        C.mcount += 1
        col = piece * D + n * CW
        c0 = n * CW
        P.op("sync", I("dma_start", out=M["wch"][s][:], in_=adaw_v[:, :, col:col + CW]), writes=[f"adaw_ch{s}"], dma=True)
        P.op("sync", I("dma_start", out=M["bch"][s][:], in_=C.dr["adab"][layer:layer + 1, col:col + CW].partition_broadcast(128)),
             writes=[f"adab_ch{s}"], dma=True)
        bank = 6 + s
        for k in range(8):
            P.op("tensor", I("matmul", C.pb[bank][:, 0:CW], lhsT=C.cbc[:, k, :], rhs=M["wch"][s][:, k, :], start=(k == 0),
                             stop=(k == 7)), reads=["cbc", f"adaw_ch{s}"], writes=[f"pb{bank}"])
        if gi is None:
            P.op("vector", I("tensor_tensor", out=dst[:, c0:c0 + CW], in0=C.pb[bank][:, 0:CW], in1=M["bch"][s][:], op=ALU.add),
                 reads=[f"pb{bank}", f"adab_ch{s}"], writes=[dres])
        else:
            P.op("vector", I("tensor_tensor", out=M["mtmp"][s][:], in0=C.pb[bank][:, 0:CW], in1=M["bch"][s][:], op=ALU.add),
                 reads=[f"pb{bank}", f"adab_ch{s}"], writes=[f"mtmp{s}"])
            if piece in (1, 4):
                P.op("vector", I("scalar_tensor_tensor", out=dst[:, c0:c0 + CW], in0=M["mtmp"][s][:], scalar=1.0,
                                 in1=dst[:, c0:c0 + CW], op0=ALU.add, op1=ALU.mult), reads=[f"mtmp{s}", dres], writes=[dres])
            else:
                P.op("vector", I("tensor_tensor", out=dst[:, c0:c0 + CW], in0=M["mtmp"][s][:], in1=dst[:, c0:c0 + CW],
                                 op=ALU.mult), reads=[f"mtmp{s}", dres], writes=[dres])


def emit_norm_mod_T(C, xt, xres, A, Ares, B, Bres, hTg, hTg_res, blk, uid, defer=False):
    P = C.P
    W = C.w
    s = uid % 2
    junk, ss, rstd, tmp, hbf = W["junk"], W["ss"][s], W["rstd"][s], W["tmp"][s], W["hbf"][s]
    P.op("vector", I("memset", ss[:], 0.0), writes=[f"ss{s}"])
    P.op("scalar", I("activation", out=junk[:], in_=xt, func=ACT.Square, accum_out=ss[:]),
         reads=[xres, f"ss{s}"], writes=["junk", f"ss{s}"])
    emit_rstd(C, ss[:], rstd[:], D, [f"ss{s}"], [f"rstd{s}"])
    P.op("vector", I("scalar_tensor_tensor", out=tmp[:], in0=xt, scalar=rstd[:], in1=A, op0=ALU.mult, op1=ALU.mult),
         reads=[xres, f"rstd{s}", Ares], writes=[f"tmp{s}"])
    P.op("gpsimd", I("tensor_tensor", out=hbf[:], in0=tmp[:], in1=B, op=ALU.add),
         reads=[f"tmp{s}", Bres], writes=[f"hbf{s}"])
    if not defer:
        emit_norm_T_part2(C, hTg, hTg_res, blk, uid)


def emit_norm_T_part2(C, hTg, hTg_res, blk, uid):
    P = C.P
    s = uid % 2
    hbf = C.w["hbf"][s]
    bank = 4 + s
    pT = C.pb[bank][:].bitcast(BF16)
    for k in range(8):
        P.op("tensor", I("transpose", out=pT[:, k * 128:(k + 1) * 128], in_=hbf[:, k * 128:(k + 1) * 128],
                         identity=C.ident[:]), reads=[f"hbf{s}", "ident"], writes=[f"pb{bank}"])
    P.op("scalar", I("copy", out=hTg[:, :, blk * 128:(blk + 1) * 128],
                     in_=pT.rearrange("p (k t) -> p k t", k=8)), reads=[f"pb{bank}"], writes=[hTg_res])


def t5_lo():
    n = np.arange(0, 4096)
    nf = np.maximum(n, 1).astype(np.float32)
    large = 16 + (np.log(nf / np.float32(16)) / np.float32(math.log(2048 / 16)) * np.float32(16)).astype(np.int32)
    large = np.minimum(large, 31)
    b = np.where(n < 16, n, large)
    assert all((b == k).any() for k in range(32))
    lo = [int(np.argmax(b == k)) for k in range(32)]
    assert lo[31] <= NEAR_DMAX + 128 - 127, lo
    return lo


def emit_setup(C):
    nc, P, dr = C.nc, C.P, C.dr
    C.consts()
    C.mcount = 0
    C.xres = C.fixed("xres", [128, TOK // 128, D], F32)
    C.cbc = C.fixed("cbc", [128, 8, 128], F32)
    C.b31 = C.fixed("b31", [128, 2], F32)
    C.oh = C.fixed("oh_sb", [128, 4], F32)
    C.phase()
    for tt in range(TOK // 128):
        P.op("sync", I("dma_start", out=C.xres[:, tt, :], in_=dr["x"][tt * 128:(tt + 1) * 128, :]), writes=[f"x{tt}"], dma=True)
    P.op("sync", I("dma_start", out=C.oh[:], in_=dr["oh"]), writes=["oh"], dma=True)
    cT_sb = C.sb("cT_sb", [128, 8], F32)
    cact = C.sb("cact", [128, 8], F32)
    ones = C.sb("ones_f", [128, 128], F32)
    P.op("sync", I("dma_start", out=cT_sb[:], in_=dr["cT"]), writes=["cT_sb"], dma=True)
    P.op("scalar", I("activation", out=cact[:], in_=cT_sb[:], func=ACT.Silu), reads=["cT_sb"], writes=["cact"])
    P.op("vector", I("memset", ones[:], 1.0), writes=["ones_f"])
    for k in range(8):
        P.op("vector", I("tensor_scalar", out=C.cbc[:, k, :], in0=ones[:], scalar1=cact[:, k:k + 1], scalar2=None,
                         op0=ALU.mult), reads=["ones_f", "cact"], writes=["cbc"])
    lo = t5_lo()
    tab = C.sb("tab", [128, 2, 32], F32)
    etab = C.sb("etab", [128, 2, 32], F32)
    cdf = C.sb("cdf", [128, 2, 32], F32)
    P.op("sync", I("dma_start", out=tab[:].rearrange("p a b -> p (a b)"), in_=dr["relbT"].partition_broadcast(128)),
         writes=["tab"], dma=True)
    P.op("scalar", I("activation", out=etab[:], in_=tab[:], func=ACT.Exp), reads=["tab"], writes=["etab"])
    P.op("vector", I("tensor_copy", out=cdf[:, :, 0:1], in_=etab[:, :, 0:1]), reads=["etab"], writes=["cdf"])
    P.op("vector", I("tensor_tensor", out=cdf[:, :, 1:32], in0=etab[:, :, 1:32], in1=etab[:, :, 0:31],
                     op=ALU.subtract), reads=["etab"], writes=["cdf"])
    P.op("vector", I("tensor_copy", out=C.b31[:], in_=tab[:, :, 31]), reads=["tab"], writes=["b31"])
    reli = C.sb("reli", [128, 20], I32)
    relf = C.sb("relf", [128, 20], F32)
    Gt = C.sb("Gt", [128, 2, 20], F32)
    Gtmp = C.sb("Gtmp", [128, 20], F32)
    P.op("gpsimd", I("iota", reli[:], pattern=[[1, 20]], base=-511, channel_multiplier=20), writes=["reli"])
    P.op("vector", I("tensor_copy", out=relf[:], in_=reli[:]), reads=["reli"], writes=["relf"])
    P.op("vector", I("memset", Gt[:], 0.0), writes=["Gt"])
    for hh in range(2):
        for b in range(32):
            P.op("vector", I("tensor_scalar", out=Gtmp[:], in0=relf[:], scalar1=float(lo[b]),
                             scalar2=cdf[:, hh, b:b + 1], op0=ALU.is_ge, op1=ALU.mult),
                 reads=["relf", "cdf"], writes=["Gtmp"])
            P.op("vector", I("tensor_tensor", out=Gt[:, hh, :], in0=Gt[:, hh, :], in1=Gtmp[:], op=ALU.add),
                 reads=["Gt", "Gtmp"], writes=["Gt"])
    P.op("sync", I("dma_start", out=dr["Gd"].rearrange("a (p j) -> p a j", j=20), in_=Gt[:]), reads=["Gt"],
         writes=["Gd"], dma=True)
    C.E = C.sb("E", [128, 2, EW], BF16)
    hank = C.sb("hank", [128, EW], BF16)
    antiI = C.sb("antiI", [128, 128], BF16)
    P.op("gpsimd", I("memset", antiI[:], 1.0), writes=["antiI"])
    P.op("gpsimd", I("affine_select", out=antiI[:], in_=antiI[:], pattern=[[1, 128]], compare_op=ALU.is_equal,
                     fill=0.0, base=-127, channel_multiplier=1), reads=["antiI"], writes=["antiI"])
    for hh in range(2):
        src = bass.AP(dr["Gd_t"], hh * 2560, [[1, 128], [1, EW]])
        P.op("gpsimd", I("dma_start", out=hank[:], in_=src), reads=["Gd"], writes=["hank"], dma=True)
        for c0 in range(0, EW, 512):
            w = min(512, EW - c0)
            P.op("tensor", I("matmul", C.pb[7][:, 0:w], lhsT=antiI[:], rhs=hank[:, c0:c0 + w], start=True, stop=True),
                 reads=["antiI", "hank"], writes=["pb7"])
            P.op("vector", I("tensor_copy", out=C.E[:, hh, c0:c0 + w], in_=C.pb[7][:, 0:w]), reads=["pb7"], writes=["E"])
    P.op("sync", I("dma_start", out=dr["E_d"], in_=C.E.rearrange("p a u -> p (a u)")), reads=["E"], writes=["E_d"], dma=True)


def emit_phase_A(C, layer):
    P, dr = C.P, C.dr
    C.phase()
    alloc_work(C, 512)
    A0 = C.sb("A0", [128, D], F32)
    shA = C.sb("shA", [128, D], F32)
    hTg = [C.sb(f"hTg{i}", [128, 8, 512], BF16) for i in range(2)]
    emit_mod_piece(C, layer, 1, A0, "A0", 0)
    emit_mod_piece(C, layer, 0, shA, "shA", None)
    for tt in range(TOK // 128):
        g, blk = tt // 4, tt % 4
        emit_norm_mod_T(C, C.xres[:, tt, :], f"x{tt}", A0, "A0", shA, "shA", hTg[g % 2], f"hTg{g % 2}", blk, tt)
        if blk == 3:
            nm = f"{layer}_{g}"
            P.op("sync", I("dma_start", out=dr[f"hT_loc{nm}"].rearrange("(k p) t -> p k t", p=128), in_=hTg[g % 2]),
                 reads=[f"hTg{g % 2}"], writes=[f"hT_loc{nm}"], dma=True)
            P.op("gpsimd", I("collective_compute", "AllGather", ALU.bypass, replica_groups=[[0, 1, 2, 3], [4, 5, 6, 7]],
                             ins=[dr[f"hT_loc{nm}_cc"].opt()], outs=[dr[f"hT_all{nm}_cc"].opt()]),
                 reads=[f"hT_loc{nm}"], writes=[f"hT_all{nm}"], cc=True)


def emit_phase_B(C, layer, kind):
    P, dr = C.P, C.dr
    moba = kind == "moba"
    DK = 128 if moba else 64
    scale = DK ** -0.5
    b31 = C.b31
    C.phase()
    E = C.sb("E", [128, 2, EW], BF16)
    P.op("sync", I("dma_start", out=E.rearrange("p a u -> p (a u)"), in_=dr["E_d"]), reads=["E_d"], writes=["E"], dma=True)
    hT_all = [dr[f"hT_all{layer}_{g}"].rearrange("(r k p) t -> r p k t", r=4, k=8) for g in range(4)]
    oT_loc = [dr[f"oT_loc{layer}_{c}"].rearrange("(h p) t -> h p t", h=2) for c in range(4)]
    wsb = C.sb("wsb", [128, 8, 768], BF16)
    P.op("gpsimd", I("dma_start", out=wsb, in_=dr["wqkv"][layer].rearrange("(k p) n -> p k n", p=128)), writes=["wsb"], dma=True)
    if moba:
        selM = C.sb("selM", [32, 32 * 128], BF16)
        P.op("gpsimd", I("memset", selM, 1.0), writes=["selM"])
        P.op("gpsimd", I("affine_select", out=selM, in_=selM, pattern=[[1, 4096]], compare_op=ALU.is_ge,
                         fill=0.0, base=0, channel_multiplier=-128), reads=["selM"], writes=["selM"])
        P.op("gpsimd", I("affine_select", out=selM, in_=selM, pattern=[[-1, 4096]], compare_op=ALU.is_ge,
                         fill=0.0, base=127, channel_multiplier=128), reads=["selM"], writes=["selM"])
        kmf = C.sb("kmf", [128, 32], F32)
        kmb = C.sb("kmb", [128, 32], BF16)
        gate = [C.sb(f"gate{i}", [128, 32], F32) for i in range(2)]
        m8 = [C.sb(f"m8{i}", [128, 8], F32) for i in range(2)]
        sel = [C.sb(f"sel{i}", [128, 32], F32) for i in range(2)]
        nmq = [C.sb(f"nmq{i}", [128, 32], BF16) for i in range(2)]
        nmT = [C.sb(f"nmT{i}", [32, 512], BF16) for i in range(2)]
    else:
        lam_sb = C.sb("lam_sb", [128, 256], F32)
        lp = C.sb("lp", [128, 128], F32)
        ls = C.sb("ls", [128, 2], F32)
        le = C.sb("le", [128, 2], F32)
        neglam = C.sb("neglam", [128, 1], F32)
        subg_bc = C.sb("subg_bc", [128, 128], F32)
        P.op("sync", I("dma_start", out=lam_sb, in_=dr["lam"].partition_broadcast(128)), writes=["lam_sb"], dma=True)
        P.op("sync", I("dma_start", out=subg_bc, in_=dr["subg"].partition_broadcast(128)), writes=["subg_bc"], dma=True)
        lv = lam_sb.rearrange("p (a b c) -> p a b c", a=2, b=2)
        P.op("vector", I("tensor_tensor", out=lp.rearrange("p (a c) -> p a c", a=2), in0=lv[:, :, 0, :],
                         in1=lv[:, :, 1, :], op=ALU.mult), reads=["lam_sb"], writes=["lp"])
        P.op("vector", I("tensor_reduce", out=ls, in_=lp.rearrange("p (a c) -> p a c", a=2), axis=AX.X,
                         op=ALU.add), reads=["lp"], writes=["ls"])
        P.op("scalar", I("activation", out=le, in_=ls, func=ACT.Exp), reads=["ls"], writes=["le"])
        P.op("vector", I("tensor_tensor", out=neglam, in0=le[:, 1:2], in1=le[:, 0:1], op=ALU.subtract),
             reads=["le"], writes=["neglam"])
        P.op("vector", I("tensor_scalar", out=neglam, in0=neglam, scalar1=-LAMBDA_INIT, scalar2=None,
                         op0=ALU.add), reads=["neglam"], writes=["neglam"])
        P.op("vector", I("tensor_scalar", out=subg_bc, in0=subg_bc, scalar1=1.0 - LAMBDA_INIT, scalar2=None,
                         op0=ALU.mult), reads=["subg_bc"], writes=["subg_bc"])
        t1 = [C.sb(f"t1_{i}", [128, 128], F32) for i in range(2)]
        of = [C.sb(f"of{i}", [128, 128], F32) for i in range(2)]
        junk = C.sb("junkd", [128, 128], BF16)
        ssd = [C.sb(f"ssd{i}", [128, 1], F32) for i in range(2)]
        rsd = [C.sb(f"rsd{i}", [128, 1], F32) for i in range(2)]
    QT = C.sb("QT", [128, SEQ], BF16)
    KT = C.sb("KT", [128, SEQ], BF16)
    Vp = C.sb("Vp", [128, 64, 130], BF16)
    hTg = [C.sb(f"hTg{i}", [128, 8, 512], BF16) for i in range(2)]
    NMAP = 1 if moba else 2
    PT = [C.sb(f"PT{i}", [128, 512], BF16) for i in range(4)]
    rec = [C.sb(f"rec{i}", [128, 2], F32) for i in range(2)]
    obf = [C.sb(f"obf{i}", [128, 128], BF16) for i in range(2)]
    OTg = [C.sb(f"OTg{i}", [128, 512], BF16) for i in range(2)]
    def cast_weights():
        for r0 in range(0, D, 256):
            P.op("gpsimd", I("dma_start", out=dr[f"wo_bf{layer}"][r0:r0 + 256, :], in_=dr["wo"][layer][r0:r0 + 256, :]),
                 reads=["QT"], writes=[f"wo_bf{layer}"], dma=True)
        for j in range(NJ):
            P.op("gpsimd", I("dma_start", out=dr[f"win_bf{layer}"][j], in_=dr["win_r"][layer * NJ + j]),
                 reads=["QT"], writes=[f"win_bf{layer}"], dma=True)
        for r0 in range(0, DFF, 256):
            P.op("gpsimd", I("dma_start", out=dr[f"wout_bf{layer}"][r0:r0 + 256, :], in_=dr["wout"][layer][r0:r0 + 256, :]),
                 reads=["QT"], writes=[f"wout_bf{layer}"], dma=True)

    cnt = dict(u=0, g=0, f=0)
    STB = [0, 1, 2, 6]
    for hh in range(2):
        P.op("vector", I("memset", Vp[:, :, 128:129], 1.0), reads=[], writes=["Vp"])
        for gi_ in range(16):
            g = (gi_ // 4) + 4 * (gi_ % 4)
            s = gi_ % 2
            r = g // 4
            P.op("sync", I("dma_start", out=hTg[s], in_=hT_all[g % 4][r]),
                 reads=[f"hT_all{layer}_{g % 4}"], writes=[f"hTg{s}"], dma=True)
            for which, dst, eng in ((0, QT, "scalar"), (1, KT, "vector")):
                bank = which
                c0 = hh * 384 + which * 128
                for k in range(8):
                    P.op("tensor", I("matmul", C.pb[bank][:], lhsT=wsb[:, k, c0:c0 + 128], rhs=hTg[s][:, k, :],
                                     start=(k == 0), stop=(k == 7)), reads=["wsb", f"hTg{s}"], writes=[f"pb{bank}"])
                if eng == "scalar":
                    P.op("scalar", I("copy", out=dst[:, g * 512:(g + 1) * 512], in_=C.pb[bank][:]),
                         reads=[f"pb{bank}"], writes=["QT"])
                else:
                    P.op("vector", I("tensor_copy", out=dst[:, g * 512:(g + 1) * 512], in_=C.pb[bank][:]),
                         reads=[f"pb{bank}"], writes=["KT"])
            c0 = hh * 384 + 256
            for i in range(4):
                for k in range(8):
                    P.op("tensor", I("matmul", C.pb[2][:, i * 128:(i + 1) * 128], lhsT=hTg[s][:, k, i * 128:(i + 1) * 128],
                                     rhs=wsb[:, k, c0:c0 + 128], start=(k == 0), stop=(k == 7)),
                         reads=["wsb", f"hTg{s}"], writes=["pb2"])
            P.op("vector", I("tensor_copy", out=Vp[:, g * 4:(g + 1) * 4, 0:128],
                             in_=C.pb[2][:].rearrange("p (i d) -> p i d", i=4)), reads=["pb2"], writes=["Vp"])
        if hh == 0:
            cast_weights()
        if moba:
            P.op("vector", I("tensor_reduce", out=kmf, in_=KT.rearrange("p (b t) -> p b t", t=256), axis=AX.X,
                             op=ALU.add), reads=["KT"], writes=["kmf"])
            P.op("vector", I("tensor_copy", out=kmb, in_=kmf), reads=["kmf"], writes=["kmb"])
        def gating(qg):
            ns = qg % 2
            for i in range(4):
                qt = qg * 4 + i
                own = qt // 2
                gs = cnt["g"] % 2
                cnt["g"] += 1
                if own <= 3:
                    P.op("vector", I("memset", sel[gs], 0.0), writes=[f"sel{gs}"])
                    P.op("vector", I("memset", sel[gs][:, 0:own + 1], 1.0), writes=[f"sel{gs}"])
                else:
                    P.op("tensor", I("matmul", C.pb[7][:, 0:32], lhsT=QT[:, qt * 128:(qt + 1) * 128], rhs=kmb,
                                     start=True, stop=True), reads=["QT", "kmb"], writes=["pb7"])
                    P.op("vector", I("tensor_copy", out=gate[gs], in_=C.pb[7][:, 0:32]), reads=["pb7"],
                         writes=[f"gate{gs}"])
                    P.op("vector", I("memset", gate[gs][:, own:32], -1e30), writes=[f"gate{gs}"])
                    P.op("vector", I("max", out=m8[gs], in_=gate[gs]), reads=[f"gate{gs}"], writes=[f"m8{gs}"])
                    P.op("vector", I("tensor_scalar", out=sel[gs], in0=gate[gs], scalar1=m8[gs][:, 2:3],
                                     scalar2=None, op0=ALU.is_ge), reads=[f"gate{gs}", f"m8{gs}"],
                         writes=[f"sel{gs}"])
                    P.op("vector", I("memset", sel[gs][:, own:own + 1], 1.0), writes=[f"sel{gs}"])
                P.op("vector", I("tensor_scalar", out=nmq[gs], in0=sel[gs], scalar1=-1.0, scalar2=30000.0,
                                 op0=ALU.add, op1=ALU.mult), reads=[f"sel{gs}"], writes=[f"nmq{gs}"])
                pT7 = C.pb[7][:].bitcast(BF16)
                P.op("tensor", I("transpose", out=pT7[0:32, 512:640], in_=nmq[gs], identity=C.ident[:]),
                     reads=[f"nmq{gs}", "ident"], writes=["pb7"])
                P.op("vector", I("tensor_copy", out=nmT[ns][:, i * 128:(i + 1) * 128], in_=pT7[0:32, 512:640]),
                     reads=["pb7"], writes=[f"nmT{ns}"])

        def emit_qk(pr):
            qg, kt, mp, u = pr
            q0 = qg * 512
            sb_ = STB[u % 4]
            r0 = mp * 64 if not moba else 0
            P.op("tensor", I("matmul", C.pb[sb_][:], lhsT=KT[r0:r0 + DK, kt * 128:(kt + 1) * 128],
                             rhs=QT[r0:r0 + DK, q0:q0 + 512], start=True, stop=not moba),
                 reads=["KT", "QT"], writes=[f"pb{sb_}"])
            if moba:
                j = kt // 2
                P.op("tensor", I("matmul", C.pb[sb_][:], lhsT=selM[:, j * 128:(j + 1) * 128], rhs=nmT[qg % 2],
                                 start=False, stop=True), reads=["selM", f"nmT{qg % 2}"], writes=[f"pb{sb_}"])

        def emit_rest(pr):
            qg, kt, mp, u = pr
            q0 = qg * 512
            sb_, ps_ = STB[u % 4], u % 4
            Dq = q0 - kt * 128
            if Dq <= NEAR_DMAX:
                P.op("scalar", I("activation", out=PT[ps_], in_=C.pb[sb_][:], func=ACT.Exp, scale=scale),
                     reads=[f"pb{sb_}"], writes=[f"PT{ps_}"])
                P.op("vector", I("tensor_tensor", out=PT[ps_], in0=PT[ps_],
                                 in1=E[:, hh, Dq + OFF:Dq + OFF + 512], op=ALU.mult),
                     reads=[f"PT{ps_}", "E"], writes=[f"PT{ps_}"])
            else:
                P.op("scalar", I("activation", out=PT[ps_], in_=C.pb[sb_][:], func=ACT.Exp, scale=scale,
                                 bias=b31[:, hh:hh + 1]), reads=[f"pb{sb_}", "b31"], writes=[f"PT{ps_}"])
            for i in range(4):
                if kt > 4 * qg + i:
                    continue
                a_ = mp * 4 + i
                ob = 3 + a_ // 3
                oc = (a_ % 3) * 160
                P.op("tensor", I("matmul", C.pb[ob][:, oc:oc + 129], lhsT=PT[ps_][:, i * 128:(i + 1) * 128],
                                 rhs=Vp[:, kt, 0:129], start=(kt == 0 and a_ % 3 == 0), stop=(kt == 4 * qg + i),
                                 skip_group_check=True),
                     reads=[f"PT{ps_}", "Vp"], writes=[f"pb{ob}"])

        def finalize(qg):
            q0 = qg * 512
            og = qg % 2
            pT7 = C.pb[7][:].bitcast(BF16)
            for i in range(4):
                fs = cnt["f"] % 2
                cnt["f"] += 1
                ob0, oc = 3 + i // 3, (i % 3) * 160
                P.op("vector", I("reciprocal", out=rec[fs][:, 0:1], in_=C.pb[ob0][:, oc + 128:oc + 129]),
                     reads=[f"pb{ob0}"], writes=[f"rec{fs}"])
                if moba:
                    P.op("vector", I("tensor_scalar", out=obf[fs], in0=C.pb[ob0][:, oc:oc + 128],
                                     scalar1=rec[fs][:, 0:1], scalar2=None, op0=ALU.mult),
                         reads=[f"pb{ob0}", f"rec{fs}"], writes=[f"obf{fs}"])
                else:
                    ob1, oc1 = 3 + (4 + i) // 3, ((4 + i) % 3) * 160
                    P.op("vector", I("reciprocal", out=rec[fs][:, 1:2], in_=C.pb[ob1][:, oc1 + 128:oc1 + 129]),
                         reads=[f"pb{ob1}"], writes=[f"rec{fs}"])
                    P.op("vector", I("tensor_tensor", out=rec[fs][:, 1:2], in0=rec[fs][:, 1:2], in1=neglam,
                                     op=ALU.mult), reads=[f"rec{fs}", "neglam"], writes=[f"rec{fs}"])
                    P.op("vector", I("tensor_scalar", out=t1[fs], in0=C.pb[ob0][:, oc:oc + 128],
                                     scalar1=rec[fs][:, 0:1], scalar2=None, op0=ALU.mult),
                         reads=[f"pb{ob0}", f"rec{fs}"], writes=[f"t1_{fs}"])
                    P.op("vector", I("scalar_tensor_tensor", out=of[fs], in0=C.pb[ob1][:, oc1:oc1 + 128],
                                     scalar=rec[fs][:, 1:2], in1=t1[fs], op0=ALU.mult, op1=ALU.add),
                         reads=[f"pb{ob1}", f"rec{fs}", f"t1_{fs}"], writes=[f"of{fs}"])
                    P.op("vector", I("memset", ssd[fs], 0.0), writes=[f"ssd{fs}"])
                    P.op("scalar", I("activation", out=junk, in_=of[fs], func=ACT.Square, accum_out=ssd[fs]),
                         reads=[f"of{fs}", f"ssd{fs}"], writes=["junkd", f"ssd{fs}"])
                    emit_rstd(C, ssd[fs], rsd[fs], 128, [f"ssd{fs}"], [f"rsd{fs}"])
                    P.op("vector", I("scalar_tensor_tensor", out=obf[fs], in0=of[fs], scalar=rsd[fs],
                                     in1=subg_bc, op0=ALU.mult, op1=ALU.mult),
                         reads=[f"of{fs}", f"rsd{fs}", "subg_bc"], writes=[f"obf{fs}"])
                P.op("tensor", I("transpose", out=pT7[:, i * 128:(i + 1) * 128], in_=obf[fs], identity=C.ident[:]),
                     reads=[f"obf{fs}", "ident"], writes=["pb7"])
            P.op("scalar", I("copy", out=OTg[og], in_=pT7[:, 0:512]), reads=["pb7"], writes=[f"OTg{og}"])
            ch = qg // 4
            nm = f"{layer}_{ch}"
            P.op("sync", I("dma_start", out=oT_loc[ch][hh][:, (qg % 4) * 512:(qg % 4 + 1) * 512], in_=OTg[og]),
                 reads=[f"OTg{og}"], writes=[f"oT_loc{nm}"], dma=True)
            if hh == 1 and qg % 4 == 3:
                P.op("gpsimd", I("collective_compute", "AllGather", ALU.bypass, replica_groups=[[0, 1, 2, 3], [4, 5, 6, 7]],
                                 ins=[dr[f"oT_loc{nm}_cc"].opt()], outs=[dr[f"oT_all{nm}_cc"].opt()]),
                     reads=[f"oT_loc{nm}"], writes=[f"oT_all{nm}"], cc=True)

        pairs = []
        for qg in range(16):
            for kt in range(4 * qg + 4):
                for mp in range(NMAP):
                    pairs.append((qg, kt, mp, cnt["u"]))
                    cnt["u"] += 1
        G = NMAP
        LOOKG = 2 if moba else 1
        n = len(pairs)
        for base in range(0, n + LOOKG * G, G):
            for idx in range(base, base + G):
                if idx < n:
                    pr = pairs[idx]
                    if moba and pr[1] == 0 and pr[2] == 0:
                        if pr[0] == 0:
                            gating(0)
                        if pr[0] + 1 < 16:
                            gating(pr[0] + 1)
                    emit_qk(pr)
            for idx in range(base - LOOKG * G, base - LOOKG * G + G):
                if 0 <= idx < n:
                    pr = pairs[idx]
                    emit_rest(pr)
                    if pr[1] == 4 * pr[0] + 3 and pr[2] == NMAP - 1:
                        finalize(pr[0])


def resid_update(C, xtile, xres, banks, Grow, Gres, u):
    P, W = C.P, C.w
    R = C.rw
    s = u % 2
    P.op("vector", I("memset", R["ssy"][s], 0.0), writes=[f"ssy{s}"])
    for half in range(2):
        P.op("scalar", I("activation", out=W["junk"][:, 0:512], in_=C.pb[banks[half]][:], func=ACT.Square,
                         accum_out=R["ssy"][s][:, half:half + 1]), reads=[f"pb{banks[half]}", f"ssy{s}"],
             writes=["junk", f"ssy{s}"])
    P.op("vector", I("tensor_tensor", out=R["ss1"][s], in0=R["ssy"][s][:, 0:1], in1=R["ssy"][s][:, 1:2], op=ALU.add),
         reads=[f"ssy{s}"], writes=[f"ss1_{s}"])
    emit_rstd(C, R["ss1"][s], R["rsy"][s], D, [f"ss1_{s}"], [f"rsy{s}"])
    for half in range(2):
        hs = slice(half * 512, (half + 1) * 512)
        P.op("vector", I("scalar_tensor_tensor", out=W["tmp"][s][:, hs], in0=C.pb[banks[half]][:], scalar=R["rsy"][s],
                         in1=Grow[:, hs], op0=ALU.mult, op1=ALU.mult),
             reads=[f"pb{banks[half]}", f"rsy{s}", Gres], writes=[f"tmp{s}"])
    P.op("gpsimd", I("tensor_tensor", out=xtile, in0=xtile, in1=W["tmp"][s], op=ALU.add),
         reads=[xres, f"tmp{s}"], writes=[xres])


def alloc_resid(C):
    C.rw = dict(
        ssy=[C.sb(f"ssy{i}", [128, 2], F32) for i in range(2)],
        ss1=[C.sb(f"ss1_{i}", [128, 1], F32) for i in range(2)],
        rsy=[C.sb(f"rsy{i}", [128, 1], F32) for i in range(2)],
    )


def emit_phase_C1(C, layer):
    P, dr = C.P, C.dr
    C.phase()
    alloc_work(C, 512)
    alloc_resid(C)
    G1 = C.sb("G1", [128, D], F32)
    emit_mod_piece(C, layer, 2, G1, "G1", 1)
    wo_sb = C.sb("wo_sb", [128, 8, D], BF16)
    wo_v = dr[f"wo_bf{layer}"].rearrange("(h p) n -> p h n", p=128)
    for h0 in range(0, 8, 4):
        P.op("sync", I("dma_start", out=wo_sb[:, h0:h0 + 4, :], in_=wo_v[:, h0:h0 + 4, :]), reads=[f"wo_bf{layer}"],
             writes=["wo_sb"], dma=True)
    cand = [C.sb(f"cand{i}", [128, 8, 512], BF16) for i in range(2)]
    OTg = [C.sb(f"OTg{i}", [128, 8, 512], BF16) for i in range(2)]
    oT_all = [dr[f"oT_all{layer}_{c}"].rearrange("(h p) t -> p h t", p=128) for c in range(4)]
    uid = 0
    cc_ = 0
    for tg in range(4):
        gs = tg % 2
        for c in range(4):
            cs = cc_ % 2
            cc_ += 1
            P.op("sync", I("dma_start", out=cand[cs], in_=oT_all[c][:, :, tg * 512:(tg + 1) * 512]),
                 reads=[f"oT_all{layer}_{c}"], writes=[f"cand{cs}"], dma=True)
            for eng, part, hs_ in (("vector", "a", slice(0, 8)),):
                if c == 0:
                    P.op(eng, I("tensor_scalar", out=OTg[gs][:, hs_, :], in0=cand[cs][:, hs_, :], scalar1=C.oh[:, 0:1],
                                scalar2=None, op0=ALU.mult), reads=[f"cand{cs}", "oh"], writes=[f"OTg{gs}{part}"])
                else:
                    P.op(eng, I("scalar_tensor_tensor", out=OTg[gs][:, hs_, :], in0=cand[cs][:, hs_, :],
                                scalar=C.oh[:, c:c + 1], in1=OTg[gs][:, hs_, :], op0=ALU.mult, op1=ALU.add),
                         reads=[f"cand{cs}", "oh", f"OTg{gs}{part}"], writes=[f"OTg{gs}{part}"])
        for i in range(4):
            tt = tg * 4 + i
            banks = (0, 1) if i % 2 == 0 else (2, 3)
            for half in range(2):
                for h in range(8):
                    P.op("tensor", I("matmul", C.pb[banks[half]][:], lhsT=OTg[gs][:, h, i * 128:(i + 1) * 128],
                                     rhs=wo_sb[:, h, half * 512:(half + 1) * 512], start=(h == 0), stop=(h == 7)),
                         reads=[f"OTg{gs}a", "wo_sb"], writes=[f"pb{banks[half]}"])
            resid_update(C, C.xres[:, tt, :], f"x{tt}", banks, G1, "G1", uid)
            uid += 1


def emit_phase_C2(C, layer):
    P, dr = C.P, C.dr
    C.phase()
    alloc_work(C)
    alloc_resid(C)
    A2 = C.sb("A2", [128, D], F32)
    shF = C.sb("shF", [128, D], F32)
    G3 = C.sb("G3", [128, D], F32)
    wout_sb = C.sb("wout_sb", [128, NJ, D], BF16)
    wout_v = dr[f"wout_bf{layer}"].rearrange("(j p) n -> p j n", p=128)
    for j0 in range(0, NJ, 6):
        j1 = min(NJ, j0 + 6)
        P.op("sync", I("dma_start", out=wout_sb[:, j0:j1, :], in_=wout_v[:, j0:j1, :]), reads=[f"wout_bf{layer}"],
             writes=["wout_sb"], dma=True)
    emit_mod_piece(C, layer, 4, A2, "A2", 2)
    emit_mod_piece(C, layer, 3, shF, "shF", None)
    emit_mod_piece(C, layer, 5, G3, "G3", 3)
    h2T = [C.sb(f"h2T{i}", [128, 8, 512], BF16) for i in range(2)]
    actT = C.sb("actT", [128, NJ, 512], BF16)
    wch = [C.sb(f"wch{i}", [128, 2, 8 * 128], BF16) for i in range(2)]
    sg = [C.sb(f"sg{i}", [128, 512], BF16) for i in range(2)]
    uid = 0
    wcount = 0
    def pre1(tg, i):
        tt = tg * 4 + i
        emit_norm_mod_T(C, C.xres[:, tt, :], f"x{tt}", A2, "A2", shF, "shF", h2T[tg % 2], f"h2T{tg % 2}", i, tg * 4 + i,
                        defer=True)

    def pre2(tg, i):
        emit_norm_T_part2(C, h2T[tg % 2], f"h2T{tg % 2}", i, tg * 4 + i)

    for i in range(4):
        pre1(0, i)
        pre2(0, i)
    for tg in range(4):
        for j in range(NJ):
            if tg + 1 < 4 and j % 5 == 1 and j // 5 < 4:
                pre1(tg + 1, j // 5)
            if tg + 1 < 4 and j % 5 == 4 and j // 5 < 4:
                pre2(tg + 1, j // 5)
            ws = wcount % 2
            wcount += 1
            P.op("sync", I("dma_start", out=wch[ws].rearrange("p a n -> p (a n)"), in_=dr[f"win_bf{layer}"][j]),
                 reads=[f"win_bf{layer}"], writes=[f"wch{ws}"], dma=True)
            bg, bu = (0, 1) if j % 2 == 0 else (2, 3)
            for a, bank in ((0, bg), (1, bu)):
                for k in range(8):
                    P.op("tensor", I("matmul", C.pb[bank][:], lhsT=wch[ws][:, a, k * 128:(k + 1) * 128],
                                     rhs=h2T[tg % 2][:, k, :], start=(k == 0), stop=(k == 7)),
                         reads=[f"wch{ws}", f"h2T{tg % 2}"], writes=[f"pb{bank}"])
            s2 = j % 2
            P.op("scalar", I("activation", out=sg[s2], in_=C.pb[bg][:], func=ACT.Silu), reads=[f"pb{bg}"],
                 writes=[f"sg{s2}"])
            P.op("vector", I("tensor_tensor", out=actT[:, j, :], in0=sg[s2], in1=C.pb[bu][:], op=ALU.mult),
                 reads=[f"sg{s2}", f"pb{bu}"], writes=["actT"])
        for i in range(4):
            tt = tg * 4 + i
            banks = (6, 7) if i % 2 == 0 else (0, 1)
            for half in range(2):
                for j in range(NJ):
                    P.op("tensor", I("matmul", C.pb[banks[half]][:], lhsT=actT[:, j, i * 128:(i + 1) * 128],
                                     rhs=wout_sb[:, j, half * 512:(half + 1) * 512], start=(j == 0), stop=(j == NJ - 1)),
                         reads=["actT", "wout_sb"], writes=[f"pb{banks[half]}"])
            resid_update(C, C.xres[:, tt, :], f"x{tt}", banks, G3, "G3", uid)
            uid += 1


def build_fused(stop=None):
    nc = bass.Bass("TRN2", target_bir_lowering=False)
    ext = lambda name, shape, dt=F32: nc.dram_tensor(name, shape, dt, kind="ExternalInput").ap()
    dr = dict(
        x=ext("x", [TOK, D]), cT=ext("cT", [128, 8]), oh=ext("oh", [128, 4]),
        adaw=ext("adaw", [2, D, 6 * D]), adab=ext("adab", [2, 6 * D]), ng=ext("ng", [8, D]),
        wqkv=ext("wqkv", [2, D, 768]), relbT=ext("relbT", [1, 64]), lam=ext("lam", [1, 256]), subg=ext("subg", [1, 128]),
        wo=ext("wo", [2, D, D]), win_r=ext("win_r", [2 * NJ, 128, 2048]), wout=ext("wout", [2, DFF, D]),
    )
    out = nc.dram_tensor("out", [TOK, D], F32, kind="ExternalOutput").ap()
    dr["Gd_t"] = nc.dram_tensor("Gd", [2, 2560], F32)
    dr["Gd"] = dr["Gd_t"].ap()
    dr["E_d"] = nc.dram_tensor("E_d", [128, 2 * EW], BF16).ap()
    for layer in range(2):
        dr[f"wo_bf{layer}"] = nc.dram_tensor(f"wo_bf{layer}", [D, D], BF16).ap()
        dr[f"win_bf{layer}"] = nc.dram_tensor(f"win_bf{layer}", [NJ, 128, 2048], BF16).ap()
        dr[f"wout_bf{layer}"] = nc.dram_tensor(f"wout_bf{layer}", [DFF, D], BF16).ap()
        for j in range(4):
            for nm, rows, cols in (("hT_loc", 8 * 128, 512), ("hT_all", 4 * 8 * 128, 512), ("oT_loc", 2 * 128, TOK),
                                   ("oT_all", 4 * 2 * 128, TOK)):
                t = nc.dram_tensor(f"{nm}{layer}_{j}", [rows, cols // 2], F32).ap()
                dr[f"{nm}{layer}_{j}_cc"] = t
                dr[f"{nm}{layer}_{j}"] = t.bitcast(BF16)
    with ExitStack() as st:
        C = Ctx(nc, st)
        C.dr = dr
        P = C.P
        emit_setup(C)
        seq = []
        for layer in range(2):
            seq += [("A", layer), ("B", layer), ("C1", layer), ("C2", layer)]
        for ph, layer in seq:
            if stop == "S":
                break
            if ph == "A":
                emit_phase_A(C, layer)
            elif ph == "B":
                emit_phase_B(C, layer, "moba" if layer == 0 else "diff")
            elif ph == "C1":
                emit_phase_C1(C, layer)
            else:
                emit_phase_C2(C, layer)
            if stop == f"{ph}{layer}":
                break
        C.P.barrier()
        for tt in range(TOK // 128):
            P.op("sync", I("dma_start", out=out[tt * 128:(tt + 1) * 128, :], in_=C.xres[:, tt, :]), reads=[f"x{tt}"], dma=True)
        print("fused", P.emit())
    return nc


_CACHE = {}


def kernel(x, c, rel_bias, ada_w, ada_b, norm_g, moba_w_qkv, moba_w_o, diff_w_qkv, diff_w_o, diff_lambda,
           diff_subln_g, ffn_w_in, ffn_w_out):
    f = lambda a: np.ascontiguousarray(np.asarray(a, dtype=np.float32))
    x, c, rel_bias, ada_w, ada_b, norm_g = f(x), f(c), f(rel_bias), f(ada_w), f(ada_b), f(norm_g)
    wqkv_l = [f(moba_w_qkv)[0], f(diff_w_qkv)[0]]
    wo = np.ascontiguousarray(np.stack([f(moba_w_o)[0], f(diff_w_o)[0]], 0))
    lam, subg = f(diff_lambda).reshape(1, 256), f(diff_subln_g).reshape(1, 128)
    ffn_w_in, ffn_w_out = f(ffn_w_in), f(ffn_w_out)
    win_r = np.ascontiguousarray(np.concatenate([
        np.stack([ffn_w_in[l][:, :DFF].reshape(8, 128, NJ, 128), ffn_w_in[l][:, DFF:].reshape(8, 128, NJ, 128)], 0)
        .transpose(3, 2, 0, 1, 4).reshape(NJ, 128, 2048) for l in range(2)], 0))
    ng = np.ascontiguousarray(norm_g.reshape(8, D))
    if "nc" not in _CACHE:
        _CACHE["nc"] = build_fused()
    in_maps = []
    for cc in range(NCORES):
        b, r = cc // 4, cc % 4
        hs = [2 * r, 2 * r + 1]
        ws = []
        for l in range(2):
            wq, wk, wv = np.split(wqkv_l[l], 3, axis=1)
            ws.append(np.concatenate([np.concatenate([m[:, h * 128:(h + 1) * 128] for m in (wq, wk, wv)], 1) for h in hs], 1))
        oh = np.zeros((128, 4), np.float32)
        oh[:, r] = 1.0
        in_maps.append(dict(
            x=np.ascontiguousarray(x[b, r * TOK:(r + 1) * TOK, :]), cT=np.ascontiguousarray(c[b].reshape(8, 128).T), oh=oh,
            adaw=ada_w, adab=ada_b, ng=ng, wqkv=np.ascontiguousarray(np.stack(ws, 0)),
            relbT=np.ascontiguousarray(rel_bias[:, hs].T.reshape(1, 64)), lam=lam, subg=subg,
            wo=wo, win_r=win_r, wout=ffn_w_out))
    res = run_bass_kernel_spmd(_CACHE["nc"], in_maps, core_ids=list(range(NCORES))).results
    out = np.empty((2, SEQ, D), np.float32)
    for cc in range(NCORES):
        out[cc // 4, (cc % 4) * TOK:(cc % 4 + 1) * TOK, :] = res[cc]["out"]
    return out
```

```python
import math
from contextlib import ExitStack
import numpy as np
import ml_dtypes
import concourse.bass as bass
import concourse.mybir as mybir
from concourse.bass_utils import run_bass_kernel_spmd

F32 = mybir.dt.float32
BF16 = mybir.dt.bfloat16
I32 = mybir.dt.int32
ACT = mybir.ActivationFunctionType
ALU = mybir.AluOpType
AX = mybir.AxisListType

D = 1024
SEQ = 8192
NH = 8
DFF = 2816
NJ = DFF // 128
TOK = 2048
NCORES = 8
EPS = 1e-6
OFF = 384
EW = 2432
NEAR_DMAX = 1536
LAMBDA_INIT = 0.8 - 0.6 * math.exp(-0.3 * 1)

ENGS = ("tensor", "vector", "scalar", "gpsimd", "sync")
SEM_CAP = 2048
DMA_RING = 8


class Ins:
    __slots__ = ("eng", "fn", "dma", "raw", "oth", "idx", "sig", "dslot", "dval", "waiters", "cc")


class Prog:
    def __init__(self, nc):
        self.nc = nc
        self.ins = []
        self.last_w = {}
        self.rd_eng = {}
        self.rd_dma = {}
        self.pending = {}
        self.last_eng = {}
        self.dma_hist = {e: [] for e in ENGS}
        self.cc_hist = []

    def barrier(self):
        deps = set(self.last_eng.values())
        for e in ENGS:
            deps.update(self.dma_hist[e][-DMA_RING:])
        deps.update(self.cc_hist)
        for e in ENGS:
            self.pending[e] = set(deps) | self.pending.get(e, set())

    def op(self, eng, fn, reads=(), writes=(), dma=False, cc=False):
        i = Ins()
        i.eng, i.fn, i.dma, i.cc = eng, fn, dma or cc, cc
        i.idx = len(self.ins)
        i.raw, i.oth = set(), set()
        i.sig = None
        i.waiters = False
        for r in reads:
            for w in self.last_w.get(r, ()):
                i.raw.add(w)
        for r in writes:
            ws = self.last_w.get(r, ())
            if not (i.dma and ws and all(self.ins[w].dma for w in ws)):
                for w in ws:
                    i.oth.add(w)
            for rd in self.rd_eng.get(r, {}).values():
                i.oth.add(rd)
            for rd in self.rd_dma.get(r, ()):
                i.oth.add(rd)
        for r in reads:
            if i.dma:
                self.rd_dma.setdefault(r, []).append(i.idx)
            else:
                self.rd_eng.setdefault(r, {})[eng] = i.idx
        for r in writes:
            ws = self.last_w.get(r, ())
            if i.dma and ws and all(self.ins[w].dma for w in ws) and not self.rd_eng.get(r) and not self.rd_dma.get(r):
                self.last_w[r] = list(ws) + [i.idx]
            else:
                self.last_w[r] = [i.idx]
            self.rd_eng[r] = {}
            self.rd_dma[r] = []
        if self.pending.get(eng):
            i.oth |= self.pending.pop(eng)
        if i.cc:
            self.cc_hist.append(i.idx)
        elif i.dma:
            self.dma_hist[eng].append(i.idx)
        else:
            self.last_eng[eng] = i.idx
        i.oth -= i.raw
        i.oth.discard(i.idx)
        i.raw.discard(i.idx)
        self.ins.append(i)
        return i

    def _needed(self, i):
        out = []
        for kind, ds in (("raw", i.raw), ("oth", i.oth)):
            for d in ds:
                dd = self.ins[d]
                if dd.dma or dd.eng != i.eng or i.dma:
                    out.append(d)
                elif i.eng == "tensor":
                    continue
                elif kind == "raw":
                    out.append(d)
        return out

    def emit(self):
        nc = self.nc
        ins = self.ins
        need = [self._needed(i) for i in ins]
        for i, nd in zip(ins, need):
            for d in nd:
                ins[d].waiters = True
        sigcount = {e: 0 for e in ENGS}
        dmacount = {e: 0 for e in ENGS}
        ncc = 0
        for i in ins:
            if i.cc:
                i.sig = ncc
                ncc += 1
            elif i.dma:
                n = dmacount[i.eng]
                dmacount[i.eng] += 1
                i.dslot = n % DMA_RING
                i.dval = 16 * (n // DMA_RING + 1)
                i.sig = n
            elif i.waiters:
                i.sig = sigcount[i.eng]
                sigcount[i.eng] += 1
        with ExitStack() as st:
            esems, dsems = {}, {}
            for e in ENGS:
                k = (sigcount[e] + SEM_CAP - 1) // SEM_CAP
                esems[e] = [st.enter_context(nc.semaphore(f"s_{e}_{j}")) for j in range(k)]
                k = min(DMA_RING, dmacount[e])
                dsems[e] = [st.enter_context(nc.semaphore(f"d_{e}_{j}")) for j in range(k)]
            ccsems = [st.enter_context(nc.semaphore(f"cc_{j}")) for j in range(ncc)]
            block = st.enter_context(nc.Block())

            def target(d):
                dd = ins[d]
                if dd.cc:
                    return (ccsems[dd.sig], 1)
                if dd.dma:
                    return (dsems[dd.eng][dd.dslot], dd.dval)
                return (esems[dd.eng][dd.sig // SEM_CAP], dd.sig % SEM_CAP + 1)

            def run(ename, eng):
                seen = {}
                for i, nd in zip(ins, need):
                    if i.eng != ename:
                        continue
                    waits = {}
                    for d in nd:
                        s, v = target(d)
                        key = id(s)
                        if seen.get(key, 0) >= v:
                            continue
                        if key not in waits or waits[key][1] < v:
                            waits[key] = (s, v)
                    if i.dma and not i.cc and i.sig >= DMA_RING:
                        s = dsems[ename][i.dslot]
                        v = i.dval - 16
                        key = id(s)
                        if seen.get(key, 0) < v and (key not in waits or waits[key][1] < v):
                            waits[key] = (s, v)
                    for key, (s, v) in waits.items():
                        eng.wait_ge(s, v)
                        seen[key] = v
                    r = i.fn(eng)
                    if i.cc:
                        r.then_inc(ccsems[i.sig])
                    elif i.dma:
                        r.then_inc(dsems[ename][i.dslot], 16)
                    elif i.sig is not None:
                        r.then_inc(esems[ename][i.sig // SEM_CAP], 1)
                n = dmacount[ename]
                for slot in range(min(DMA_RING, n)):
                    uses = (n - slot + DMA_RING - 1) // DMA_RING
                    s = dsems[ename][slot]
                    if seen.get(id(s), 0) < 16 * uses:
                        eng.wait_ge(s, 16 * uses)
                if ename == "gpsimd":
                    for s in ccsems:
                        if seen.get(id(s), 0) < 1:
                            eng.wait_ge(s, 1)

            block.tensor(lambda e: run("tensor", e))
            block.vector(lambda e: run("vector", e))
            block.scalar(lambda e: run("scalar", e))
            block.gpsimd(lambda e: run("gpsimd", e))
            block.sync(lambda e: run("sync", e))
        return dict(sig=sigcount, dma=dmacount, n=len(ins))


def I(m, *a, **k):
    return lambda e: getattr(e, m)(*a, **k)


def _dsize(dt):
    return 4 if dt in (F32, I32) else 2


ARENA_BYTES = 134 * 1024


class Ctx:
    def __init__(self, nc, st):
        self.nc, self.st = nc, st
        self.P = Prog(nc)
        self.pb = [st.enter_context(nc.psum_tensor(f"pb{i}", [128, 512], F32)) for i in range(8)]
        self.arena = None
        self.aoff = 0

    def fixed(self, name, shape, dt):
        return self.st.enter_context(self.nc.sbuf_tensor(name, shape, dt))

    def phase(self):
        if self.arena is None:
            self.arena = self.fixed("arena", [128, ARENA_BYTES // 4], F32)
        self.P.barrier()
        self.aoff = 0

    def sb(self, name, shape, dt):
        n = 1
        for v in shape[1:]:
            n *= v
        nb = (n * _dsize(dt) + 31) // 32 * 32
        assert self.aoff + nb <= ARENA_BYTES, (name, self.aoff, nb)
        v = self.arena[0:shape[0], self.aoff // 4:(self.aoff + nb) // 4]
        self.aoff += nb
        if dt != F32:
            v = v.bitcast(dt)
        v = v[:, 0:n]
        if len(shape) == 3:
            v = v.rearrange("p (a b) -> p a b", a=shape[1])
        return v

    def consts(self):
        P = self.P
        self.ident = self.fixed("ident", [128, 128], BF16)
        P.op("gpsimd", I("memset", self.ident[:], 1.0), writes=["ident"])
        P.op("gpsimd", I("affine_select", out=self.ident[:], in_=self.ident[:], pattern=[[-1, 128]],
                         compare_op=ALU.is_equal, fill=0.0, base=0, channel_multiplier=1),
             reads=["ident"], writes=["ident"])
        self.eps = self.fixed("eps", [128, 1], F32)
        P.op("vector", I("memset", self.eps[:], EPS), writes=["eps"])


def emit_rstd(C, ss, rstd, n, res_r, res_w):
    P = C.P
    P.op("scalar", I("activation", out=rstd, in_=ss, func=ACT.Ln, scale=1.0 / n, bias=C.eps[:]),
         reads=list(res_r) + ["eps"], writes=res_w)
    P.op("scalar", I("activation", out=rstd, in_=rstd, func=ACT.Exp, scale=-0.5), reads=res_w, writes=res_w)


def alloc_work(C, CW=128):
    C.w = dict(
        junk=C.sb("junk", [128, D], BF16),
        ss=[C.sb(f"ss{i}", [128, 1], F32) for i in range(2)],
        rstd=[C.sb(f"rstd{i}", [128, 1], F32) for i in range(2)],
        tmp=[C.sb(f"tmp{i}", [128, D], F32) for i in range(2)],
        hbf=[C.sb(f"hbf{i}", [128, D], BF16) for i in range(2)],
    )
    C.mw = dict(
        CW=CW,
        wch=[C.sb(f"adaw_ch{i}", [128, 8, CW], F32) for i in range(2)],
        bch=[C.sb(f"adab_ch{i}", [128, CW], F32) for i in range(2)],
        mtmp=[C.sb(f"mtmp{i}", [128, CW], F32) for i in range(2)],
    )


def emit_mod_piece(C, layer, piece, dst, dres, gi):
    P, M = C.P, C.mw
    CW = M["CW"]
    if gi is not None:
        P.op("sync", I("dma_start", out=dst, in_=C.dr["ng"][layer * 4 + gi:layer * 4 + gi + 1, :].partition_broadcast(128)),
             writes=[dres], dma=True)
    adaw_v = C.dr["adaw"][layer].rearrange("(k p) n -> p k n", p=128)
    for n in range(D // CW):
        s = C.mcount % 2
        C.mcount += 1
        col = piece * D + n * CW
        c0 = n * CW
        P.op("sync", I("dma_start", out=M["wch"][s][:], in_=adaw_v[:, :, col:col + CW]), writes=[f"adaw_ch{s}"], dma=True)
        P.op("sync", I("dma_start", out=M["bch"][s][:], in_=C.dr["adab"][layer:layer + 1, col:col + CW].partition_broadcast(128)),
             writes=[f"adab_ch{s}"], dma=True)
        bank = 6 + s
        for k in range(8):
            P.op("tensor", I("matmul", C.pb[bank][:, 0:CW], lhsT=C.cbc[:, k, :], rhs=M["wch"][s][:, k, :], start=(k == 0),
                             stop=(k == 7)), reads=["cbc", f"adaw_ch{s}"], writes=[f"pb{bank}"])
        if gi is None:
            P.op("vector", I("tensor_tensor", out=dst[:, c0:c0 + CW], in0=C.pb[bank][:, 0:CW], in1=M["bch"][s][:], op=ALU.add),
                 reads=[f"pb{bank}", f"adab_ch{s}"], writes=[dres])
        else:
            P.op("vector", I("tensor_tensor", out=M["mtmp"][s][:], in0=C.pb[bank][:, 0:CW], in1=M["bch"][s][:], op=ALU.add),
                 reads=[f"pb{bank}", f"adab_ch{s}"], writes=[f"mtmp{s}"])
            if piece in (1, 4):
                P.op("vector", I("scalar_tensor_tensor", out=dst[:, c0:c0 + CW], in0=M["mtmp"][s][:], scalar=1.0,
                                 in1=dst[:, c0:c0 + CW], op0=ALU.add, op1=ALU.mult), reads=[f"mtmp{s}", dres], writes=[dres])
            else:
                P.op("vector", I("tensor_tensor", out=dst[:, c0:c0 + CW], in0=M["mtmp"][s][:], in1=dst[:, c0:c0 + CW],
                                 op=ALU.mult), reads=[f"mtmp{s}", dres], writes=[dres])


def emit_norm_mod_T(C, xt, xres, A, Ares, B, Bres, hTg, hTg_res, blk, uid, defer=False):
    P = C.P
    W = C.w
    s = uid % 2
    junk, ss, rstd, tmp, hbf = W["junk"], W["ss"][s], W["rstd"][s], W["tmp"][s], W["hbf"][s]
    P.op("vector", I("memset", ss[:], 0.0), writes=[f"ss{s}"])
    P.op("scalar", I("activation", out=junk[:], in_=xt, func=ACT.Square, accum_out=ss[:]),
         reads=[xres, f"ss{s}"], writes=["junk", f"ss{s}"])
    emit_rstd(C, ss[:], rstd[:], D, [f"ss{s}"], [f"rstd{s}"])
    P.op("vector", I("scalar_tensor_tensor", out=tmp[:], in0=xt, scalar=rstd[:], in1=A, op0=ALU.mult, op1=ALU.mult),
         reads=[xres, f"rstd{s}", Ares], writes=[f"tmp{s}"])
    P.op("gpsimd", I("tensor_tensor", out=hbf[:], in0=tmp[:], in1=B, op=ALU.add),
         reads=[f"tmp{s}", Bres], writes=[f"hbf{s}"])
    if not defer:
        emit_norm_T_part2(C, hTg, hTg_res, blk, uid)


def emit_norm_T_part2(C, hTg, hTg_res, blk, uid):
    P = C.P
    s = uid % 2
    hbf = C.w["hbf"][s]
    bank = 4 + s
    pT = C.pb[bank][:].bitcast(BF16)
    for k in range(8):
        P.op("tensor", I("transpose", out=pT[:, k * 128:(k + 1) * 128], in_=hbf[:, k * 128:(k + 1) * 128],
                         identity=C.ident[:]), reads=[f"hbf{s}", "ident"], writes=[f"pb{bank}"])
    P.op("scalar", I("copy", out=hTg[:, :, blk * 128:(blk + 1) * 128],
                     in_=pT.rearrange("p (k t) -> p k t", k=8)), reads=[f"pb{bank}"], writes=[hTg_res])


def t5_lo():
    n = np.arange(0, 4096)
    nf = np.maximum(n, 1).astype(np.float32)
    large = 16 + (np.log(nf / np.float32(16)) / np.float32(math.log(2048 / 16)) * np.float32(16)).astype(np.int32)
    large = np.minimum(large, 31)
    b = np.where(n < 16, n, large)
    assert all((b == k).any() for k in range(32))
    lo = [int(np.argmax(b == k)) for k in range(32)]
    assert lo[31] <= NEAR_DMAX + 128 - 127, lo
    return lo


def emit_setup(C):
    nc, P, dr = C.nc, C.P, C.dr
    C.consts()
    C.mcount = 0
    C.xres = C.fixed("xres", [128, TOK // 128, D], F32)
    C.cbc = C.fixed("cbc", [128, 8, 128], F32)
    C.b31 = C.fixed("b31", [128, 2], F32)
    C.oh = C.fixed("oh_sb", [128, 4], F32)
    C.phase()
    for tt in range(TOK // 128):
        P.op("sync", I("dma_start", out=C.xres[:, tt, :], in_=dr["x"][tt * 128:(tt + 1) * 128, :]), writes=[f"x{tt}"], dma=True)
    P.op("sync", I("dma_start", out=C.oh[:], in_=dr["oh"]), writes=["oh"], dma=True)
    cT_sb = C.sb("cT_sb", [128, 8], F32)
    cact = C.sb("cact", [128, 8], F32)
    ones = C.sb("ones_f", [128, 128], F32)
    P.op("sync", I("dma_start", out=cT_sb[:], in_=dr["cT"]), writes=["cT_sb"], dma=True)
    P.op("scalar", I("activation", out=cact[:], in_=cT_sb[:], func=ACT.Silu), reads=["cT_sb"], writes=["cact"])
    P.op("vector", I("memset", ones[:], 1.0), writes=["ones_f"])
    for k in range(8):
        P.op("vector", I("tensor_scalar", out=C.cbc[:, k, :], in0=ones[:], scalar1=cact[:, k:k + 1], scalar2=None,
                         op0=ALU.mult), reads=["ones_f", "cact"], writes=["cbc"])
    lo = t5_lo()
    tab = C.sb("tab", [128, 2, 32], F32)
    etab = C.sb("etab", [128, 2, 32], F32)
    cdf = C.sb("cdf", [128, 2, 32], F32)
    P.op("sync", I("dma_start", out=tab[:].rearrange("p a b -> p (a b)"), in_=dr["relbT"].partition_broadcast(128)),
         writes=["tab"], dma=True)
    P.op("scalar", I("activation", out=etab[:], in_=tab[:], func=ACT.Exp), reads=["tab"], writes=["etab"])
    P.op("vector", I("tensor_copy", out=cdf[:, :, 0:1], in_=etab[:, :, 0:1]), reads=["etab"], writes=["cdf"])
    P.op("vector", I("tensor_tensor", out=cdf[:, :, 1:32], in0=etab[:, :, 1:32], in1=etab[:, :, 0:31],
                     op=ALU.subtract), reads=["etab"], writes=["cdf"])
    P.op("vector", I("tensor_copy", out=C.b31[:], in_=tab[:, :, 31]), reads=["tab"], writes=["b31"])
    reli = C.sb("reli", [128, 20], I32)
    relf = C.sb("relf", [128, 20], F32)
    Gt = C.sb("Gt", [128, 2, 20], F32)
    Gtmp = C.sb("Gtmp", [128, 20], F32)
    P.op("gpsimd", I("iota", reli[:], pattern=[[1, 20]], base=-511, channel_multiplier=20), writes=["reli"])
    P.op("vector", I("tensor_copy", out=relf[:], in_=reli[:]), reads=["reli"], writes=["relf"])
    P.op("vector", I("memset", Gt[:], 0.0), writes=["Gt"])
    for hh in range(2):
        for b in range(32):
            P.op("vector", I("tensor_scalar", out=Gtmp[:], in0=relf[:], scalar1=float(lo[b]),
                             scalar2=cdf[:, hh, b:b + 1], op0=ALU.is_ge, op1=ALU.mult),
                 reads=["relf", "cdf"], writes=["Gtmp"])
            P.op("vector", I("tensor_tensor", out=Gt[:, hh, :], in0=Gt[:, hh, :], in1=Gtmp[:], op=ALU.add),
                 reads=["Gt", "Gtmp"], writes=["Gt"])
    P.op("sync", I("dma_start", out=dr["Gd"].rearrange("a (p j) -> p a j", j=20), in_=Gt[:]), reads=["Gt"],
         writes=["Gd"], dma=True)
    C.E = C.sb("E", [128, 2, EW], BF16)
    hank = C.sb("hank", [128, EW], BF16)
    antiI = C.sb("antiI", [128, 128], BF16)
    P.op("gpsimd", I("memset", antiI[:], 1.0), writes=["antiI"])
    P.op("gpsimd", I("affine_select", out=antiI[:], in_=antiI[:], pattern=[[1, 128]], compare_op=ALU.is_equal,
                     fill=0.0, base=-127, channel_multiplier=1), reads=["antiI"], writes=["antiI"])
    for hh in range(2):
        src = bass.AP(dr["Gd_t"], hh * 2560, [[1, 128], [1, EW]])
        P.op("gpsimd", I("dma_start", out=hank[:], in_=src), reads=["Gd"], writes=["hank"], dma=True)
        for c0 in range(0, EW, 512):
            w = min(512, EW - c0)
            P.op("tensor", I("matmul", C.pb[7][:, 0:w], lhsT=antiI[:], rhs=hank[:, c0:c0 + w], start=True, stop=True),
                 reads=["antiI", "hank"], writes=["pb7"])
            P.op("vector", I("tensor_copy", out=C.E[:, hh, c0:c0 + w], in_=C.pb[7][:, 0:w]), reads=["pb7"], writes=["E"])
    P.op("sync", I("dma_start", out=dr["E_d"], in_=C.E.rearrange("p a u -> p (a u)")), reads=["E"], writes=["E_d"], dma=True)


def emit_phase_A(C, layer):
    P, dr = C.P, C.dr
    C.phase()
    alloc_work(C, 512)
    A0 = C.sb("A0", [128, D], F32)
    shA = C.sb("shA", [128, D], F32)
    hTg = [C.sb(f"hTg{i}", [128, 8, 512], BF16) for i in range(2)]
    emit_mod_piece(C, layer, 1, A0, "A0", 0)
    emit_mod_piece(C, layer, 0, shA, "shA", None)
    for tt in range(TOK // 128):
        g, blk = tt // 4, tt % 4
        emit_norm_mod_T(C, C.xres[:, tt, :], f"x{tt}", A0, "A0", shA, "shA", hTg[g % 2], f"hTg{g % 2}", blk, tt)
        if blk == 3:
            nm = f"{layer}_{g}"
            P.op("sync", I("dma_start", out=dr[f"hT_loc{nm}"].rearrange("(k p) t -> p k t", p=128), in_=hTg[g % 2]),
                 reads=[f"hTg{g % 2}"], writes=[f"hT_loc{nm}"], dma=True)
            P.op("gpsimd", I("collective_compute", "AllGather", ALU.bypass, replica_groups=[[0, 1, 2, 3], [4, 5, 6, 7]],
                             ins=[dr[f"hT_loc{nm}_cc"].opt()], outs=[dr[f"hT_all{nm}_cc"].opt()]),
                 reads=[f"hT_loc{nm}"], writes=[f"hT_all{nm}"], cc=True)


def emit_phase_B(C, layer, kind):
    P, dr = C.P, C.dr
    moba = kind == "moba"
    DK = 128 if moba else 64
    scale = DK ** -0.5
    b31 = C.b31
    C.phase()
    E = C.sb("E", [128, 2, EW], BF16)
    P.op("sync", I("dma_start", out=E.rearrange("p a u -> p (a u)"), in_=dr["E_d"]), reads=["E_d"], writes=["E"], dma=True)
    hT_all = [dr[f"hT_all{layer}_{g}"].rearrange("(r k p) t -> r p k t", r=4, k=8) for g in range(4)]
    oT_loc = [dr[f"oT_loc{layer}_{c}"].rearrange("(h p) t -> h p t", h=2) for c in range(4)]
    wsb = C.sb("wsb", [128, 8, 768], BF16)
    P.op("gpsimd", I("dma_start", out=wsb, in_=dr["wqkv"][layer].rearrange("(k p) n -> p k n", p=128)), writes=["wsb"], dma=True)
    if moba:
        selM = C.sb("selM", [32, 32 * 128], BF16)
        P.op("gpsimd", I("memset", selM, 1.0), writes=["selM"])
        P.op("gpsimd", I("affine_select", out=selM, in_=selM, pattern=[[1, 4096]], compare_op=ALU.is_ge,
                         fill=0.0, base=0, channel_multiplier=-128), reads=["selM"], writes=["selM"])
        P.op("gpsimd", I("affine_select", out=selM, in_=selM, pattern=[[-1, 4096]], compare_op=ALU.is_ge,
                         fill=0.0, base=127, channel_multiplier=128), reads=["selM"], writes=["selM"])
        kmf = C.sb("kmf", [128, 32], F32)
        kmb = C.sb("kmb", [128, 32], BF16)
        gate = [C.sb(f"gate{i}", [128, 32], F32) for i in range(2)]
        m8 = [C.sb(f"m8{i}", [128, 8], F32) for i in range(2)]
        sel = [C.sb(f"sel{i}", [128, 32], F32) for i in range(2)]
        nmq = [C.sb(f"nmq{i}", [128, 32], BF16) for i in range(2)]
        nmT = [C.sb(f"nmT{i}", [32, 512], BF16) for i in range(2)]
    else:
        lam_sb = C.sb("lam_sb", [128, 256], F32)
        lp = C.sb("lp", [128, 128], F32)
        ls = C.sb("ls", [128, 2], F32)
        le = C.sb("le", [128, 2], F32)
        neglam = C.sb("neglam", [128, 1], F32)
        subg_bc = C.sb("subg_bc", [128, 128], F32)
        P.op("sync", I("dma_start", out=lam_sb, in_=dr["lam"].partition_broadcast(128)), writes=["lam_sb"], dma=True)
        P.op("sync", I("dma_start", out=subg_bc, in_=dr["subg"].partition_broadcast(128)), writes=["subg_bc"], dma=True)
        lv = lam_sb.rearrange("p (a b c) -> p a b c", a=2, b=2)
        P.op("vector", I("tensor_tensor", out=lp.rearrange("p (a c) -> p a c", a=2), in0=lv[:, :, 0, :],
                         in1=lv[:, :, 1, :], op=ALU.mult), reads=["lam_sb"], writes=["lp"])
        P.op("vector", I("tensor_reduce", out=ls, in_=lp.rearrange("p (a c) -> p a c", a=2), axis=AX.X,
                         op=ALU.add), reads=["lp"], writes=["ls"])
        P.op("scalar", I("activation", out=le, in_=ls, func=ACT.Exp), reads=["ls"], writes=["le"])
        P.op("vector", I("tensor_tensor", out=neglam, in0=le[:, 1:2], in1=le[:, 0:1], op=ALU.subtract),
             reads=["le"], writes=["neglam"])
        P.op("vector", I("tensor_scalar", out=neglam, in0=neglam, scalar1=-LAMBDA_INIT, scalar2=None,
                         op0=ALU.add), reads=["neglam"], writes=["neglam"])
        P.op("vector", I("tensor_scalar", out=subg_bc, in0=subg_bc, scalar1=1.0 - LAMBDA_INIT, scalar2=None,
                         op0=ALU.mult), reads=["subg_bc"], writes=["subg_bc"])
        t1 = [C.sb(f"t1_{i}", [128, 128], F32) for i in range(2)]
        of = [C.sb(f"of{i}", [128, 128], F32) for i in range(2)]
        junk = C.sb("junkd", [128, 128], BF16)
        ssd = [C.sb(f"ssd{i}", [128, 1], F32) for i in range(2)]
        rsd = [C.sb(f"rsd{i}", [128, 1], F32) for i in range(2)]
    QT = C.sb("QT", [128, SEQ], BF16)
    KT = C.sb("KT", [128, SEQ], BF16)
    Vp = C.sb("Vp", [128, 64, 130], BF16)
    hTg = [C.sb(f"hTg{i}", [128, 8, 512], BF16) for i in range(2)]
    NMAP = 1 if moba else 2
    PT = [C.sb(f"PT{i}", [128, 512], BF16) for i in range(4)]
    rec = [C.sb(f"rec{i}", [128, 2], F32) for i in range(2)]
    obf = [C.sb(f"obf{i}", [128, 128], BF16) for i in range(2)]
    OTg = [C.sb(f"OTg{i}", [128, 512], BF16) for i in range(2)]
    def cast_weights():
        for r0 in range(0, D, 256):
            P.op("gpsimd", I("dma_start", out=dr[f"wo_bf{layer}"][r0:r0 + 256, :], in_=dr["wo"][layer][r0:r0 + 256, :]),
                 reads=["QT"], writes=[f"wo_bf{layer}"], dma=True)
        for j in range(NJ):
            P.op("gpsimd", I("dma_start", out=dr[f"win_bf{layer}"][j], in_=dr["win_r"][layer * NJ + j]),
                 reads=["QT"], writes=[f"win_bf{layer}"], dma=True)
        for r0 in range(0, DFF, 256):
            P.op("gpsimd", I("dma_start", out=dr[f"wout_bf{layer}"][r0:r0 + 256, :], in_=dr["wout"][layer][r0:r0 + 256, :]),
                 reads=["QT"], writes=[f"wout_bf{layer}"], dma=True)

    cnt = dict(u=0, g=0, f=0)
    STB = [0, 1, 2, 6]
    for hh in range(2):
        P.op("vector", I("memset", Vp[:, :, 128:129], 1.0), reads=[], writes=["Vp"])
        for gi_ in range(16):
            g = (gi_ // 4) + 4 * (gi_ % 4)
            s = gi_ % 2
            r = g // 4
            P.op("sync", I("dma_start", out=hTg[s], in_=hT_all[g % 4][r]),
                 reads=[f"hT_all{layer}_{g % 4}"], writes=[f"hTg{s}"], dma=True)
            for which, dst, eng in ((0, QT, "scalar"), (1, KT, "vector")):
                bank = which
                c0 = hh * 384 + which * 128
                for k in range(8):
                    P.op("tensor", I("matmul", C.pb[bank][:], lhsT=wsb[:, k, c0:c0 + 128], rhs=hTg[s][:, k, :],
                                     start=(k == 0), stop=(k == 7)), reads=["wsb", f"hTg{s}"], writes=[f"pb{bank}"])
                if eng == "scalar":
                    P.op("scalar", I("copy", out=dst[:, g * 512:(g + 1) * 512], in_=C.pb[bank][:]),
                         reads=[f"pb{bank}"], writes=["QT"])
                else:
                    P.op("vector", I("tensor_copy", out=dst[:, g * 512:(g + 1) * 512], in_=C.pb[bank][:]),
                         reads=[f"pb{bank}"], writes=["KT"])
            c0 = hh * 384 + 256
            for i in range(4):
                for k in range(8):
                    P.op("tensor", I("matmul", C.pb[2][:, i * 128:(i + 1) * 128], lhsT=hTg[s][:, k, i * 128:(i + 1) * 128],
                                     rhs=wsb[:, k, c0:c0 + 128], start=(k == 0), stop=(k == 7)),
                         reads=["wsb", f"hTg{s}"], writes=["pb2"])
            P.op("vector", I("tensor_copy", out=Vp[:, g * 4:(g + 1) * 4, 0:128],
                             in_=C.pb[2][:].rearrange("p (i d) -> p i d", i=4)), reads=["pb2"], writes=["Vp"])
        if hh == 0:
            cast_weights()
        if moba:
            P.op("vector", I("tensor_reduce", out=kmf, in_=KT.rearrange("p (b t) -> p b t", t=256), axis=AX.X,
                             op=ALU.add), reads=["KT"], writes=["kmf"])
            P.op("vector", I("tensor_copy", out=kmb, in_=kmf), reads=["kmf"], writes=["kmb"])
        def gating(qg):
            ns = qg % 2
            for i in range(4):
                qt = qg * 4 + i
                own = qt // 2
                gs = cnt["g"] % 2
                cnt["g"] += 1
                if own <= 3:
                    P.op("vector", I("memset", sel[gs], 0.0), writes=[f"sel{gs}"])
                    P.op("vector", I("memset", sel[gs][:, 0:own + 1], 1.0), writes=[f"sel{gs}"])
                else:
                    P.op("tensor", I("matmul", C.pb[7][:, 0:32], lhsT=QT[:, qt * 128:(qt + 1) * 128], rhs=kmb,
                                     start=True, stop=True), reads=["QT", "kmb"], writes=["pb7"])
                    P.op("vector", I("tensor_copy", out=gate[gs], in_=C.pb[7][:, 0:32]), reads=["pb7"],
                         writes=[f"gate{gs}"])
                    P.op("vector", I("memset", gate[gs][:, own:32], -1e30), writes=[f"gate{gs}"])
                    P.op("vector", I("max", out=m8[gs], in_=gate[gs]), reads=[f"gate{gs}"], writes=[f"m8{gs}"])
                    P.op("vector", I("tensor_scalar", out=sel[gs], in0=gate[gs], scalar1=m8[gs][:, 2:3],
                                     scalar2=None, op0=ALU.is_ge), reads=[f"gate{gs}", f"m8{gs}"],
                         writes=[f"sel{gs}"])
                    P.op("vector", I("memset", sel[gs][:, own:own + 1], 1.0), writes=[f"sel{gs}"])
                P.op("vector", I("tensor_scalar", out=nmq[gs], in0=sel[gs], scalar1=-1.0, scalar2=30000.0,
                                 op0=ALU.add, op1=ALU.mult), reads=[f"sel{gs}"], writes=[f"nmq{gs}"])
                pT7 = C.pb[7][:].bitcast(BF16)
                P.op("tensor", I("transpose", out=pT7[0:32, 512:640], in_=nmq[gs], identity=C.ident[:]),
                     reads=[f"nmq{gs}", "ident"], writes=["pb7"])
                P.op("vector", I("tensor_copy", out=nmT[ns][:, i * 128:(i + 1) * 128], in_=pT7[0:32, 512:640]),
                     reads=["pb7"], writes=[f"nmT{ns}"])

        def emit_qk(pr):
            qg, kt, mp, u = pr
            q0 = qg * 512
            sb_ = STB[u % 4]
            r0 = mp * 64 if not moba else 0
            need_mask = moba and (kt // 2) != 2 * qg + 1
            P.op("tensor", I("matmul", C.pb[sb_][:], lhsT=KT[r0:r0 + DK, kt * 128:(kt + 1) * 128],
                             rhs=QT[r0:r0 + DK, q0:q0 + 512], start=True, stop=not need_mask),
                 reads=["KT", "QT"], writes=[f"pb{sb_}"])
            if need_mask:
                j = kt // 2
                P.op("tensor", I("matmul", C.pb[sb_][:], lhsT=selM[:, j * 128:(j + 1) * 128], rhs=nmT[qg % 2],
                                 start=False, stop=True), reads=["selM", f"nmT{qg % 2}"], writes=[f"pb{sb_}"])

        def emit_rest(pr):
            qg, kt, mp, u = pr
            q0 = qg * 512
            sb_, ps_ = STB[u % 4], u % 4
            Dq = q0 - kt * 128
            if Dq <= NEAR_DMAX:
                P.op("scalar", I("activation", out=PT[ps_], in_=C.pb[sb_][:], func=ACT.Exp, scale=scale),
                     reads=[f"pb{sb_}"], writes=[f"PT{ps_}"])
                P.op("vector", I("tensor_tensor", out=PT[ps_], in0=PT[ps_],
                                 in1=E[:, hh, Dq + OFF:Dq + OFF + 512], op=ALU.mult),
                     reads=[f"PT{ps_}", "E"], writes=[f"PT{ps_}"])
            else:
                P.op("scalar", I("activation", out=PT[ps_], in_=C.pb[sb_][:], func=ACT.Exp, scale=scale,
                                 bias=b31[:, hh:hh + 1]), reads=[f"pb{sb_}", "b31"], writes=[f"PT{ps_}"])
            for i in range(4):
                if kt > 4 * qg + i:
                    continue
                a_ = mp * 4 + i
                ob = 3 + a_ // 3
                oc = (a_ % 3) * 160
                P.op("tensor", I("matmul", C.pb[ob][:, oc:oc + 129], lhsT=PT[ps_][:, i * 128:(i + 1) * 128],
                                 rhs=Vp[:, kt, 0:129], start=(kt == 0 and a_ % 3 == 0), stop=(kt == 4 * qg + i),
                                 skip_group_check=True),
                     reads=[f"PT{ps_}", "Vp"], writes=[f"pb{ob}"])

        def finalize(qg):
            q0 = qg * 512
            og = qg % 2
            pT7 = C.pb[7][:].bitcast(BF16)
            for i in range(4):
                fs = cnt["f"] % 2
                cnt["f"] += 1
                ob0, oc = 3 + i // 3, (i % 3) * 160
                P.op("vector", I("reciprocal", out=rec[fs][:, 0:1], in_=C.pb[ob0][:, oc + 128:oc + 129]),
                     reads=[f"pb{ob0}"], writes=[f"rec{fs}"])
                if moba:
                    P.op("vector", I("tensor_scalar", out=obf[fs], in0=C.pb[ob0][:, oc:oc + 128],
                                     scalar1=rec[fs][:, 0:1], scalar2=None, op0=ALU.mult),
                         reads=[f"pb{ob0}", f"rec{fs}"], writes=[f"obf{fs}"])
                else:
                    ob1, oc1 = 3 + (4 + i) // 3, ((4 + i) % 3) * 160
                    P.op("vector", I("reciprocal", out=rec[fs][:, 1:2], in_=C.pb[ob1][:, oc1 + 128:oc1 + 129]),
                         reads=[f"pb{ob1}"], writes=[f"rec{fs}"])
                    P.op("vector", I("tensor_tensor", out=rec[fs][:, 1:2], in0=rec[fs][:, 1:2], in1=neglam,
                                     op=ALU.mult), reads=[f"rec{fs}", "neglam"], writes=[f"rec{fs}"])
                    P.op("vector", I("tensor_scalar", out=t1[fs], in0=C.pb[ob0][:, oc:oc + 128],
                                     scalar1=rec[fs][:, 0:1], scalar2=None, op0=ALU.mult),
                         reads=[f"pb{ob0}", f"rec{fs}"], writes=[f"t1_{fs}"])
                    P.op("vector", I("scalar_tensor_tensor", out=of[fs], in0=C.pb[ob1][:, oc1:oc1 + 128],
                                     scalar=rec[fs][:, 1:2], in1=t1[fs], op0=ALU.mult, op1=ALU.add),
                         reads=[f"pb{ob1}", f"rec{fs}", f"t1_{fs}"], writes=[f"of{fs}"])
                    P.op("vector", I("memset", ssd[fs], 0.0), writes=[f"ssd{fs}"])
                    P.op("scalar", I("activation", out=junk, in_=of[fs], func=ACT.Square, accum_out=ssd[fs]),
                         reads=[f"of{fs}", f"ssd{fs}"], writes=["junkd", f"ssd{fs}"])
                    emit_rstd(C, ssd[fs], rsd[fs], 128, [f"ssd{fs}"], [f"rsd{fs}"])
                    P.op("vector", I("scalar_tensor_tensor", out=obf[fs], in0=of[fs], scalar=rsd[fs],
                                     in1=subg_bc, op0=ALU.mult, op1=ALU.mult),
                         reads=[f"of{fs}", f"rsd{fs}", "subg_bc"], writes=[f"obf{fs}"])
                P.op("tensor", I("transpose", out=pT7[:, i * 128:(i + 1) * 128], in_=obf[fs], identity=C.ident[:]),
                     reads=[f"obf{fs}", "ident"], writes=["pb7"])
            P.op("scalar", I("copy", out=OTg[og], in_=pT7[:, 0:512]), reads=["pb7"], writes=[f"OTg{og}"])
            ch = qg // 4
            nm = f"{layer}_{ch}"
            P.op("sync", I("dma_start", out=oT_loc[ch][hh][:, (qg % 4) * 512:(qg % 4 + 1) * 512], in_=OTg[og]),
                 reads=[f"OTg{og}"], writes=[f"oT_loc{nm}"], dma=True)
            if hh == 1 and qg % 4 == 3:
                P.op("gpsimd", I("collective_compute", "AllGather", ALU.bypass, replica_groups=[[0, 1, 2, 3], [4, 5, 6, 7]],
                                 ins=[dr[f"oT_loc{nm}_cc"].opt()], outs=[dr[f"oT_all{nm}_cc"].opt()]),
                     reads=[f"oT_loc{nm}"], writes=[f"oT_all{nm}"], cc=True)

        pairs = []
        for qg in range(16):
            for kt in range(4 * qg + 4):
                for mp in range(NMAP):
                    pairs.append((qg, kt, mp, cnt["u"]))
                    cnt["u"] += 1
        G = NMAP
        LOOKG = 3 if moba else 1
        n = len(pairs)
        for base in range(0, n + LOOKG * G, G):
            for idx in range(base, base + G):
                if idx < n:
                    pr = pairs[idx]
                    if moba and pr[1] == 0 and pr[2] == 0:
                        if pr[0] == 0:
                            gating(0)
                        if pr[0] + 1 < 16:
                            gating(pr[0] + 1)
                    emit_qk(pr)
            for idx in range(base - LOOKG * G, base - LOOKG * G + G):
                if 0 <= idx < n:
                    pr = pairs[idx]
                    emit_rest(pr)
                    if pr[1] == 4 * pr[0] + 3 and pr[2] == NMAP - 1:
                        finalize(pr[0])


def resid_update(C, xtile, xres, banks, Grow, Gres, u):
    P, W = C.P, C.w
    R = C.rw
    s = u % 2
    P.op("vector", I("memset", R["ssy"][s], 0.0), writes=[f"ssy{s}"])
    for half in range(2):
        P.op("scalar", I("activation", out=W["junk"][:, 0:512], in_=C.pb[banks[half]][:], func=ACT.Square,
                         accum_out=R["ssy"][s][:, half:half + 1]), reads=[f"pb{banks[half]}", f"ssy{s}"],
             writes=["junk", f"ssy{s}"])
    P.op("vector", I("tensor_tensor", out=R["ss1"][s], in0=R["ssy"][s][:, 0:1], in1=R["ssy"][s][:, 1:2], op=ALU.add),
         reads=[f"ssy{s}"], writes=[f"ss1_{s}"])
    emit_rstd(C, R["ss1"][s], R["rsy"][s], D, [f"ss1_{s}"], [f"rsy{s}"])
    for half in range(2):
        hs = slice(half * 512, (half + 1) * 512)
        P.op("vector", I("scalar_tensor_tensor", out=W["tmp"][s][:, hs], in0=C.pb[banks[half]][:], scalar=R["rsy"][s],
                         in1=Grow[:, hs], op0=ALU.mult, op1=ALU.mult),
             reads=[f"pb{banks[half]}", f"rsy{s}", Gres], writes=[f"tmp{s}"])
    P.op("gpsimd", I("tensor_tensor", out=xtile, in0=xtile, in1=W["tmp"][s], op=ALU.add),
         reads=[xres, f"tmp{s}"], writes=[xres])


def alloc_resid(C):
    C.rw = dict(
        ssy=[C.sb(f"ssy{i}", [128, 2], F32) for i in range(2)],
        ss1=[C.sb(f"ss1_{i}", [128, 1], F32) for i in range(2)],
        rsy=[C.sb(f"rsy{i}", [128, 1], F32) for i in range(2)],
    )


def emit_phase_C1(C, layer):
    P, dr = C.P, C.dr
    C.phase()
    alloc_work(C, 512)
    alloc_resid(C)
    G1 = C.sb("G1", [128, D], F32)
    emit_mod_piece(C, layer, 2, G1, "G1", 1)
    wo_sb = C.sb("wo_sb", [128, 8, D], BF16)
    wo_v = dr[f"wo_bf{layer}"].rearrange("(h p) n -> p h n", p=128)
    for h0 in range(0, 8, 4):
        P.op("sync", I("dma_start", out=wo_sb[:, h0:h0 + 4, :], in_=wo_v[:, h0:h0 + 4, :]), reads=[f"wo_bf{layer}"],
             writes=["wo_sb"], dma=True)
    cand = [C.sb(f"cand{i}", [128, 8, 512], BF16) for i in range(2)]
    OTg = [C.sb(f"OTg{i}", [128, 8, 512], BF16) for i in range(2)]
    oT_all = [dr[f"oT_all{layer}_{c}"].rearrange("(h p) t -> p h t", p=128) for c in range(4)]
    uid = 0
    cc_ = 0
    for tg in range(4):
        gs = tg % 2
        for c in range(4):
            cs = cc_ % 2
            cc_ += 1
            P.op("sync", I("dma_start", out=cand[cs], in_=oT_all[c][:, :, tg * 512:(tg + 1) * 512]),
                 reads=[f"oT_all{layer}_{c}"], writes=[f"cand{cs}"], dma=True)
            for eng, part, hs_ in (("vector", "a", slice(0, 8)),):
                if c == 0:
                    P.op(eng, I("tensor_scalar", out=OTg[gs][:, hs_, :], in0=cand[cs][:, hs_, :], scalar1=C.oh[:, 0:1],
                                scalar2=None, op0=ALU.mult), reads=[f"cand{cs}", "oh"], writes=[f"OTg{gs}{part}"])
                else:
                    P.op(eng, I("scalar_tensor_tensor", out=OTg[gs][:, hs_, :], in0=cand[cs][:, hs_, :],
                                scalar=C.oh[:, c:c + 1], in1=OTg[gs][:, hs_, :], op0=ALU.mult, op1=ALU.add),
                         reads=[f"cand{cs}", "oh", f"OTg{gs}{part}"], writes=[f"OTg{gs}{part}"])
        for i in range(4):
            tt = tg * 4 + i
            banks = (0, 1) if i % 2 == 0 else (2, 3)
            for half in range(2):
                for h in range(8):
                    P.op("tensor", I("matmul", C.pb[banks[half]][:], lhsT=OTg[gs][:, h, i * 128:(i + 1) * 128],
                                     rhs=wo_sb[:, h, half * 512:(half + 1) * 512], start=(h == 0), stop=(h == 7)),
                         reads=[f"OTg{gs}a", "wo_sb"], writes=[f"pb{banks[half]}"])
            resid_update(C, C.xres[:, tt, :], f"x{tt}", banks, G1, "G1", uid)
            uid += 1


def emit_phase_C2(C, layer):
    P, dr = C.P, C.dr
    C.phase()
    alloc_work(C)
    alloc_resid(C)
    A2 = C.sb("A2", [128, D], F32)
    shF = C.sb("shF", [128, D], F32)
    G3 = C.sb("G3", [128, D], F32)
    wout_sb = C.sb("wout_sb", [128, NJ, D], BF16)
    wout_v = dr[f"wout_bf{layer}"].rearrange("(j p) n -> p j n", p=128)
    for j0 in range(0, NJ, 6):
        j1 = min(NJ, j0 + 6)
        P.op("sync", I("dma_start", out=wout_sb[:, j0:j1, :], in_=wout_v[:, j0:j1, :]), reads=[f"wout_bf{layer}"],
             writes=["wout_sb"], dma=True)
    emit_mod_piece(C, layer, 4, A2, "A2", 2)
    emit_mod_piece(C, layer, 3, shF, "shF", None)
    emit_mod_piece(C, layer, 5, G3, "G3", 3)
    h2T = [C.sb(f"h2T{i}", [128, 8, 512], BF16) for i in range(2)]
    actT = C.sb("actT", [128, NJ, 512], BF16)
    wch = [C.sb(f"wch{i}", [128, 2, 8 * 128], BF16) for i in range(2)]
    sg = [C.sb(f"sg{i}", [128, 512], BF16) for i in range(2)]
    uid = 0
    wcount = 0
    def pre1(tg, i):
        tt = tg * 4 + i
        emit_norm_mod_T(C, C.xres[:, tt, :], f"x{tt}", A2, "A2", shF, "shF", h2T[tg % 2], f"h2T{tg % 2}", i, tg * 4 + i,
                        defer=True)

    def pre2(tg, i):
        emit_norm_T_part2(C, h2T[tg % 2], f"h2T{tg % 2}", i, tg * 4 + i)

    for i in range(4):
        pre1(0, i)
        pre2(0, i)
    for tg in range(4):
        for j in range(NJ):
            if tg + 1 < 4 and j % 5 == 1 and j // 5 < 4:
                pre1(tg + 1, j // 5)
            if tg + 1 < 4 and j % 5 == 4 and j // 5 < 4:
                pre2(tg + 1, j // 5)
            ws = wcount % 2
            wcount += 1
            P.op("sync", I("dma_start", out=wch[ws].rearrange("p a n -> p (a n)"), in_=dr[f"win_bf{layer}"][j]),
                 reads=[f"win_bf{layer}"], writes=[f"wch{ws}"], dma=True)
            bg, bu = (0, 1) if j % 2 == 0 else (2, 3)
            for a, bank in ((0, bg), (1, bu)):
                for k in range(8):
                    P.op("tensor", I("matmul", C.pb[bank][:], lhsT=wch[ws][:, a, k * 128:(k + 1) * 128],
                                     rhs=h2T[tg % 2][:, k, :], start=(k == 0), stop=(k == 7)),
                         reads=[f"wch{ws}", f"h2T{tg % 2}"], writes=[f"pb{bank}"])
            s2 = j % 2
            P.op("scalar", I("activation", out=sg[s2], in_=C.pb[bg][:], func=ACT.Silu), reads=[f"pb{bg}"],
                 writes=[f"sg{s2}"])
            P.op("vector", I("tensor_tensor", out=actT[:, j, :], in0=sg[s2], in1=C.pb[bu][:], op=ALU.mult),
                 reads=[f"sg{s2}", f"pb{bu}"], writes=["actT"])
        for i in range(4):
            tt = tg * 4 + i
            banks = (6, 7) if i % 2 == 0 else (0, 1)
            for half in range(2):
                for j in range(NJ):
                    P.op("tensor", I("matmul", C.pb[banks[half]][:], lhsT=actT[:, j, i * 128:(i + 1) * 128],
                                     rhs=wout_sb[:, j, half * 512:(half + 1) * 512], start=(j == 0), stop=(j == NJ - 1)),
                         reads=["actT", "wout_sb"], writes=[f"pb{banks[half]}"])
            resid_update(C, C.xres[:, tt, :], f"x{tt}", banks, G3, "G3", uid)
            uid += 1


def build_fused(stop=None):
    nc = bass.Bass("TRN2", target_bir_lowering=False)
    ext = lambda name, shape, dt=F32: nc.dram_tensor(name, shape, dt, kind="ExternalInput").ap()
    dr = dict(
        x=ext("x", [TOK, D]), cT=ext("cT", [128, 8]), oh=ext("oh", [128, 4]),
        adaw=ext("adaw", [2, D, 6 * D]), adab=ext("adab", [2, 6 * D]), ng=ext("ng", [8, D]),
        wqkv=ext("wqkv", [2, D, 768]), relbT=ext("relbT", [1, 64]), lam=ext("lam", [1, 256]), subg=ext("subg", [1, 128]),
        wo=ext("wo", [2, D, D]), win_r=ext("win_r", [2 * NJ, 128, 2048]), wout=ext("wout", [2, DFF, D]),
    )
    out = nc.dram_tensor("out", [TOK, D], F32, kind="ExternalOutput").ap()
    dr["Gd_t"] = nc.dram_tensor("Gd", [2, 2560], F32)
    dr["Gd"] = dr["Gd_t"].ap()
    dr["E_d"] = nc.dram_tensor("E_d", [128, 2 * EW], BF16).ap()
    for layer in range(2):
        dr[f"wo_bf{layer}"] = nc.dram_tensor(f"wo_bf{layer}", [D, D], BF16).ap()
        dr[f"win_bf{layer}"] = nc.dram_tensor(f"win_bf{layer}", [NJ, 128, 2048], BF16).ap()
        dr[f"wout_bf{layer}"] = nc.dram_tensor(f"wout_bf{layer}", [DFF, D], BF16).ap()
        for j in range(4):
            for nm, rows, cols in (("hT_loc", 8 * 128, 512), ("hT_all", 4 * 8 * 128, 512), ("oT_loc", 2 * 128, TOK),
                                   ("oT_all", 4 * 2 * 128, TOK)):
                t = nc.dram_tensor(f"{nm}{layer}_{j}", [rows, cols // 2], F32).ap()
                dr[f"{nm}{layer}_{j}_cc"] = t
                dr[f"{nm}{layer}_{j}"] = t.bitcast(BF16)
    with ExitStack() as st:
        C = Ctx(nc, st)
        C.dr = dr
        P = C.P
        emit_setup(C)
        seq = []
        for layer in range(2):
            seq += [("A", layer), ("B", layer), ("C1", layer), ("C2", layer)]
        for ph, layer in seq:
            if stop == "S":
                break
            if ph == "A":
                emit_phase_A(C, layer)
            elif ph == "B":
                emit_phase_B(C, layer, "moba" if layer == 0 else "diff")
            elif ph == "C1":
                emit_phase_C1(C, layer)
            else:
                emit_phase_C2(C, layer)
            if stop == f"{ph}{layer}":
                break
        C.P.barrier()
        for tt in range(TOK // 128):
            P.op("sync", I("dma_start", out=out[tt * 128:(tt + 1) * 128, :], in_=C.xres[:, tt, :]), reads=[f"x{tt}"], dma=True)
        print("fused", P.emit())
    return nc


_CACHE = {}


def kernel(x, c, rel_bias, ada_w, ada_b, norm_g, moba_w_qkv, moba_w_o, diff_w_qkv, diff_w_o, diff_lambda,
           diff_subln_g, ffn_w_in, ffn_w_out):
    f = lambda a: np.ascontiguousarray(np.asarray(a, dtype=np.float32))
    x, c, rel_bias, ada_w, ada_b, norm_g = f(x), f(c), f(rel_bias), f(ada_w), f(ada_b), f(norm_g)
    wqkv_l = [f(moba_w_qkv)[0], f(diff_w_qkv)[0]]
    wo = np.ascontiguousarray(np.stack([f(moba_w_o)[0], f(diff_w_o)[0]], 0))
    lam, subg = f(diff_lambda).reshape(1, 256), f(diff_subln_g).reshape(1, 128)
    ffn_w_in, ffn_w_out = f(ffn_w_in), f(ffn_w_out)
    win_r = np.ascontiguousarray(np.concatenate([
        np.stack([ffn_w_in[l][:, :DFF].reshape(8, 128, NJ, 128), ffn_w_in[l][:, DFF:].reshape(8, 128, NJ, 128)], 0)
        .transpose(3, 2, 0, 1, 4).reshape(NJ, 128, 2048) for l in range(2)], 0))
    ng = np.ascontiguousarray(norm_g.reshape(8, D))
    if "nc" not in _CACHE:
        _CACHE["nc"] = build_fused()
    in_maps = []
    for cc in range(NCORES):
        b, r = cc // 4, cc % 4
        hs = [2 * r, 2 * r + 1]
        ws = []
        for l in range(2):
            wq, wk, wv = np.split(wqkv_l[l], 3, axis=1)
            ws.append(np.concatenate([np.concatenate([m[:, h * 128:(h + 1) * 128] for m in (wq, wk, wv)], 1) for h in hs], 1))
        oh = np.zeros((128, 4), np.float32)
        oh[:, r] = 1.0
        in_maps.append(dict(
            x=np.ascontiguousarray(x[b, r * TOK:(r + 1) * TOK, :]), cT=np.ascontiguousarray(c[b].reshape(8, 128).T), oh=oh,
            adaw=ada_w, adab=ada_b, ng=ng, wqkv=np.ascontiguousarray(np.stack(ws, 0)),
            relbT=np.ascontiguousarray(rel_bias[:, hs].T.reshape(1, 64)), lam=lam, subg=subg,
            wo=wo, win_r=win_r, wout=ffn_w_out))
    res = run_bass_kernel_spmd(_CACHE["nc"], in_maps, core_ids=list(range(NCORES))).results
    out = np.empty((2, SEQ, D), np.float32)
    for cc in range(NCORES):
        out[cc // 4, (cc % 4) * TOK:(cc % 4 + 1) * TOK, :] = res[cc]["out"]
    return out
```
